# Optimizing a Trainium2 kernel written in Bass

```python
import math
import jax, jax.numpy as jnp
from jax import lax
import numpy as np

D_MODEL = 2048
BATCH = 8
SEQ = 2048
DEPTH = 2

A_HEADS = 16
A_KV_HEADS = 2
A_HEAD_DIM = 64
A_WINDOW = 128
A_BLOCK = 128
REL_BUCKETS = 32
REL_MAX_DIST = 128
B_HEADS = 8
B_DK = 128
B_DV = 128
B_CHUNK = 64
C_GROUPS = 8
C_GROUP_DIM = 128
C_CHUNK = 128
D_CHANNELS = 1024
D_CONV = 31

A_QW = A_HEADS * A_HEAD_DIM
A_KVW = A_KV_HEADS * A_HEAD_DIM
B_KW = B_HEADS * B_DK
B_VW = B_HEADS * B_DV
C_W = C_GROUPS * C_GROUP_DIM
EVEN_IN = A_QW + 2 * A_KVW + 2 * B_KW + 2 * B_VW
EVEN_MIX = A_QW + B_VW
EVEN_SPLITS = [A_QW, A_QW + A_KVW, A_QW + 2 * A_KVW, A_QW + 2 * A_KVW + B_KW,
               A_QW + 2 * A_KVW + 2 * B_KW, A_QW + 2 * A_KVW + 2 * B_KW + B_VW]
ODD_IN = 2 * C_W + 2 * D_CHANNELS
ODD_MIX = C_W + D_CHANNELS
ODD_SPLITS = [C_W, 2 * C_W, 2 * C_W + D_CHANNELS]

N_GROUPS = 4
EXPERTS_PER_GROUP = 8
N_EXPERTS = N_GROUPS * EXPERTS_PER_GROUP
TOP_K = 2
D_EXPERT = 512
MOE_BLOCK = 256

DN_ALPHA = (2 * DEPTH) ** 0.25
DN_BETA = (8 * DEPTH) ** -0.25
LN_EPS = 1e-5
RMS_EPS = 1e-6
N_EVEN = (DEPTH + 1) // 2
N_ODD = DEPTH // 2

kernel_name = 'hybrid_swa_hgrn2_gmlp_conv_hmoe'


def layer_norm(x, g, b):
    xf = x.astype(jnp.float32)
    mu = jnp.mean(xf, axis=-1, keepdims=True)
    var = jnp.mean(jnp.square(xf - mu), axis=-1, keepdims=True)
    y = (xf - mu) * lax.rsqrt(var + LN_EPS)
    return (y * g.astype(jnp.float32) + b.astype(jnp.float32)).astype(x.dtype)


def t5_causal_bucket(dist):
    max_exact = REL_BUCKETS // 2
    d = jnp.maximum(dist, 1).astype(jnp.float32)
    large = max_exact + (jnp.log(d / max_exact) / math.log(REL_MAX_DIST / max_exact)
                         * (REL_BUCKETS - max_exact)).astype(jnp.int32)
    large = jnp.minimum(large, REL_BUCKETS - 1)
    return jnp.where(dist < max_exact, dist, large)


def sliding_window_sink_attention(q, k, v, sinks, rel_bias):
    bsz, seq = q.shape[0], q.shape[1]
    nb = seq // A_BLOCK
    grp = A_HEADS // A_KV_HEADS
    qb = q.reshape(bsz, nb, A_BLOCK, A_KV_HEADS, grp, A_HEAD_DIM)
    pad = jnp.zeros((bsz, A_BLOCK, A_KV_HEADS, A_HEAD_DIM), k.dtype)

    def band(t):
        tp = jnp.concatenate([pad, t], axis=1)
        prev = tp[:, :seq].reshape(bsz, nb, A_BLOCK, A_KV_HEADS, A_HEAD_DIM)
        cur = t.reshape(bsz, nb, A_BLOCK, A_KV_HEADS, A_HEAD_DIM)
        return jnp.concatenate([prev, cur], axis=2)

    kb, vb = band(k), band(v)
    scores = jnp.einsum('bnqhgd,bnkhd->bnhgqk', qb, kb).astype(jnp.float32) * (A_HEAD_DIM ** -0.5)
    t_loc = jnp.arange(A_BLOCK, dtype=jnp.int32)[:, None]
    s_loc = jnp.arange(2 * A_BLOCK, dtype=jnp.int32)[None, :]
    dist = t_loc + A_BLOCK - s_loc
    bucket = t5_causal_bucket(jnp.maximum(dist, 0))
    bias = rel_bias.astype(jnp.float32)[bucket]
    bias = bias.transpose(2, 0, 1).reshape(A_KV_HEADS, grp, A_BLOCK, 2 * A_BLOCK)
    key_pos = (jnp.arange(nb, dtype=jnp.int32)[:, None, None] - 1) * A_BLOCK + s_loc[None]
    valid = (dist >= 0)[None] & (dist < A_WINDOW)[None] & (key_pos >= 0)
    logits = jnp.where(valid[None, :, None, None], scores + bias, -jnp.inf)
    sink = jnp.broadcast_to(sinks.astype(jnp.float32).reshape(1, 1, A_KV_HEADS, grp, 1, 1),
                            logits.shape[:-1] + (1,))
    probs = jax.nn.softmax(jnp.concatenate([logits, sink], axis=-1), axis=-1)[..., :-1]
    out = jnp.einsum('bnhgqk,bnkhd->bnqhgd', probs.astype(v.dtype), vb)
    return out.reshape(bsz, seq, A_QW)


def hgrn2_recurrence(q, f_logit, i, g, lower_bound, norm_g):
    bsz, seq = q.shape[0], q.shape[1]
    nc = seq // B_CHUNK
    qf = jax.nn.silu(q.astype(jnp.float32))
    lb = lower_bound.astype(jnp.float32)
    f = lb + (1.0 - lb) * jax.nn.sigmoid(f_logit.astype(jnp.float32))
    log_f = jnp.log(f)
    k_in = 1.0 - f

    def to_chunks(t, dh):
        return t.reshape(bsz, nc, B_CHUNK, B_HEADS, dh).transpose(1, 0, 3, 2, 4)

    qc = to_chunks(qf, B_DK)
    kc = to_chunks(k_in, B_DK)
    vc = to_chunks(i.astype(jnp.float32), B_DV)
    bc = jnp.cumsum(to_chunks(log_f, B_DK), axis=3)
    causal = jnp.tril(jnp.ones((B_CHUNK, B_CHUNK), dtype=bool))[:, :, None]

    def chunk_step(state, xs):
        qx, kx, vx, bx = xs
        diff = bx[:, :, :, None, :] - bx[:, :, None, :, :]
        decay = jnp.exp(jnp.where(causal, diff, -jnp.inf))
        scores = jnp.einsum('bhtk,bhsk,bhtsk->bhts', qx, kx, decay)
        o = (jnp.einsum('bhts,bhsv->bhtv', scores, vx)
             + jnp.einsum('bhtk,bhkv->bhtv', qx * jnp.exp(bx), state))
        b_last = bx[:, :, -1, :]
        k_dec = kx * jnp.exp(b_last[:, :, None, :] - bx)
        state = state * jnp.exp(b_last)[..., None] + jnp.einsum('bhsk,bhsv->bhkv', k_dec, vx)
        return state, o

    state0 = jnp.zeros((bsz, B_HEADS, B_DK, B_DV), jnp.float32)
    _, oc = lax.scan(chunk_step, state0, (qc, kc, vc, bc))
    o = oc.transpose(1, 0, 3, 2, 4).reshape(bsz, seq, B_HEADS, B_DV)
    o = o * lax.rsqrt(jnp.mean(jnp.square(o), axis=-1, keepdims=True) + RMS_EPS)
    return o.reshape(bsz, seq, B_VW) * norm_g.astype(jnp.float32) * jax.nn.silu(g.astype(jnp.float32))


def chunked_spatial_gating(u, v, ln_g, ln_b, w_s, b_s):
    bsz, seq = u.shape[0], u.shape[1]
    nc = seq // C_CHUNK
    u = jax.nn.gelu(u)
    v = layer_norm(jax.nn.gelu(v), ln_g, ln_b)
    vc = v.reshape(bsz, nc, C_CHUNK, C_GROUPS, C_GROUP_DIM)
    w = w_s * jnp.tril(jnp.ones((C_CHUNK, C_CHUNK), w_s.dtype))
    mixed = jnp.einsum('gts,bnsgc->bntgc', w, vc) + b_s.T[None, None, :, :, None]
    return u * mixed.reshape(bsz, seq, C_W)


def conformer_conv(a, gate, conv_w, conv_b, ln_g, ln_b):
    h = a * jax.nn.sigmoid(gate)
    hp = jnp.pad(h, ((0, 0), (D_CONV - 1, 0), (0, 0)))
    y = lax.conv_general_dilated(hp, conv_w[:, None, :], window_strides=(1,), padding='VALID',
                                 dimension_numbers=('NWC', 'WIO', 'NWC'),
                                 feature_group_count=D_CHANNELS)
    y = layer_norm(y + conv_b, ln_g, ln_b)
    return jax.nn.silu(y)


def hierarchical_moe(x, w_group, b_group, w_router, b_router, w1, w3, w2):
    bsz, seq, dm = x.shape
    n_tok = bsz * seq
    xf = x.reshape(n_tok, dm)
    g_prob = jax.nn.softmax((xf @ w_group).astype(jnp.float32) + b_group.astype(jnp.float32), axis=-1)
    g_w, g_idx = lax.top_k(g_prob, 1)
    e_logits = ((xf @ w_router).astype(jnp.float32) + b_router.astype(jnp.float32))
    e_logits = e_logits.reshape(n_tok, N_GROUPS, EXPERTS_PER_GROUP)
    e_in = jnp.take_along_axis(e_logits, g_idx[:, :, None], axis=1)[:, 0]
    e_top, e_loc = lax.top_k(e_in, TOP_K)
    e_w = jax.nn.softmax(e_top, axis=-1) * g_w
    e_idx = g_idx * EXPERTS_PER_GROUP + e_loc

    m = n_tok * TOP_K
    flat_e = e_idx.reshape(m).astype(jnp.int32)
    flat_tok = jnp.repeat(jnp.arange(n_tok, dtype=jnp.int32), TOP_K)
    flat_w = e_w.reshape(m)
    order = jnp.argsort(flat_e)
    se, stok, sw = flat_e[order], flat_tok[order], flat_w[order]
    counts = jnp.bincount(flat_e, length=N_EXPERTS).astype(jnp.int32)
    starts = jnp.cumsum(counts) - counts
    pcounts = (counts + MOE_BLOCK - 1) // MOE_BLOCK * MOE_BLOCK
    pends = jnp.cumsum(pcounts)
    pstarts = pends - pcounts
    n_blocks = -(-(m + N_EXPERTS * (MOE_BLOCK - 1)) // MOE_BLOCK)
    n_pad = n_blocks * MOE_BLOCK
    dest = pstarts[se] + (jnp.arange(m, dtype=jnp.int32) - starts[se])
    tok_pad = jnp.zeros((n_pad,), jnp.int32).at[dest].set(stok)
    w_pad = jnp.zeros((n_pad,), jnp.float32).at[dest].set(sw)
    blk_start = jnp.arange(n_blocks, dtype=jnp.int32) * MOE_BLOCK
    blk_e = jnp.minimum(jnp.searchsorted(pends, blk_start, side='right'), N_EXPERTS - 1)
    xb = xf[tok_pad].reshape(n_blocks, MOE_BLOCK, dm)

    def expert_block(args):
        xblk, e = args
        h = jax.nn.silu(xblk @ w1[e]) * (xblk @ w3[e])
        return h @ w2[e]

    yb = lax.map(expert_block, (xb, blk_e))
    y = jnp.zeros((n_tok, dm), jnp.float32).at[tok_pad].add(
        yb.reshape(n_pad, dm).astype(jnp.float32) * w_pad[:, None])
    return y.astype(x.dtype).reshape(bsz, seq, dm)


def setup_inputs(seed: int = 0) -> dict:
    key = jax.random.key(seed)
    ks = jax.random.split(key, 28)
    f32 = jnp.float32

    def nrm(k, shape, scale):
        return jax.random.normal(k, shape, f32) * scale

    return {
        'x': nrm(ks[0], (BATCH, SEQ, D_MODEL), 1.0),
        'w_in_ab': nrm(ks[1], (N_EVEN, D_MODEL, EVEN_IN), D_MODEL ** -0.5),
        'attn_sinks': nrm(ks[2], (N_EVEN, A_HEADS), 0.5),
        'rel_bias': nrm(ks[3], (REL_BUCKETS, A_HEADS), 0.3),
        'hgrn_lb_logits': nrm(ks[4], (DEPTH + 1, B_KW), 0.5),
        'hgrn_norm_g': 1.0 + nrm(ks[5], (N_EVEN, B_VW), 0.1),
        'w_out_ab': nrm(ks[6], (N_EVEN, EVEN_MIX, D_MODEL), EVEN_MIX ** -0.5 * DN_BETA),
        'w_in_cd': nrm(ks[7], (N_ODD, D_MODEL, ODD_IN), D_MODEL ** -0.5),
        'gmlp_ln_g': 1.0 + nrm(ks[8], (N_ODD, C_W), 0.1),
        'gmlp_ln_b': nrm(ks[9], (N_ODD, C_W), 0.02),
        'gmlp_w_s': nrm(ks[10], (N_ODD, C_GROUPS, C_CHUNK, C_CHUNK), C_CHUNK ** -0.5),
        'gmlp_b_s': 1.0 + nrm(ks[11], (N_ODD, C_GROUPS, C_CHUNK), 0.1),
        'conv_w': nrm(ks[12], (N_ODD, D_CONV, D_CHANNELS), D_CONV ** -0.5),
        'conv_b': nrm(ks[13], (N_ODD, D_CHANNELS), 0.02),
        'conv_ln_g': 1.0 + nrm(ks[14], (N_ODD, D_CHANNELS), 0.1),
        'conv_ln_b': nrm(ks[15], (N_ODD, D_CHANNELS), 0.02),
        'w_out_cd': nrm(ks[16], (N_ODD, ODD_MIX, D_MODEL), ODD_MIX ** -0.5 * DN_BETA),
        'ln_mix_g': 1.0 + nrm(ks[17], (DEPTH, D_MODEL), 0.1),
        'ln_mix_b': nrm(ks[18], (DEPTH, D_MODEL), 0.02),
        'ln_ffn_g': 1.0 + nrm(ks[19], (DEPTH, D_MODEL), 0.1),
        'ln_ffn_b': nrm(ks[20], (DEPTH, D_MODEL), 0.02),
        'moe_w_group': nrm(ks[21], (DEPTH, D_MODEL, N_GROUPS), D_MODEL ** -0.5),
        'moe_b_group': nrm(ks[22], (DEPTH, N_GROUPS), 0.01),
        'moe_w_router': nrm(ks[23], (DEPTH, D_MODEL, N_EXPERTS), D_MODEL ** -0.5),
        'moe_b_router': nrm(ks[24], (DEPTH, N_EXPERTS), 0.01),
        'moe_w1': nrm(ks[25], (DEPTH, N_EXPERTS, D_MODEL, D_EXPERT), D_MODEL ** -0.5),
        'moe_w3': nrm(ks[26], (DEPTH, N_EXPERTS, D_MODEL, D_EXPERT), D_MODEL ** -0.5),
        'moe_w2': nrm(ks[27], (DEPTH, N_EXPERTS, D_EXPERT, D_MODEL), D_EXPERT ** -0.5 * DN_BETA),
    }


def reference(x, w_in_ab, attn_sinks, rel_bias, hgrn_lb_logits, hgrn_norm_g, w_out_ab,
              w_in_cd, gmlp_ln_g, gmlp_ln_b, gmlp_w_s, gmlp_b_s, conv_w, conv_b, conv_ln_g, conv_ln_b,
              w_out_cd, ln_mix_g, ln_mix_b, ln_ffn_g, ln_ffn_b,
              moe_w_group, moe_b_group, moe_w_router, moe_b_router, moe_w1, moe_w3, moe_w2):
    bsz, seq = x.shape[0], x.shape[1]
    lb_table = jnp.cumsum(jax.nn.softmax(hgrn_lb_logits.astype(jnp.float32), axis=0), axis=0)
    for layer in range(DEPTH):
        j = layer // 2
        if layer % 2 == 0:
            proj = x @ w_in_ab[j]
            qa, ka, va, qb, fb, ib, gb = jnp.split(proj, EVEN_SPLITS, axis=-1)
            ya = sliding_window_sink_attention(
                qa.reshape(bsz, seq, A_HEADS, A_HEAD_DIM),
                ka.reshape(bsz, seq, A_KV_HEADS, A_HEAD_DIM),
                va.reshape(bsz, seq, A_KV_HEADS, A_HEAD_DIM),
                attn_sinks[j], rel_bias)
            yb = hgrn2_recurrence(qb, fb, ib, gb, lb_table[layer], hgrn_norm_g[j]).astype(x.dtype)
            mix = jnp.concatenate([ya, yb], axis=-1) @ w_out_ab[j]
        else:
            proj = x @ w_in_cd[j]
            uc, vc, ad, gd = jnp.split(proj, ODD_SPLITS, axis=-1)
            yc = chunked_spatial_gating(uc, vc, gmlp_ln_g[j], gmlp_ln_b[j], gmlp_w_s[j], gmlp_b_s[j])
            yd = conformer_conv(ad, gd, conv_w[j], conv_b[j], conv_ln_g[j], conv_ln_b[j])
            mix = jnp.concatenate([yc, yd], axis=-1) @ w_out_cd[j]
        x = layer_norm(DN_ALPHA * x + mix, ln_mix_g[layer], ln_mix_b[layer])
        ffn = hierarchical_moe(x, moe_w_group[layer], moe_b_group[layer], moe_w_router[layer],
                               moe_b_router[layer], moe_w1[layer], moe_w3[layer], moe_w2[layer])
        x = layer_norm(DN_ALPHA * x + ffn, ln_ffn_g[layer], ln_ffn_b[layer])
    return x
```

```python
import numpy as np
from contextlib import ExitStack
import concourse.bass as bass
import concourse.mybir as mybir

F32 = mybir.dt.float32
BF16 = mybir.dt.bfloat16
I32 = mybir.dt.int32
U32 = mybir.dt.uint32
AF = mybir.ActivationFunctionType
ALU = mybir.AluOpType
AX = mybir.AxisListType

SEM_ROT = 20000
DMA_K = 6


class Prog:
    ENGS = ['pe', 'act', 'dve', 'pool', 'sp']

    def __init__(self, nc):
        self.nc = nc
        self.ops = []
        self.es = ExitStack()
        self.last_w = {}
        self.readers = {}
        self.ndma = {e: 0 for e in self.ENGS}
        self.dma_ops = {e: [] for e in self.ENGS}

    def sb(self, name, shape, dt):
        return self.es.enter_context(self.nc.sbuf_tensor("sb_" + name, list(shape), dt))

    def ps(self, name, shape, dt=F32):
        return self.es.enter_context(self.nc.psum_tensor("pp_" + name, list(shape), dt))

    def add(self, eng, fn, r=(), w=(), dma=False):
        i = len(self.ops)
        deps = set()
        psk = [k for k in list(r) + list(w) if isinstance(k, str) and k.startswith('ps')]
        r = [k for k in r if k not in psk] + ['phase']
        w = [k for k in w if k not in psk]
        for k in psk:
            lw = self.last_w.get(k)
            if lw is not None and self.ops[lw]['eng'] != eng:
                deps.add(lw)
        for k in r:
            lw = self.last_w.get(k)
            if lw is not None:
                deps.add(lw)
        for k in w:
            lw = self.last_w.get(k)
            if lw is not None:
                deps.add(lw)
            for rd in self.readers.get(k, ()):
                deps.add(rd)
        deps.discard(i)
        op = dict(i=i, eng=eng, fn=fn, deps=deps, dma=dma, sig=False, tag=getattr(self, 'tag', 'x'))
        if dma:
            j = self.ndma[eng]
            self.ndma[eng] += 1
            op['dj'] = j
            self.dma_ops[eng].append(i)
            if j >= DMA_K:
                deps.add(self.dma_ops[eng][j - DMA_K])
        self.ops.append(op)
        for k in psk:
            self.last_w[k] = i
        for k in r:
            self.readers.setdefault(k, []).append(i)
        for k in w:
            self.last_w[k] = i
            self.readers[k] = []
        return i

    def pe(self, fn, r=(), w=()): return self.add('pe', fn, r, w)
    def act(self, fn, r=(), w=()): return self.add('act', fn, r, w)
    def dve(self, fn, r=(), w=()): return self.add('dve', fn, r, w)
    def pool(self, fn, r=(), w=()): return self.add('pool', fn, r, w)
    def dma(self, eng, fn, r=(), w=()): return self.add(eng, fn, r, w, dma=True)

    def emit(self):
        nc = self.nc
        ops = self.ops
        for op in ops:
            if op['eng'] == 'pe' and not op['dma']:
                op['deps'] = {d for d in op['deps'] if not (ops[d]['eng'] == 'pe' and not ops[d]['dma'])}
        cnt = {e: 0 for e in self.ENGS}
        needed = set()
        for op in ops:
            for d in op['deps']:
                needed.add(d)
        for op in ops:
            if op['dma']:
                continue
            if op['i'] in needed:
                cnt[op['eng']] += 1
                op['sidx'] = cnt[op['eng']]
        nsem_eng = {e: (cnt[e] + SEM_ROT - 1) // SEM_ROT for e in self.ENGS}
        sems = {}
        for e in self.ENGS:
            sems[e] = [self.es.enter_context(nc.semaphore(f"s_{e}_{k}")) for k in range(max(1, nsem_eng[e]))]
        dsems = {}
        for e in self.ENGS:
            if self.ndma[e]:
                dsems[e] = [self.es.enter_context(nc.semaphore(f"d_{e}_{k}")) for k in range(DMA_K)]
        know = {e: {x: 0 for x in self.ENGS} for e in self.ENGS}
        know_dma = {e: set() for e in self.ENGS}
        snap_sig = {}
        snap_dma = {}
        nwaits = 0

        def inherit(e, sn):
            ks, kd = sn
            for x in self.ENGS:
                if ks[x] > know[e][x]:
                    know[e][x] = ks[x]
            know_dma[e] |= kd

        for op in ops:
            e = op['eng']
            waits = []
            need_eng = {}
            need_dma = []
            for d in sorted(op['deps']):
                dop = ops[d]
                if dop['dma']:
                    need_dma.append((dop['eng'], dop['dj']))
                else:
                    need_eng[dop['eng']] = max(need_eng.get(dop['eng'], 0), dop['sidx'])
            for x, s in sorted(need_eng.items(), key=lambda t: -t[1]):
                if know[e][x] >= s:
                    continue
                waits.append(('eng', x, s))
                know[e][x] = s
                inherit(e, snap_sig[(x, s)])
            for key in need_dma:
                if key in know_dma[e]:
                    continue
                waits.append(('dma', key[0], key[1]))
                know_dma[e].add(key)
                inherit(e, snap_dma[key])
            op['waits'] = waits
            nwaits += len(waits)
            if op['dma']:
                snap_dma[(e, op['dj'])] = (dict(know[e]), set(know_dma[e]))
            elif 'sidx' in op:
                snap_sig[(e, op['sidx'])] = (dict(know[e]), set(know_dma[e]))
        self.nwaits = nwaits

        def sem_of(x, s):
            return sems[x][(s - 1) // SEM_ROT], (s - 1) % SEM_ROT + 1

        def dsem_of(x, j):
            return dsems[x][j % DMA_K], 16 * (j // DMA_K + 1)

        streams = {e: [op for op in ops if op['eng'] == e] for e in self.ENGS}

        scoped = getattr(self, 'use_scopes', False)

        def run_stream(e, h):
            import itertools
            for tag, grp in itertools.groupby(streams[e], key=lambda o: o.get('tag')):
                if scoped:
                    with nc.named_scope(str(tag)):
                        for op in grp:
                            self._emit_one(e, h, op, sem_of, dsem_of)
                else:
                    for op in grp:
                        self._emit_one(e, h, op, sem_of, dsem_of)

        def _unused(e, h):
            for op in streams[e]:
                for wt in op['waits']:
                    if wt[0] == 'eng':
                        s, v = sem_of(wt[1], wt[2])
                    else:
                        s, v = dsem_of(wt[1], wt[2])
                    h.wait_ge(s, v)
                ins = op['fn'](h)
                if op['dma']:
                    s, v = dsem_of(e, op['dj'])
                    ins.then_inc(s, 16)
                elif 'sidx' in op:
                    s, v = sem_of(e, op['sidx'])
                    ins.then_inc(s, 1)

        with nc.Block() as block:
            if streams['sp']:
                @block.sync
                def _(h):
                    run_stream('sp', h)
            if streams['pe']:
                @block.tensor
                def _(h):
                    run_stream('pe', h)
            if streams['act']:
                @block.scalar
                def _(h):
                    run_stream('act', h)
            if streams['dve']:
                @block.vector
                def _(h):
                    run_stream('dve', h)
            if streams['pool']:
                @block.gpsimd
                def _(h):
                    run_stream('pool', h)

    def _emit_one(self, e, h, op, sem_of, dsem_of):
        for wt in op['waits']:
            if wt[0] == 'eng':
                s, v = sem_of(wt[1], wt[2])
            else:
                s, v = dsem_of(wt[1], wt[2])
            h.wait_ge(s, v)
        ins = op['fn'](h)
        if op['dma']:
            s, v = dsem_of(e, op['dj'])
            ins.then_inc(s, 16)
        elif 'sidx' in op:
            s, v = sem_of(e, op['sidx'])
            ins.then_inc(s, 1)

    def barrier(self):
        self.add('sp', lambda h: h.nop(), r=(), w=['phase'])

    def finish(self, final_deps_keys):
        deps = [self.last_w[k] for k in final_deps_keys]
        i = len(self.ops)
        op = dict(i=i, eng='sp', fn=lambda h: h.nop(), deps=set(deps), dma=False, sig=False)
        self.ops.append(op)


class Arena:
    def __init__(self, P, nbytes):
        self.t = P.sb('arena', [128, nbytes // 4], F32)
        self.n = nbytes // 4
        self.off = 0

    def reset(self):
        self.off = 0

    def f32(self, n):
        assert self.off + n <= self.n, ("arena overflow", self.off, n, self.n)
        ap = self.t[:, self.off:self.off + n]
        self.off += n
        return ap

    def bf16(self, n):
        w = (n + 1) // 2
        return self.f32(w).bitcast(BF16)[:, 0:n]

    def i32(self, n):
        return self.f32(n).bitcast(I32)

import math
from concourse.bass_utils import run_bass_kernel_spmd

S = 2048
D = 2048
NT = 16
NE = 32
CAP = 384
NSLOT = NE * CAP
ALPHA = float((2 * 2) ** 0.25)
LN_EPS = 1e-5
RMS_EPS = 1e-6
NEG = -30000.0


def _t5_bucket(dist):
    max_exact = 16
    d = np.maximum(dist, 1).astype(np.float32)
    large = max_exact + (np.log(d / np.float32(max_exact)) / np.float32(math.log(128 / max_exact))
                         * np.float32(32 - max_exact)).astype(np.int32)
    large = np.minimum(large, 31)
    return np.where(dist < max_exact, dist, large)


def _bc(v, n=128):
    return np.ascontiguousarray(np.broadcast_to(np.asarray(v, np.float32)[None], (n,) + tuple(np.shape(v))))


def prep_shared(inp):
    f32 = np.float32
    sh = {}
    w = np.asarray(inp['w_in_ab'][0])
    cols = []
    for c in range(8):
        cols += list(range(c * 64, c * 64 + 64)) + list(range((8 + c) * 64, (8 + c) * 64 + 64))
    cols += list(range(1024, 1152)) + list(range(1152, 1280))
    for h in range(8):
        for part in range(4):
            b0 = 1280 + part * 1024 + h * 128
            cols += list(range(b0, b0 + 128))
    wp = w[:, cols]
    sh['w_in0'] = np.ascontiguousarray(wp.reshape(16, 128, 42, 128).transpose(2, 1, 0, 3))
    rows = []
    for c in range(8):
        rows += list(range(c * 64, c * 64 + 64)) + list(range((8 + c) * 64, (8 + c) * 64 + 64))
    rows += list(range(1024, 2048))
    wo = np.asarray(inp['w_out_ab'][0])[rows]
    sh['w_out0'] = np.ascontiguousarray(wo.reshape(16, 128, 2048).transpose(1, 0, 2))
    t_loc = np.arange(128, dtype=np.int32)[:, None]
    s_loc = np.arange(256, dtype=np.int32)[None, :]
    dist = t_loc + 128 - s_loc
    bucket = _t5_bucket(np.maximum(dist, 0))
    rb = np.asarray(inp['rel_bias'])
    bg = rb[bucket]
    hb = np.zeros((128, 8, 2, 256), f32)
    for c in range(8):
        hb[:, c, 0, :] = bg[:, :, c]
        hb[:, c, 1, :] = bg[:, :, 8 + c]
    sh['bias_g'] = hb
    valid = (dist >= 0) & (dist < 128)
    sh['maskneg'] = np.where(valid, 0.0, NEG).astype(f32)
    sk = np.asarray(inp['attn_sinks'][0])
    sk2 = np.stack([sk[0:8], sk[8:16]], axis=1)
    sh['sinks'] = _bc(sk2)
    lbl = np.asarray(inp['hgrn_lb_logits'])
    sh['lbl'] = np.ascontiguousarray(lbl.reshape(3, 8, 128).transpose(2, 1, 0))
    sh['normg'] = _bc(np.asarray(inp['hgrn_norm_g'][0]))
    sh['ident'] = np.eye(128, dtype=f32)
    ii = np.arange(128)
    sh['mask_bd'] = ((ii[:, None] <= ii[None, :]) & ((ii[:, None] // 64) == (ii[None, :] // 64))).astype(f32)
    sh['tri_strict'] = (ii[:, None] < ii[None, :]).astype(f32)
    sh['tril_st'] = (ii[:, None] <= ii[None, :]).astype(f32)
    sh['ecap'] = _bc(np.arange(NE, dtype=f32) * CAP)
    sh['pidx'] = (np.arange(128, dtype=f32) + NSLOT).reshape(128, 1)
    for l in range(2):
        sh[f'lnmix{l}'] = np.stack([_bc(inp['ln_mix_g'][l]), _bc(inp['ln_mix_b'][l])], axis=1)
        sh[f'lnffn{l}'] = np.stack([_bc(inp['ln_ffn_g'][l]), _bc(inp['ln_ffn_b'][l])], axis=1)
        wr = np.concatenate([np.asarray(inp['moe_w_group'][l]), np.asarray(inp['moe_w_router'][l])], axis=1)
        sh[f'wr{l}'] = np.ascontiguousarray(wr.reshape(16, 128, 36).transpose(1, 0, 2))
        sh[f'br{l}'] = _bc(np.concatenate([np.asarray(inp['moe_b_group'][l]), np.asarray(inp['moe_b_router'][l])]))
    w1 = np.asarray(inp['w_in_cd'][0])
    sh['w_in1'] = np.ascontiguousarray(w1.reshape(16, 128, 32, 128).transpose(2, 1, 0, 3))
    wo1 = np.asarray(inp['w_out_cd'][0])
    sh['w_out1'] = np.ascontiguousarray(wo1.reshape(16, 128, 2048).transpose(1, 0, 2))
    sh['wsT'] = np.ascontiguousarray(np.asarray(inp['gmlp_w_s'][0]).transpose(2, 0, 1))
    sh['bsT'] = np.ascontiguousarray(np.asarray(inp['gmlp_b_s'][0]).T)
    sh['glnp'] = np.stack([_bc(inp['gmlp_ln_g'][0]), _bc(inp['gmlp_ln_b'][0])], axis=1)
    sh['cw'] = np.ascontiguousarray(np.asarray(inp['conv_w'][0]).reshape(31, 8, 128).transpose(2, 1, 0))
    cp = np.stack([np.asarray(inp['conv_b'][0]), np.asarray(inp['conv_ln_g'][0]), np.asarray(inp['conv_ln_b'][0])], axis=0)
    sh['cpar'] = np.ascontiguousarray(cp.reshape(3, 8, 128).transpose(2, 0, 1))
    sh['moe_w1'] = np.asarray(inp['moe_w1'])
    sh['moe_w3'] = np.asarray(inp['moe_w3'])
    sh['moe_w2'] = np.asarray(inp['moe_w2'])
    return sh


IN_SPECS = dict(
    x_tm=([S, D], 'f'), xT=([D, S], 'f'),
    w_in0=([42, 128, 16, 128], 'f'), w_out0=([128, 16, 2048], 'f'),
    bias_g=([128, 8, 2, 256], 'f'), maskneg=([128, 256], 'f'), sinks=([128, 8, 2], 'f'),
    lbl=([128, 8, 3], 'f'), normg=([128, 1024], 'f'),
    ident=([128, 128], 'f'), mask_bd=([128, 128], 'f'), tri_strict=([128, 128], 'f'), tril_st=([128, 128], 'f'),
    ecap=([128, 32], 'f'), pidx=([128, 1], 'f'),
    lnmix0=([128, 2, 2048], 'f'), lnffn0=([128, 2, 2048], 'f'), lnmix1=([128, 2, 2048], 'f'), lnffn1=([128, 2, 2048], 'f'),
    wr0=([128, 16, 36], 'f'), wr1=([128, 16, 36], 'f'), br0=([128, 36], 'f'), br1=([128, 36], 'f'),
    w_in1=([32, 128, 16, 128], 'f'), w_out1=([128, 16, 2048], 'f'),
    wsT=([128, 8, 128], 'f'), bsT=([128, 8], 'f'), glnp=([128, 2, 1024], 'f'),
    cw=([128, 8, 31], 'f'), cpar=([128, 3, 8], 'f'),
    moe_w1=([2, 32, 2048, 512], 'f'), moe_w3=([2, 32, 2048, 512], 'f'), moe_w2=([2, 32, 512, 2048], 'f'),
)


class K:
    pass


class _Cut(Exception):
    pass


def build(stage=99, debug=None, cut=None, scopes=False):
    nc = bass.Bass("TRN2", target_bir_lowering=False)
    P = Prog(nc)
    P.use_scopes = scopes
    P.tag = 'init'
    din = {}
    for name, (shape, _) in IN_SPECS.items():
        din[name] = nc.dram_tensor(name, list(shape), F32, kind="ExternalInput").ap()
    out_d = nc.dram_tensor("out", [S, D], F32, kind="ExternalOutput").ap()
    dbg_d = None
    if debug:
        dbg_d = nc.dram_tensor("dbg", list(debug), F32, kind="ExternalOutput").ap()
    x1_d = nc.dram_tensor("x1_scr", [S, D], F32, kind="Internal").ap()
    x2_d = nc.dram_tensor("x2_scr", [S, D], F32, kind="Internal").ap()
    x3_d = nc.dram_tensor("x3_scr", [S, D], F32, kind="Internal").ap()
    xg_d = nc.dram_tensor("xg_scr", [NSLOT + 128, D], BF16, kind="Internal").ap()
    yb_d = nc.dram_tensor("yb_scr", [NSLOT + 128, D], BF16, kind="Internal").ap()
    gu_d = nc.dram_tensor("gu_scr", [S, 1024], BF16, kind="Internal").ap()
    x1b_d = nc.dram_tensor("x1b_scr", [S, D], BF16, kind="Internal").ap()

    bigA = P.sb("bigA", [128, 16, 2048], BF16)
    bigB = P.sb("bigB", [128, 16, 2048], BF16)
    AR = Arena(P, 76 * 1024)
    ps = [P.ps(f"ps{i}", [128, 512]) for i in range(8)]
    ident = P.sb("ident", [128, 128], BF16)
    mask_bd = P.sb("mask_bd", [128, 128], BF16)
    tri_strict = P.sb("tri_strict", [128, 128], BF16)
    tril_st = P.sb("tril_st", [128, 128], F32)
    ones_bf = P.sb("ones_bf", [128, 128], BF16)
    ones_f = P.sb("ones_f", [128, 128], F32)
    ecap = P.sb("ecap", [128, 32], F32)
    pidx = P.sb("pidx", [128, 1], F32)
    dests = P.sb("dests", [128, 16, 2], I32)
    wts = P.sb("wts", [128, 16, 2], F32)
    zt = P.sb("zt", [128, 512], BF16)

    k = K()
    k.nc, k.P, k.din, k.ps, k.AR = nc, P, din, ps, AR
    k.bank = 0

    def nb():
        b = k.bank
        k.bank = (k.bank + 1) % 8
        return b
    k.nb = nb
    k.cnt = 0

    def uid(s):
        k.cnt += 1
        return f"{s}#{k.cnt}"

    pdma = lambda out, in_, r=(), w=(): P.dma('pool', lambda h: h.dma_start(out=out, in_=in_), r=r, w=w)
    sdma = lambda out, in_, r=(), w=(): P.dma('sp', lambda h: h.dma_start(out=out, in_=in_), r=r, w=w)

    def mm(out, lhsT, rhs, start, stop, r, w):
        P.pe(lambda h: h.matmul(out, lhsT=lhsT, rhs=rhs, start=start, stop=stop), r=r, w=w)

    pdma(ident[:], din['ident'], w=['ident'])
    pdma(mask_bd[:], din['mask_bd'], w=['mask_bd'])
    pdma(tri_strict[:], din['tri_strict'], w=['tri_strict'])
    sdma(tril_st[:], din['tril_st'], w=['tril_st'])
    sdma(ecap[:], din['ecap'], w=['ecap'])
    sdma(pidx[:], din['pidx'], w=['pidx'])
    P.dve(lambda h: h.memset(ones_bf[:], 1.0), w=['ones_bf'])
    P.dve(lambda h: h.memset(ones_f[:], 1.0), w=['ones_f'])
    zrow = AR.f32(2048)
    P.dve(lambda h: h.memset(zrow, 0.0), w=['zrow'])
    sdma(yb_d[NSLOT:NSLOT + 128, :], zrow.bitcast(BF16)[:, 0:2048], r=['zrow'], w=['yb_trash'])
    P.barrier()

    def load_wblk(dst, src_ap, key):
        pdma(dst, src_ap, w=[key])

    def proj_fm(xT, xkey, wb, wkey, evac):
        for tt in range(4):
            b = nb()
            for kc in range(16):
                mm(ps[b][:, :], wb[:, kc, :], xT[:, kc, tt * 512:(tt + 1) * 512], kc == 0, kc == 15,
                   r=[xkey, wkey], w=[f'ps{b}'])
            evac(tt, b)

    def proj_tm(xT, xkey, wb, wkey, ncol, evac):
        for g in range(4):
            b = nb()
            for jj in range(4):
                j = g * 4 + jj
                for kc in range(16):
                    mm(ps[b][:, jj * 128:jj * 128 + ncol], xT[:, kc, j * 128:(j + 1) * 128], wb[:, kc, 0:ncol],
                       kc == 0, kc == 15, r=[xkey, wkey], w=[f'ps{b}'])
            evac(g, b)

    def phase_a0():
        AR.reset()
        xT, yT = bigA, bigB
        if cut == 'const':
            raise _Cut()
        wblk = [AR.bf16(2048).rearrange("p (a b) -> p a b", b=128) for _ in range(2)]
        wcnt = [0]

        def next_w(blk):
            i = wcnt[0] % 2
            wcnt[0] += 1
            load_wblk(wblk[i], din['w_in0'][blk], f'wblk{i}')
            return wblk[i], f'wblk{i}'
        xsrc = din['xT'].rearrange("(kc p) t -> p kc t", p=128)
        for q in range(4):
            pdma(xT[:, 4 * q:4 * q + 4, :], xsrc[:, 4 * q:4 * q + 4, :], w=[f'xT'])
        if cut == 'xT':
            raise _Cut()
        mark = AR.off
        bm = AR.f32(8 * 512).rearrange("p (c x) -> p c x", x=512)
        kT = AR.bf16(128 + 2048)
        V0 = AR.bf16(2048).rearrange("p (a b) -> p a b", b=128)
        V1 = AR.bf16(2048).rearrange("p (a b) -> p a b", b=128)
        q0 = AR.bf16(2048)
        q1 = AR.bf16(2048)
        tb = [AR.f32(512) for _ in range(4)]
        eb = [AR.f32(512) for _ in range(4)]
        pb = [AR.bf16(512) for _ in range(4)]
        pTb = [AR.bf16(512) for _ in range(4)]
        sinks = AR.f32(16).rearrange("p (c x) -> p c x", x=2)
        mskn = AR.f32(256)
        sm = [AR.f32(16) for _ in range(4)]
        sdma(bm, din['bias_g'].rearrange("p c s k -> p c (s k)"), w=['bm'])
        sdma(mskn, din['maskneg'], w=['mskn'])
        sdma(sinks, din['sinks'], w=['sinks'])
        P.pool(lambda h: h.memset(zt[:], 0.0), w=['zt'])
        k.zkeys = []
        for q in range(NSLOT // 128 + 1):
            zk = uid('xgz')
            sdma(xg_d[q * 128:(q + 1) * 128, :].rearrange("p (a c) -> p a c", c=512), zt[:].unsqueeze(1).to_broadcast([128, 4, 512]), r=['zt'], w=[zk])
            k.zkeys.append(zk)
        P.dve(lambda h: h.tensor_tensor(out=bm.rearrange("p c (s k) -> p (c s) k", k=256),
                                        in0=bm.rearrange("p c (s k) -> p (c s) k", k=256),
                                        in1=mskn.unsqueeze(1).to_broadcast([128, 16, 256]), op=ALU.add),
              r=['bm', 'mskn'], w=['bm'])
        P.pool(lambda h: h.memset(kT[:, 0:128], 0.0), w=['kT'])
        P.pool(lambda h: h.memset(V0, 0.0), w=['V0'])
        P.pool(lambda h: h.memset(V1, 0.0), w=['V1'])
        P.pool(lambda h: h.memset(q0, 0.0), w=['q0'])
        P.pool(lambda h: h.memset(q1, 0.0), w=['q1'])
        if cut == 'setup':
            raise _Cut()
        wb, wk = next_w(8)
        proj_fm(xT, 'xT', wb, wk, lambda tt, b: P.act(
            lambda h: h.copy(out=kT[:, 128 + tt * 512:128 + (tt + 1) * 512], in_=ps[b][:, :]), r=[f'ps{b}'], w=['kT']))
        if cut == 'k':
            raise _Cut()
        wb, wk = next_w(9)

        def ev_v(g, b):
            for jj in range(4):
                j = 4 * g + jj
                P.act(lambda h, j=j, jj=jj: h.copy(out=V0[:, j, 0:64], in_=ps[b][:, jj * 128:jj * 128 + 64]), r=[f'ps{b}'], w=['V0'])
                P.dve(lambda h, j=j, jj=jj: h.tensor_copy(out=V1[:, j, 64:128], in_=ps[b][:, jj * 128 + 64:jj * 128 + 128]), r=[f'ps{b}'], w=['V1'])
        proj_tm(xT, 'xT', wb, wk, 128, ev_v)
        if cut == 'kv':
            raise _Cut()
        for c in range(8):
            if cut == 'att1' and c == 1:
                raise _Cut()
            wb, wk = next_w(c)

            def ev_q(tt, b):
                P.act(lambda h: h.copy(out=q0[0:64, tt * 512:(tt + 1) * 512], in_=ps[b][0:64, :]), r=[f'ps{b}'], w=['q0'])
                P.dve(lambda h: h.tensor_copy(out=q1[64:128, tt * 512:(tt + 1) * 512], in_=ps[b][64:128, :]), r=[f'ps{b}'], w=['q1'])
            proj_fm(xT, 'xT', wb, wk, ev_q)
            for n0 in range(0, 16, 4):
                by = (n0 // 4) % 2
                sbank = [2, 3, 4, 5]
                tbank = [6, 7, 2, 3]
                ctx = []
                for i in range(4):
                    n = n0 + i
                    t, e, p_bf, pT, s = tb[i], eb[i], pb[i], pTb[i], sm[i]
                    d = dict(n=n, t=t, e=e, p=p_bf, pT=pT, tk=f't{i}', ek=f'e{i}', pk=f'p{i}', ptk=f'pT{i}', sk=f'sm{i}',
                             mx=s[:, 0:2], nmx=s[:, 2:4], rs=s[:, 4:6], es=s[:, 6:8], sd=s[:, 8:10], rinv=s[:, 10:12],
                             t3=t.rearrange("p (s k) -> p s k", k=256), e3=e.rearrange("p (s k) -> p s k", k=256),
                             p3=p_bf.rearrange("p (s k) -> p s k", k=256), pT3=pT.rearrange("p (a b) -> p a b", b=128),
                             b=sbank[i], bT=tbank[i], halves=([1] if n == 0 else [0, 1]))
                    ctx.append(d)
                for d in ctx:
                    n, bq = d['n'], d['b']
                    qs = slice(n * 128, (n + 1) * 128)
                    mm(ps[bq][:, 0:256], q0[:, qs], kT[:, n * 128:n * 128 + 256], True, True, r=['q0', 'kT'], w=[f'ps{bq}'])
                    mm(ps[bq][:, 256:512], q1[:, qs], kT[:, n * 128:n * 128 + 256], True, True, r=['q1', 'kT'], w=[f'ps{bq}'])
                for d in ctx:
                    P.dve(lambda h, d=d, c=c: h.scalar_tensor_tensor(out=d['t'], in0=ps[d['b']][:, :], scalar=0.125, in1=bm[:, c, :],
                                                                    op0=ALU.mult, op1=ALU.add), r=[f"ps{d['b']}", 'bm'], w=[d['tk']])
                    if d['n'] == 0:
                        P.pool(lambda h, d=d: h.memset(d['t3'][:, :, 0:128], NEG), w=[d['tk']])
                for d in ctx:
                    P.dve(lambda h, d=d: h.tensor_reduce(out=d['mx'], in_=d['t3'], axis=AX.X, op=ALU.max), r=[d['tk']], w=[d['sk']])
                for d in ctx:
                    P.dve(lambda h, d=d, c=c: h.tensor_tensor(out=d['mx'], in0=d['mx'], in1=sinks[:, c, :], op=ALU.max), r=[d['sk'], 'sinks'], w=[d['sk']])
                for d in ctx:
                    P.dve(lambda h, d=d: h.tensor_scalar(out=d['nmx'], in0=d['mx'], scalar1=-1.0, scalar2=None, op0=ALU.mult), r=[d['sk']], w=[d['sk']])
                for d in ctx:
                    P.dve(lambda h, d=d, c=c: h.tensor_tensor(out=d['sd'], in0=sinks[:, c, :], in1=d['nmx'], op=ALU.add), r=[d['sk'], 'sinks'], w=[d['sk']])
                for d in ctx:
                    for hs in range(2):
                        P.act(lambda h, hs=hs, d=d: h.activation(
                            out=d['e3'][:, hs, :], in_=d['t3'][:, hs, :], func=AF.Exp, bias=d['nmx'][:, hs:hs + 1], scale=1.0,
                            accum_out=d['rs'][:, hs:hs + 1]), r=[d['tk'], d['sk']], w=[f"{d['ek']}{hs}", f"{d['sk']}r{hs}"])
                    P.act(lambda h, d=d: h.activation(out=d['es'], in_=d['sd'], func=AF.Exp), r=[d['sk']], w=[d['sk'] + 'e'])
                for d in ctx:
                    sk = d['sk']
                    P.dve(lambda h, d=d: h.tensor_tensor(out=d['rs'], in0=d['rs'], in1=d['es'], op=ALU.add), r=[sk + 'r0', sk + 'r1', sk + 'e'], w=[sk + 'r0', sk + 'r1'])
                for d in ctx:
                    sk = d['sk']
                    P.dve(lambda h, d=d: h.reciprocal(out=d['rinv'], in_=d['rs']), r=[sk + 'r0', sk + 'r1'], w=[sk + 'i'])
                for d in ctx:
                    P.pool(lambda h, d=d: h.tensor_tensor(
                        out=d['p3'], in0=d['e3'], in1=d['rinv'].unsqueeze(2).to_broadcast([128, 2, 256]), op=ALU.mult),
                        r=[d['ek'] + '0', d['ek'] + '1', d['sk'] + 'i'], w=[d['pk']])
                for d in ctx:
                    bT = d['bT']
                    for hs in range(2):
                        for hf in d['halves']:
                            mm(ps[bT][:, (hs * 2 + hf) * 128:(hs * 2 + hf + 1) * 128], d['p3'][:, hs, hf * 128:(hf + 1) * 128], ident[:],
                               True, True, r=[d['pk'], 'ident'], w=[f'ps{bT}'])
                for i, d in enumerate(ctx):
                    if i % 2 == 0:
                        P.act(lambda h, d=d: h.copy(out=d['pT'], in_=ps[d['bT']][:, :]), r=[f"ps{d['bT']}"], w=[d['ptk']])
                    else:
                        P.dve(lambda h, d=d: h.tensor_copy(out=d['pT'], in_=ps[d['bT']][:, :]), r=[f"ps{d['bT']}"], w=[d['ptk']])
                for d in ctx:
                    n = d['n']
                    items = [(hs, hf) for hs in range(2) for hf in d['halves']]
                    for ii, (hs, hf) in enumerate(items):
                        Vx, vk = (V0, 'V0') if hs == 0 else (V1, 'V1')
                        mm(ps[by][:, (n % 4) * 128:(n % 4 + 1) * 128], Vx[:, n - 1 + hf, :], d['pT3'][:, hs * 2 + hf, :],
                           ii == 0, ii == len(items) - 1, r=[vk, d['ptk']], w=[f'ps{by}'])
                P.dve(lambda h, by=by, c=c, n0=n0: h.tensor_copy(out=yT[:, c, n0 * 128:(n0 + 4) * 128], in_=ps[by][:, :]),
                      r=[f'ps{by}'], w=['yT'])
        if cut == 'att':
            raise _Cut()
        P.barrier()
        P.tag = 'phase_a0_hgrn'
        AR.off = mark
        sq = AR.bf16(2048)
        omf = AR.bf16(2048)
        fA = AR.f32(2048)
        bt = AR.f32(2048)
        tmp1 = AR.f32(2048)
        qd2 = AR.bf16(2048)
        qd = AR.bf16(2048)
        kd = AR.bf16(2048)
        kd2 = AR.bf16(2048)
        kdx = AR.bf16(2048).rearrange("p (j s) -> p j s", s=128)
        Vh = AR.bf16(2048).rearrange("p (j s) -> p j s", s=128)
        gs = AR.bf16(2048).rearrange("p (j s) -> p j s", s=128)
        lbt = AR.f32(24).rearrange("p (h l) -> p h l", l=3)
        lb = AR.f32(8)
        oml = AR.f32(8)
        lsum = AR.f32(8)
        ebl = AR.f32(16)
        Sst = AR.f32(128)
        y_sbs = [AR.bf16(128) for _ in range(2)]
        ss_all = AR.f32(48).rearrange("p (j s) -> p j s", s=3)
        normg = AR.f32(1024)
        tmp2 = fA
        sdma(lbt, din['lbl'], w=['lbt'])
        sdma(normg, din['normg'], w=['normg'])
        P.act(lambda h: h.activation(out=lbt, in_=lbt, func=AF.Exp), r=['lbt'], w=['lbt'])
        P.dve(lambda h: h.tensor_reduce(out=lsum, in_=lbt, axis=AX.X, op=ALU.add), r=['lbt'], w=['lsum'])
        P.dve(lambda h: h.reciprocal(out=lsum, in_=lsum), r=['lsum'], w=['lsum'])
        P.dve(lambda h: h.tensor_tensor(out=lb, in0=lbt[:, :, 0], in1=lsum, op=ALU.mult), r=['lbt', 'lsum'], w=['lb'])
        P.dve(lambda h: h.tensor_scalar(out=oml, in0=lb, scalar1=-1.0, scalar2=1.0, op0=ALU.mult, op1=ALU.add), r=['lb'], w=['oml'])
        P.pool(lambda h: h.memset(kdx, 0.0), w=['kdx'])
        def projqf(hd):
            wb, wk = next_w(10 + 4 * hd + 0)
            proj_fm(xT, 'xT', wb, wk, lambda tt, b: P.act(
                lambda h: h.activation(out=sq[:, tt * 512:(tt + 1) * 512], in_=ps[b][:, :], func=AF.Silu), r=[f'ps{b}'], w=['sq']))
            wb, wk = next_w(10 + 4 * hd + 1)
            proj_fm(xT, 'xT', wb, wk, lambda tt, b: P.act(
                lambda h: h.activation(out=fA[:, tt * 512:(tt + 1) * 512], in_=ps[b][:, :], func=AF.Sigmoid), r=[f'ps{b}'], w=['fA']))
        sqj = AR.f32(256)
        projqf(0)
        for hd in range(8):
            P.dve(lambda h, hd=hd: h.tensor_scalar(out=fA, in0=fA, scalar1=oml[:, hd:hd + 1], scalar2=lb[:, hd:hd + 1],
                                                  op0=ALU.mult, op1=ALU.add), r=['fA', 'oml', 'lb'], w=['fA'])
            P.act(lambda h: h.activation(out=tmp1, in_=fA, func=AF.Ln), r=['fA'], w=['tmp1'])
            for j in range(16):
                P.dve(lambda h, j=j: h.tensor_tensor_scan(out=bt[:, j * 128:(j + 1) * 128], data0=ones_f[:, :],
                                                         data1=tmp1[:, j * 128:(j + 1) * 128], initial=0.0, op0=ALU.mult, op1=ALU.add),
                      r=['tmp1', 'ones_f'], w=['bt'])
            P.pool(lambda h: h.tensor_scalar(out=omf, in0=fA, scalar1=-1.0, scalar2=1.0, op0=ALU.mult, op1=ALU.add), r=['fA'], w=['omf'])
            P.act(lambda h: h.activation(out=tmp1, in_=bt, func=AF.Exp), r=['bt'], w=['tmp1'])
            P.pool(lambda h: h.tensor_tensor(out=qd2, in0=sq, in1=tmp1, op=ALU.mult), r=['sq', 'tmp1'], w=['qd2'])
            bt64 = bt.rearrange("p (c s) -> p c s", s=64)
            P.dve(lambda h: h.tensor_tensor(out=tmp2.rearrange("p (c s) -> p c s", s=64), in0=bt64,
                                            in1=bt64[:, :, 31:32].to_broadcast([128, 32, 64]), op=ALU.subtract), r=['bt', 'omf'], w=['fA'])
            P.act(lambda h: h.activation(out=tmp1, in_=tmp2, func=AF.Exp), r=['fA', 'qd2'], w=['tmp1'])
            P.pool(lambda h: h.tensor_tensor(out=qd, in0=sq, in1=tmp1, op=ALU.mult), r=['sq', 'tmp1'], w=['qd'])
            P.act(lambda h: h.activation(out=tmp1, in_=tmp2, func=AF.Exp, scale=-1.0), r=['fA', 'qd'], w=['tmp1'])
            P.dve(lambda h: h.tensor_tensor(out=kd, in0=omf, in1=tmp1, op=ALU.mult), r=['omf', 'tmp1'], w=['kd'])
            bt128 = bt.rearrange("p (j s) -> p j s", s=128)
            P.dve(lambda h: h.tensor_tensor(out=tmp2.rearrange("p (j s) -> p j s", s=128), in0=bt128,
                                            in1=bt128[:, :, 127:128].to_broadcast([128, 16, 128]), op=ALU.subtract), r=['bt', 'kd'], w=['fA'])
            P.act(lambda h: h.activation(out=tmp1, in_=tmp2, func=AF.Exp, scale=-1.0), r=['fA', 'kd'], w=['tmp1'])
            P.pool(lambda h: h.tensor_tensor(out=kd2, in0=omf, in1=tmp1, op=ALU.mult), r=['omf', 'tmp1'], w=['kd2'])
            P.dve(lambda h: h.tensor_tensor(out=tmp2.rearrange("p (j s) -> p j s", s=128)[:, :, 0:64], in0=bt128[:, :, 0:64],
                                            in1=bt128[:, :, 95:96].to_broadcast([128, 16, 64]), op=ALU.subtract), r=['bt', 'kd2'], w=['fA'])
            P.act(lambda h: h.activation(out=tmp1.rearrange("p (j s) -> p j s", s=128)[:, :, 0:64],
                                         in_=tmp2.rearrange("p (j s) -> p j s", s=128)[:, :, 0:64], func=AF.Exp, scale=-1.0),
                  r=['fA', 'kd2'], w=['tmp1'])
            P.dve(lambda h: h.tensor_tensor(out=kdx[:, :, 0:64], in0=omf.rearrange("p (j s) -> p j s", s=128)[:, :, 0:64],
                                            in1=tmp1.rearrange("p (j s) -> p j s", s=128)[:, :, 0:64], op=ALU.mult),
                  r=['omf', 'tmp1'], w=['kdx'])
            P.act(lambda h: h.activation(out=ebl, in_=bt128[:, :, 127], func=AF.Exp), r=['bt'], w=['ebl'])
            wb, wk = next_w(10 + 4 * hd + 2)
            proj_tm(xT, 'xT', wb, wk, 128, lambda g, b: P.act(
                lambda h: h.copy(out=Vh[:, 4 * g:4 * g + 4, :], in_=ps[b][:, :].rearrange("p (j c) -> p j c", c=128)),
                r=[f'ps{b}'], w=['Vh']))
            wb, wk = next_w(10 + 4 * hd + 3)

            def ev_g(g, b, hd=hd):
                t4 = tmp1[:, 0:512].rearrange("p (j c) -> p j c", c=128)
                P.act(lambda h: h.activation(out=tmp1[:, 0:512], in_=ps[b][:, :], func=AF.Silu), r=[f'ps{b}', 'kdx'], w=['tmp1'])
                P.pool(lambda h: h.tensor_tensor(out=gs[:, 4 * g:4 * g + 4, :], in0=t4,
                                                 in1=normg[:, hd * 128:(hd + 1) * 128].unsqueeze(1).to_broadcast([128, 4, 128]),
                                                 op=ALU.mult), r=['tmp1', 'normg'], w=['gs'])
            proj_tm(xT, 'xT', wb, wk, 128, ev_g)
            A_all = tmp1.bitcast(BF16)[:, 0:2048].rearrange("p (j s) -> p j s", s=128)
            kd2T_all = tmp1.bitcast(BF16)[:, 2048:4096].rearrange("p (j s) -> p j s", s=128)
            Sbf_all = bt.bitcast(BF16)[:, 0:17 * 128].rearrange("p (j s) -> p j s", s=128)
            for j in range(16):
                js = slice(j * 128, (j + 1) * 128)
                bA = nb()
                mm(ps[bA][:, 0:128], kd[:, js], qd[:, js], True, True, r=['kd', 'qd'], w=[f'ps{bA}'])
                mm(ps[bA][:, 128:192], kdx[:, j, :], qd[:, j * 128 + 64:(j + 1) * 128], True, True, r=['kdx', 'qd'], w=[f'ps{bA}'])
                mm(ps[bA][:, 256:384], kd2[:, js], ident[:], True, True, r=['kd2', 'ident'], w=[f'ps{bA}'])
                P.dve(lambda h, bA=bA, j=j: h.tensor_tensor(out=A_all[:, j, :], in0=ps[bA][:, 0:128], in1=mask_bd[:], op=ALU.mult),
                      r=[f'ps{bA}', 'mask_bd', 'ebl', 'gs'], w=['tmp1'])
                P.dve(lambda h, bA=bA, j=j: h.tensor_copy(out=A_all[0:64, j, 64:128], in_=ps[bA][0:64, 128:192]), r=[f'ps{bA}'], w=['tmp1'])
                P.act(lambda h, bA=bA, j=j: h.copy(out=kd2T_all[:, j, :], in_=ps[bA][:, 256:384]), r=[f'ps{bA}', 'tmp1'], w=['tmp1k'])
            if hd + 1 < 8:
                projqf(hd + 1)
            P.dve(lambda h: h.memset(Sst, 0.0), w=['S'])
            P.pool(lambda h: h.memset(Sbf_all[:, 0, :], 0.0), r=['ebl'], w=['bt'])
            bU = None
            for j in range(16):
                if j % 4 == 0:
                    bU = nb()
                mm(ps[bU][:, (j % 4) * 128:(j % 4 + 1) * 128], kd2T_all[:, j, :], Vh[:, j, :], True, True, r=['tmp1k', 'Vh'], w=[f'ps{bU}'])
                P.dve(lambda h, bU=bU, j=j: h.scalar_tensor_tensor(out=Sst, in0=Sst, scalar=ebl[:, j:j + 1], in1=ps[bU][:, (j % 4) * 128:(j % 4 + 1) * 128],
                                                                  op0=ALU.mult, op1=ALU.add), r=['S', 'ebl', f'ps{bU}'], w=['S'])
                P.act(lambda h, j=j: h.copy(out=Sbf_all[:, j + 1, :], in_=Sst), r=['S'], w=['bt'])
            bTr = None
            for j in range(16):
                js = slice(j * 128, (j + 1) * 128)
                bO = nb()
                ysb = y_sbs[j % 2]
                yk = f'y_sb{j % 2}'
                mm(ps[bO][:, 0:128], A_all[:, j, :], Vh[:, j, :], True, False, r=['tmp1', 'Vh'], w=[f'ps{bO}'])
                mm(ps[bO][:, 0:128], qd2[:, js], Sbf_all[:, j, :], False, True, r=['qd2', 'bt'], w=[f'ps{bO}'])
                P.act(lambda h, bO=bO, j=j: h.activation(out=sqj[:, (j % 2) * 128:(j % 2 + 1) * 128], in_=ps[bO][:, 0:128], func=AF.Square,
                                                          accum_out=ss_all[:, j, 0:1]), r=[f'ps{bO}'], w=[f'ss{j}', f'sqj{j % 2}'])
                P.act(lambda h, j=j: h.activation(out=ss_all[:, j, 1:2], in_=ss_all[:, j, 0:1], func=AF.Sqrt, bias=RMS_EPS, scale=1.0 / 128),
                      r=[f'ss{j}'], w=[f'ss1{j}'])
                P.dve(lambda h, j=j: h.reciprocal(out=ss_all[:, j, 2:3], in_=ss_all[:, j, 1:2]), r=[f'ss1{j}'], w=[f'ss2{j}'])
                P.dve(lambda h, bO=bO, j=j, ysb=ysb: h.scalar_tensor_tensor(out=ysb, in0=ps[bO][:, 0:128], scalar=ss_all[:, j, 2:3], in1=gs[:, j, :],
                                                                           op0=ALU.mult, op1=ALU.mult), r=[f'ps{bO}', f'ss2{j}', 'gs'], w=[yk])
                if j % 4 == 0:
                    bTr = nb()
                mm(ps[bTr][:, (j % 4) * 128:(j % 4 + 1) * 128], ysb, ident[:], True, True, r=[yk, 'ident'], w=[f'ps{bTr}'])
                if j % 4 == 3:
                    P.act(lambda h, bTr=bTr, j=j, hd=hd: h.copy(out=yT[:, 8 + hd, (j - 3) * 128:(j + 1) * 128], in_=ps[bTr][:, :]),
                          r=[f'ps{bTr}'], w=['yT'])
        P.barrier()


    def layer_norm_gen(src, skey, dst, dkey, lnp, lnkey, junk, st, jk='junk', sx=''):
        P.act(lambda h: h.activation(out=junk, in_=src, func=AF.Copy, accum_out=st[:, 0:1]), r=[skey], w=[jk, 'st0' + sx])
        yield
        P.act(lambda h: h.activation(out=junk, in_=src, func=AF.Square, accum_out=st[:, 1:2]), r=[skey], w=[jk, 'st1' + sx])
        yield
        P.dve(lambda h: h.tensor_scalar(out=st[:, 2:3], in0=st[:, 0:1], scalar1=1.0 / D, scalar2=None, op0=ALU.mult), r=['st0' + sx], w=['st2' + sx])
        yield
        P.dve(lambda h: h.tensor_tensor(out=st[:, 3:4], in0=st[:, 2:3], in1=st[:, 2:3], op=ALU.mult), r=['st2' + sx], w=['st3' + sx])
        yield
        P.dve(lambda h: h.scalar_tensor_tensor(out=st[:, 4:5], in0=st[:, 1:2], scalar=1.0 / D, in1=st[:, 3:4], op0=ALU.mult, op1=ALU.subtract),
              r=['st1' + sx, 'st3' + sx], w=['st4' + sx])
        yield
        P.act(lambda h: h.activation(out=st[:, 5:6], in_=st[:, 4:5], func=AF.Sqrt, bias=LN_EPS, scale=1.0), r=['st4' + sx], w=['st5' + sx])
        yield
        P.dve(lambda h: h.reciprocal(out=st[:, 6:7], in_=st[:, 5:6]), r=['st5' + sx], w=['st6' + sx])
        yield
        P.dve(lambda h: h.tensor_scalar(out=dst, in0=src, scalar1=st[:, 2:3], scalar2=st[:, 6:7], op0=ALU.subtract, op1=ALU.mult),
              r=[skey, 'st2' + sx, 'st6' + sx], w=[dkey])
        yield
        P.pool(lambda h: h.tensor_tensor(out=dst, in0=dst, in1=lnp[:, 0, :], op=ALU.mult), r=[dkey, lnkey], w=[dkey])
        yield
        P.pool(lambda h: h.tensor_tensor(out=dst, in0=dst, in1=lnp[:, 1, :], op=ALU.add), r=[dkey, lnkey], w=[dkey])
        yield


    def layer_norm_tile(src, skey, dst, dkey, lnp, lnkey, junk, st):
        for _ in layer_norm_gen(src, skey, dst, dkey, lnp, lnkey, junk, st):
            pass

    def zip_run(gens):
        gens = list(gens)
        while gens:
            for g in list(gens):
                try:
                    next(g)
                except StopIteration:
                    gens.remove(g)

    def phase_c(l, xin_d, xout_d):
        AR.reset()
        Wout, yT = bigA, bigB
        wsrc = din[f'w_out{l}']
        for q in range(4):
            pdma(Wout[:, 4 * q:4 * q + 4, :], wsrc[:, 4 * q:4 * q + 4, :], w=['xT'])
        lnp = AR.f32(4096).rearrange("p (a b) -> p a b", b=2048)
        sdma(lnp, din[f'lnmix{l}'], w=['lnp'])
        wr_bf = AR.bf16(16 * 36).rearrange("p (a b) -> p a b", b=36)
        pdma(wr_bf, din[f'wr{l}'], w=['wr'])
        brt = AR.f32(36)
        sdma(brt, din[f'br{l}'], w=['brt'])
        zkeys = k.zkeys if l == 0 else []
        lg_all = AR.f32(16 * 36).rearrange("p (j c) -> p j c", c=36)
        markc = AR.off
        xs = [AR.f32(2048) for _ in range(2)]
        rbufs = [AR.f32(2048) for _ in range(2)]
        x1Ts = [AR.bf16(2048).rearrange("p (a b) -> p a b", b=128) for _ in range(2)]
        sts = [AR.f32(8) for _ in range(2)]
        BIGV = 1000.0

        def load_x(j):
            sdma(xs[j % 2], xin_d[j * 128:(j + 1) * 128, :], w=[f'xs{j % 2}'])

        def tile_c(j):
            pr = j % 2
            x_s, xk = xs[pr], f'xs{pr}'
            rbuf, rk = rbufs[pr], f'rbuf{pr}'
            x1T, st = x1Ts[pr], sts[pr]
            junk_j = x_s.bitcast(BF16)[:, 0:2048]
            xb = x_s.bitcast(BF16)[:, 2048:4096]
            for db in range(4):
                b = nb()
                for fc in range(16):
                    mm(ps[b][:, :], yT[:, fc, j * 128:(j + 1) * 128], Wout[:, fc, db * 512:(db + 1) * 512], fc == 0, fc == 15,
                       r=['yT', 'xT'], w=[f'ps{b}'])
                P.dve(lambda h, b=b, db=db: h.scalar_tensor_tensor(
                    out=rbuf[:, db * 512:(db + 1) * 512], in0=x_s[:, db * 512:(db + 1) * 512], scalar=ALPHA, in1=ps[b][:, :],
                    op0=ALU.mult, op1=ALU.add), r=[xk, f'ps{b}'], w=[f'{rk}_{db}'])
                yield
            P.dve(lambda h: h.tensor_copy(out=st[:, 7:8], in_=st[:, 7:8]), r=[f'{rk}_{d_}' for d_ in range(4)], w=[rk] + [f'{rk}_{d_}' for d_ in range(4)])
            yield
            yield from layer_norm_gen(rbuf, rk, rbuf, rk, lnp, 'lnp', junk_j, st, jk=xk, sx=f'_{pr}')
            sdma(xout_d[j * 128:(j + 1) * 128, :], rbuf, r=[rk], w=[uid('xout'), rk])
            yield
            P.act(lambda h: h.copy(out=xb, in_=rbuf), r=[rk], w=[xk])
            yield
            sdma(x1b_d[j * 128:(j + 1) * 128, :], xb, r=[xk], w=[uid('x1bd'), xk])
            yield
            for g4 in range(4):
                b = nb()
                for q in range(4):
                    kc = g4 * 4 + q
                    mm(ps[b][:, q * 128:(q + 1) * 128], xb[:, kc * 128:(kc + 1) * 128], ident[:], True, True, r=[xk, 'ident'], w=[f'ps{b}'])
                if g4 % 2 == 0:
                    P.act(lambda h, b=b, g4=g4: h.copy(out=x1T[:, 4 * g4:4 * g4 + 4, :], in_=ps[b][:, :].rearrange("p (a b) -> p a b", b=128)),
                          r=[f'ps{b}'], w=[f'x1T{pr}_{g4}'])
                else:
                    P.dve(lambda h, b=b, g4=g4: h.tensor_copy(out=x1T[:, 4 * g4:4 * g4 + 4, :], in_=ps[b][:, :].rearrange("p (a b) -> p a b", b=128)),
                          r=[f'ps{b}'], w=[f'x1T{pr}_{g4}'])
                yield
            b = nb()
            for kc in range(16):
                mm(ps[b][:, 0:36], x1T[:, kc, :], wr_bf[:, kc, :], kc == 0, kc == 15, r=[f'x1T{pr}_{kc // 4}', 'wr'], w=[f'ps{b}'])
            P.dve(lambda h, b=b: h.tensor_tensor(out=lg_all[:, j, :], in0=ps[b][:, 0:36], in1=brt, op=ALU.add), r=[f'ps{b}', 'brt'], w=[uid('lg')])
            yield
        load_x(0)
        load_x(1)
        for j in range(0, NT, 2):
            zip_run([tile_c(j), tile_c(j + 1)])
            if j + 2 < NT:
                load_x(j + 2)
                load_x(j + 3)
        P.barrier()
        AR.off = markc
        V = P.dve
        T3 = lambda n: AR.f32(16 * n).rearrange("p (j c) -> p j c", c=n)
        gmask, eg, pen = T3(4), T3(4), T3(4)
        em, em2, mask1, mask2, tmpe, rank, slot, okm, cs, base = [T3(32) for _ in range(10)]
        m12b = AR.bf16(512).rearrange("p (j c) -> p j c", c=32)
        gmax, gsum, gw, m1, m2, dd, e2, den, w1, w2 = [AR.f32(16) for _ in range(10)]
        dstk, okk, dfin = AR.f32(16), AR.f32(16), AR.f32(16)
        G = lg_all[:, :, 0:4]
        L = lg_all[:, :, 4:36]
        bc = lambda a, n: a.unsqueeze(2).to_broadcast([128, 16, n])
        V(lambda h: h.tensor_reduce(out=gmax, in_=G, axis=AX.X, op=ALU.max), r=['lg'], w=['gmax'])
        V(lambda h: h.tensor_tensor(out=gmask, in0=G, in1=bc(gmax, 4), op=ALU.is_equal), r=['lg', 'gmax'], w=['gmask'])
        V(lambda h: h.tensor_tensor(out=eg, in0=G, in1=bc(gmax, 4), op=ALU.subtract), r=['lg', 'gmax'], w=['eg'])
        P.act(lambda h: h.activation(out=eg, in_=eg, func=AF.Exp), r=['eg'], w=['eg'])
        V(lambda h: h.tensor_reduce(out=gsum, in_=eg, axis=AX.X, op=ALU.add), r=['eg'], w=['gsum'])
        V(lambda h: h.reciprocal(out=gw, in_=gsum), r=['gsum'], w=['gw'])
        V(lambda h: h.tensor_scalar(out=pen, in0=gmask, scalar1=BIGV, scalar2=-BIGV, op0=ALU.mult, op1=ALU.add), r=['gmask'], w=['pen'])
        em64 = em.rearrange("p j (g e) -> p (j g) e", e=8)
        pen64 = pen.rearrange("p j g -> p (j g)")
        V(lambda h: h.tensor_copy(out=em, in_=L), r=['lg'], w=['em'])
        V(lambda h: h.tensor_tensor(out=em64, in0=em64, in1=pen64.unsqueeze(2).to_broadcast([128, 64, 8]), op=ALU.add), r=['em', 'pen'], w=['em'])
        V(lambda h: h.tensor_reduce(out=m1, in_=em, axis=AX.X, op=ALU.max), r=['em'], w=['m1'])
        V(lambda h: h.tensor_tensor(out=mask1, in0=em, in1=bc(m1, 32), op=ALU.is_equal), r=['em', 'm1'], w=['mask1'])
        V(lambda h: h.scalar_tensor_tensor(out=em2, in0=mask1, scalar=-BIGV, in1=em, op0=ALU.mult, op1=ALU.add), r=['mask1', 'em'], w=['em2'])
        V(lambda h: h.tensor_reduce(out=m2, in_=em2, axis=AX.X, op=ALU.max), r=['em2'], w=['m2'])
        V(lambda h: h.tensor_tensor(out=mask2, in0=em2, in1=bc(m2, 32), op=ALU.is_equal), r=['em2', 'm2'], w=['mask2'])
        V(lambda h: h.tensor_tensor(out=dd, in0=m2, in1=m1, op=ALU.subtract), r=['m1', 'm2'], w=['dd'])
        P.act(lambda h: h.activation(out=e2, in_=dd, func=AF.Exp), r=['dd'], w=['e2'])
        V(lambda h: h.tensor_scalar(out=den, in0=e2, scalar1=1.0, scalar2=None, op0=ALU.add), r=['e2'], w=['den'])
        V(lambda h: h.reciprocal(out=den, in_=den), r=['den'], w=['den'])
        V(lambda h: h.tensor_tensor(out=w1, in0=den, in1=gw, op=ALU.mult), r=['den', 'gw'], w=['w1'])
        V(lambda h: h.tensor_tensor(out=w2, in0=w1, in1=e2, op=ALU.mult), r=['w1', 'e2'], w=['w2'])
        V(lambda h: h.tensor_tensor(out=tmpe, in0=mask1, in1=mask2, op=ALU.add), r=['mask1', 'mask2'], w=['tmpe'])
        V(lambda h: h.tensor_copy(out=m12b, in_=tmpe), r=['tmpe'], w=['m12b'])
        bR, bC = nb(), nb()
        for j in range(NT):
            mm(ps[bR][:, j * 32:(j + 1) * 32], tri_strict[:], m12b[:, j, :], True, True, r=['tri_strict', 'm12b'], w=[f'ps{bR}'])
        for j in range(NT):
            mm(ps[bC][:, j * 32:(j + 1) * 32], ones_bf[:], m12b[:, j, :], True, True, r=['ones_bf', 'm12b'], w=[f'ps{bC}'])
        V(lambda h: h.tensor_copy(out=cs, in_=ps[bC][:, :].rearrange("p (j c) -> p j c", c=32)), r=[f'ps{bC}'], w=['cs'])
        V(lambda h: h.memset(base[:, 0, :], 0.0), w=['base'])
        for j in range(1, NT):
            V(lambda h, j=j: h.tensor_tensor(out=base[:, j, :], in0=base[:, j - 1, :], in1=cs[:, j - 1, :], op=ALU.add), r=['base', 'cs'], w=['base'])
        V(lambda h: h.tensor_tensor(out=rank, in0=ps[bR][:, :].rearrange("p (j c) -> p j c", c=32), in1=base, op=ALU.add), r=[f'ps{bR}', 'base'], w=['rank'])
        V(lambda h: h.tensor_tensor(out=slot, in0=rank, in1=ecap[:].unsqueeze(1).to_broadcast([128, 16, 32]), op=ALU.add), r=['rank', 'ecap'], w=['slot'])
        V(lambda h: h.tensor_single_scalar(out=okm, in_=rank, scalar=float(CAP), op=ALU.is_lt), r=['rank'], w=['okm'])
        for kk, (mk, mkey, wk_, wkey) in enumerate([(mask1, 'mask1', w1, 'w1'), (mask2, 'mask2', w2, 'w2')]):
            V(lambda h, mk=mk: h.tensor_tensor(out=tmpe, in0=mk, in1=slot, op=ALU.mult), r=[mkey, 'slot', 'm12b'], w=['tmpe'])
            V(lambda h: h.tensor_reduce(out=dstk, in_=tmpe, axis=AX.X, op=ALU.add), r=['tmpe'], w=['dstk'])
            V(lambda h, mk=mk: h.tensor_tensor(out=tmpe, in0=mk, in1=okm, op=ALU.mult), r=[mkey, 'okm', 'dstk'], w=['tmpe'])
            V(lambda h: h.tensor_reduce(out=okk, in_=tmpe, axis=AX.X, op=ALU.add), r=['tmpe'], w=['okk'])
            V(lambda h: h.tensor_scalar(out=dfin, in0=dstk, scalar1=pidx[:, 0:1], scalar2=None, op0=ALU.subtract), r=['dstk', 'pidx'], w=['dfin'])
            V(lambda h: h.tensor_tensor(out=dfin, in0=dfin, in1=okk, op=ALU.mult), r=['dfin', 'okk'], w=['dfin'])
            V(lambda h: h.tensor_scalar(out=dfin, in0=dfin, scalar1=pidx[:, 0:1], scalar2=None, op0=ALU.add), r=['dfin', 'pidx'], w=['dfin'])
            V(lambda h, kk=kk: h.tensor_copy(out=dests[:, :, kk], in_=dfin), r=['dfin'], w=[f'dests{kk}'])
            V(lambda h, kk=kk, wk_=wk_: h.tensor_tensor(out=wts[:, :, kk], in0=wk_, in1=okk, op=ALU.mult), r=[wkey, 'okk'], w=[f'wts{kk}'])
        xsc = [AR.bf16(2048) for _ in range(2)]
        for j in range(NT):
            xb = xsc[j % 2]
            xbk = f'xsc{j % 2}'
            sdma(xb, x1b_d[j * 128:(j + 1) * 128, :], w=[xbk])
            for kk in range(2):
                P.dma('pool', lambda h, kk=kk, j=j, xb=xb: h.indirect_dma_start(
                    out=xg_d, out_offset=bass.IndirectOffsetOnAxis(ap=dests[:, j, kk:kk + 1], axis=0), in_=xb, in_offset=None),
                    r=[xbk, f'dests{kk}'] + (zkeys if j == 0 else []), w=[uid('xg'), xbk])
        P.barrier()

    def phase_d(l):
        AR.reset()
        NSC = CAP // 128
        xg = [AR.bf16(NSC * 2048).rearrange("p (a b) -> p a b", b=2048) for _ in range(2)]
        xgT = AR.bf16(16 * CAP).rearrange("p (a b) -> p a b", b=CAP)
        hT = AR.bf16(4 * CAP).rearrange("p (a b) -> p a b", b=CAP)
        sa = [AR.f32(CAP) for _ in range(2)]
        ybs = [AR.bf16(2048) for _ in range(4)]
        ycnt = 0

        def wviews(e):
            big = bigA if e % 2 == 0 else bigB
            return (big[:, 0:4, :].rearrange("p a (b c) -> p (a b) c", c=512),
                    big[:, 4:8, :].rearrange("p a (b c) -> p (a b) c", c=512),
                    big[:, 8:12, :])

        def prefetch(e):
            w1v, w3v, w2v = wviews(e)
            wk = f'wset{e % 2}'
            pdma(w1v, din['moe_w1'][l, e].rearrange("(kc p) f -> p kc f", p=128), w=[wk + 'a'])
            pdma(w3v, din['moe_w3'][l, e].rearrange("(kc p) f -> p kc f", p=128), w=[wk + 'b'])
            pdma(w2v, din['moe_w2'][l, e].rearrange("(fc p) d -> p fc d", p=128), w=[wk + 'c'])
            sdma(xg[e % 2], xg_d[e * CAP:(e + 1) * CAP, :].rearrange("(sc p) d -> p sc d", p=128), w=[f'xg{e % 2}'])
        prefetch(0)
        for e in range(NE):
            if e + 1 < NE:
                prefetch(e + 1)
            wk = f'wset{e % 2}'
            w1v, w3v, w2v = wviews(e)
            xge = xg[e % 2]
            xgk = f'xg{e % 2}'
            ec = 0
            for sc in range(NSC):
                for g4 in range(4):
                    b = nb()
                    for q in range(4):
                        kc = g4 * 4 + q
                        mm(ps[b][:, q * 128:(q + 1) * 128], xge[:, sc, kc * 128:(kc + 1) * 128], ident[:], True, True, r=[xgk, 'ident'], w=[f'ps{b}'])
                    src = ps[b][:, :].rearrange("p (a b) -> p a b", b=128)
                    dstv = xgT[:, 4 * g4:4 * g4 + 4, sc * 128:(sc + 1) * 128]
                    if ec % 2 == 0:
                        P.act(lambda h, src=src, dstv=dstv: h.copy(out=dstv, in_=src), r=[f'ps{b}'], w=[uid('xgT')])
                    else:
                        P.dve(lambda h, src=src, dstv=dstv: h.tensor_copy(out=dstv, in_=src), r=[f'ps{b}'], w=[uid('xgT')])
                    ec += 1
            P.dve(lambda h: h.tensor_copy(out=sa[0][:, 0:1], in_=sa[0][:, 0:1]), r=[f'xgT#{k.cnt - i_}' for i_ in range(NSC * 4)], w=['xgT'])
            for fc in range(4):
                ba = nb()
                for kc in range(16):
                    mm(ps[ba][:, 0:CAP], w1v[:, kc, fc * 128:(fc + 1) * 128], xgT[:, kc, :], kc == 0, kc == 15, r=[wk + 'a', 'xgT'], w=[f'ps{ba}'])
                bb = nb()
                for kc in range(16):
                    mm(ps[bb][:, 0:CAP], w3v[:, kc, fc * 128:(fc + 1) * 128], xgT[:, kc, :], kc == 0, kc == 15, r=[wk + 'b', 'xgT'], w=[f'ps{bb}'])
                s_ = sa[fc % 2]
                P.act(lambda h, ba=ba, s_=s_: h.activation(out=s_, in_=ps[ba][:, 0:CAP], func=AF.Silu), r=[f'ps{ba}'], w=[f'sa{fc % 2}'])
                P.dve(lambda h, bb=bb, s_=s_, fc=fc: h.tensor_tensor(out=hT[:, fc, :], in0=s_, in1=ps[bb][:, 0:CAP], op=ALU.mult),
                      r=[f'sa{fc % 2}', f'ps{bb}'], w=[f'hT{fc}'])
            for sc in range(NSC):
                yb_ = ybs[ycnt % 4]
                ybk = f'ybs{ycnt % 4}'
                ycnt += 1
                for db in range(4):
                    b = nb()
                    for fc in range(4):
                        mm(ps[b][:, :], hT[:, fc, sc * 128:(sc + 1) * 128], w2v[:, fc, db * 512:(db + 1) * 512], fc == 0, fc == 3,
                           r=[f'hT{fc}', wk + 'c'], w=[f'ps{b}'])
                    if db % 2 == 0:
                        P.act(lambda h, b=b, db=db, yb_=yb_: h.copy(out=yb_[:, db * 512:(db + 1) * 512], in_=ps[b][:, :]), r=[f'ps{b}'], w=[f'{ybk}_{db}'])
                    else:
                        P.dve(lambda h, b=b, db=db, yb_=yb_: h.tensor_copy(out=yb_[:, db * 512:(db + 1) * 512], in_=ps[b][:, :]), r=[f'ps{b}'], w=[f'{ybk}_{db}'])
                sdma(yb_d[e * CAP + sc * 128:e * CAP + (sc + 1) * 128, :], yb_, r=[f'{ybk}_{d_}' for d_ in range(4)], w=[uid('ybd')] + [f'{ybk}_{d_}' for d_ in range(4)])
        P.barrier()

    def phase_e(l, xin_d, xout_d, make_xT):
        AR.reset()
        lnp = AR.f32(4096).rearrange("p (a b) -> p a b", b=2048)
        sdma(lnp, din[f'lnffn{l}'], w=['lnp'])
        r0 = [AR.f32(2048) for _ in range(2)]
        r1 = [AR.f32(2048) for _ in range(2)]
        g0 = [AR.bf16(2048) for _ in range(2)]
        g1 = [r1[i].bitcast(BF16)[:, 2048:4096] for i in range(2)]
        xs = [AR.f32(2048) for _ in range(2)]
        st = AR.f32(8)
        outs = []
        def loads(j):
            a0, a1, x_s = r0[j % 2], r1[j % 2], xs[j % 2]
            k0, k1, xk = f'r0{j % 2}', f'r1{j % 2}', f'xs{j % 2}'
            P.dma('pool', lambda h, j=j: h.indirect_dma_start(
                out=g0[j % 2], out_offset=None, in_=yb_d, in_offset=bass.IndirectOffsetOnAxis(ap=dests[:, j, 0:1], axis=0)), w=[f'g0{j % 2}'])
            P.dma('pool', lambda h, j=j: h.indirect_dma_start(
                out=g1[j % 2], out_offset=None, in_=yb_d, in_offset=bass.IndirectOffsetOnAxis(ap=dests[:, j, 1:2], axis=0)), w=[k1])
            sdma(x_s, xin_d[j * 128:(j + 1) * 128, :], w=[xk])
        def tile_gen(j):
            a0, a1, x_s = r0[j % 2], r1[j % 2], xs[j % 2]
            k0, k1, xk = f'r0{j % 2}', f'r1{j % 2}', f'xs{j % 2}'
            sx = f'_{j % 2}'
            stj = st2[j % 2]
            junk_j = a1.bitcast(BF16)[:, 0:2048]
            x2b_j = x_s.bitcast(BF16)[:, 0:2048]
            P.dve(lambda h: h.tensor_scalar(out=a0, in0=g0[j % 2], scalar1=wts[:, j, 0:1], scalar2=None, op0=ALU.mult), r=[f'g0{j % 2}'], w=[k0])
            yield
            P.dve(lambda h: h.scalar_tensor_tensor(out=a0, in0=g1[j % 2], scalar=wts[:, j, 1:2], in1=a0, op0=ALU.mult, op1=ALU.add), r=[k0, k1], w=[k0])
            yield
            P.dve(lambda h: h.scalar_tensor_tensor(out=a0, in0=x_s, scalar=ALPHA, in1=a0, op0=ALU.mult, op1=ALU.add), r=[k0, xk], w=[k0])
            yield
            yield from layer_norm_gen(a0, k0, a0, k0, lnp, 'lnp', junk_j, stj, jk=k1, sx=sx)
            ok_ = uid('xout')
            sdma(xout_d[j * 128:(j + 1) * 128, :], a0, r=[k0], w=[ok_, k0])
            outs.append(ok_)
            yield
            if make_xT:
                P.act(lambda h: h.copy(out=x2b_j, in_=a0), r=[k0], w=[xk])
                yield
                for g4 in range(4):
                    b = nb()
                    for q in range(4):
                        kc = g4 * 4 + q
                        mm(ps[b][:, q * 128:(q + 1) * 128], x2b_j[:, kc * 128:(kc + 1) * 128], ident[:], True, True, r=[xk, 'ident'], w=[f'ps{b}'])
                    if g4 % 2 == 0:
                        P.act(lambda h, b=b, g4=g4: h.copy(out=bigA[:, 4 * g4:4 * g4 + 4, j * 128:(j + 1) * 128],
                                                           in_=ps[b][:, :].rearrange("p (a b) -> p a b", b=128)), r=[f'ps{b}'], w=['xT'])
                    else:
                        P.dve(lambda h, b=b, g4=g4: h.tensor_copy(out=bigA[:, 4 * g4:4 * g4 + 4, j * 128:(j + 1) * 128],
                                                                  in_=ps[b][:, :].rearrange("p (a b) -> p a b", b=128)), r=[f'ps{b}'], w=['xT'])
                    yield
        st2 = [st, AR.f32(8)]
        if make_xT:
            loads(0)
            loads(1)
            for j in range(0, NT, 2):
                zip_run([tile_gen(j), tile_gen(j + 1)])
                if j + 2 < NT:
                    loads(j + 2)
                    loads(j + 3)
        else:
            loads(0)
            for j in range(NT):
                if j + 1 < NT:
                    loads(j + 1)
                zip_run([tile_gen(j)])
        P.barrier()
        return outs

    def phase_a1():
        AR.reset()
        xT, yT = bigA, bigB
        wblk = [AR.bf16(2048).rearrange("p (a b) -> p a b", b=128) for _ in range(2)]
        wcnt = [0]

        def next_w(blk):
            i = wcnt[0] % 2
            wcnt[0] += 1
            load_wblk(wblk[i], din['w_in1'][blk], f'wblk{i}')
            return wblk[i], f'wblk{i}'
        mark = AR.off
        gu = bigB[:, 8:16, :].rearrange("p a (b c) -> p (a b) c", c=1024)
        gv = AR.bf16(16 * 1024).rearrange("p (a b) -> p a b", b=1024)
        glnp = AR.f32(2048).rearrange("p (a b) -> p a b", b=1024)
        wsT = AR.f32(1024).rearrange("p (a b) -> p a b", b=128)
        wsTm = AR.bf16(1024).rearrange("p (a b) -> p a b", b=128)
        bsT = AR.f32(8)
        s1 = AR.f32(128).rearrange("p (a b) -> p a b", b=8)
        s2 = AR.f32(128).rearrange("p (a b) -> p a b", b=8)
        mean, msq, var, rstd = AR.f32(16), AR.f32(16), AR.f32(16), AR.f32(16)
        vn = AR.f32(1024)
        vnb = AR.bf16(1024)
        tmp = AR.f32(1024)
        ycb = AR.bf16(1024)
        tmpf = [AR.f32(128) for _ in range(2)]
        junkb = AR.bf16(128)
        sdma(glnp, din['glnp'], w=['glnp'])
        sdma(wsT, din['wsT'], w=['wsT'])
        sdma(bsT, din['bsT'], w=['bsT'])
        P.dve(lambda h: h.tensor_tensor(out=wsTm, in0=wsT, in1=tril_st[:].unsqueeze(1).to_broadcast([128, 8, 128]), op=ALU.mult),
              r=['wsT', 'tril_st'], w=['wsTm'])
        for ub in range(8):
            wb, wk = next_w(ub)
            proj_tm(xT, 'xT', wb, wk, 128, lambda g, b, ub=ub: P.act(
                lambda h: h.activation(out=gu[:, 4 * g:4 * g + 4, ub * 128:(ub + 1) * 128],
                                       in_=ps[b][:, :].rearrange("p (j c) -> p j c", c=128), func=AF.Gelu), r=[f'ps{b}'], w=['gu']))
        tcnt = [0]
        for vb in range(8):
            wb, wk = next_w(8 + vb)

            def ev_v(g, b, vb=vb):
                for jj in range(4):
                    j = 4 * g + jj
                    tf = tmpf[tcnt[0] % 2]
                    tk = f'tmpf{tcnt[0] % 2}'
                    tcnt[0] += 1
                    P.act(lambda h, jj=jj, j=j, tf=tf: h.activation(out=tf, in_=ps[b][:, jj * 128:(jj + 1) * 128], func=AF.Gelu,
                                                                    accum_out=s1[:, j, vb:vb + 1]), r=[f'ps{b}'], w=[tk, uid('s1')])
                    P.act(lambda h, j=j, tf=tf: h.activation(out=junkb, in_=tf, func=AF.Square, accum_out=s2[:, j, vb:vb + 1]),
                          r=[tk], w=['junkb', uid('s2')])
                    P.dve(lambda h, j=j, tf=tf: h.tensor_copy(out=gv[:, j, vb * 128:(vb + 1) * 128], in_=tf), r=[tk], w=['gv'])
            proj_tm(xT, 'xT', wb, wk, 128, ev_v)
        P.barrier()
        P.dve(lambda h: h.tensor_reduce(out=mean, in_=s1, axis=AX.X, op=ALU.add), w=['mean'])
        P.dve(lambda h: h.tensor_reduce(out=var, in_=s2, axis=AX.X, op=ALU.add), w=['var'])
        P.dve(lambda h: h.tensor_scalar(out=mean, in0=mean, scalar1=1.0 / 1024, scalar2=None, op0=ALU.mult), r=['mean'], w=['mean'])
        P.dve(lambda h: h.tensor_tensor(out=msq, in0=mean, in1=mean, op=ALU.mult), r=['mean'], w=['msq'])
        P.dve(lambda h: h.scalar_tensor_tensor(out=var, in0=var, scalar=1.0 / 1024, in1=msq, op0=ALU.mult, op1=ALU.subtract), r=['var', 'msq'], w=['var'])
        P.act(lambda h: h.activation(out=rstd, in_=var, func=AF.Sqrt, bias=LN_EPS, scale=1.0), r=['var'], w=['rstd'])
        P.dve(lambda h: h.reciprocal(out=rstd, in_=rstd), r=['rstd'], w=['rstd'])
        for j in range(NT):
            P.dve(lambda h, j=j: h.tensor_scalar(out=vn, in0=gv[:, j, :], scalar1=mean[:, j:j + 1], scalar2=rstd[:, j:j + 1],
                                                op0=ALU.subtract, op1=ALU.mult), r=['gv', 'mean', 'rstd'], w=['vn'])
            P.pool(lambda h: h.tensor_tensor(out=vn, in0=vn, in1=glnp[:, 0, :], op=ALU.mult), r=['vn', 'glnp'], w=['vn'])
            P.pool(lambda h: h.tensor_tensor(out=vnb, in0=vn, in1=glnp[:, 1, :], op=ALU.add), r=['vn', 'glnp'], w=['vnb'])
            for half in range(2):
                b = nb()
                for q in range(4):
                    g = half * 4 + q
                    mm(ps[b][:, q * 128:(q + 1) * 128], wsTm[:, g, :], vnb[:, g * 128:(g + 1) * 128], True, True, r=['wsTm', 'vnb'], w=[f'ps{b}'])
                P.dve(lambda h, b=b, half=half: h.tensor_tensor(
                    out=tmp[:, half * 512:(half + 1) * 512].rearrange("p (g c) -> p g c", c=128),
                    in0=ps[b][:, :].rearrange("p (g c) -> p g c", c=128),
                    in1=bsT[:, half * 4:half * 4 + 4].unsqueeze(2).to_broadcast([128, 4, 128]), op=ALU.add), r=[f'ps{b}', 'bsT'], w=[f'tmp{half}'])
                P.pool(lambda h, half=half, j=j: h.tensor_tensor(out=ycb[:, half * 512:(half + 1) * 512], in0=tmp[:, half * 512:(half + 1) * 512],
                                                                in1=gu[:, j, half * 512:(half + 1) * 512], op=ALU.mult), r=[f'tmp{half}', 'gu'], w=[f'ycb{half}'])
            for half in range(2):
                b = nb()
                for q in range(4):
                    g = half * 4 + q
                    mm(ps[b][:, q * 128:(q + 1) * 128], ycb[:, g * 128:(g + 1) * 128], ident[:], True, True, r=[f'ycb{half}', 'ident'], w=[f'ps{b}'])
                if half == 0:
                    P.act(lambda h, b=b, half=half, j=j: h.copy(out=yT[:, 4 * half:4 * half + 4, j * 128:(j + 1) * 128],
                                                                in_=ps[b][:, :].rearrange("p (a b) -> p a b", b=128)), r=[f'ps{b}'], w=['yTc'])
                else:
                    P.dve(lambda h, b=b, half=half, j=j: h.tensor_copy(out=yT[:, 4 * half:4 * half + 4, j * 128:(j + 1) * 128],
                                                                       in_=ps[b][:, :].rearrange("p (a b) -> p a b", b=128)), r=[f'ps{b}'], w=['yTc'])
        P.barrier()
        P.tag = 'phase_a1_conv'
        AR.off = mark
        cw = AR.f32(8 * 31).rearrange("p (a b) -> p a b", b=31)
        cpar = AR.f32(24).rearrange("p (a b) -> p a b", b=8)
        sgf = AR.f32(2048)
        hp = [AR.bf16(30 + 2048) for _ in range(2)]
        Dg = [AR.bf16(31 * 128).rearrange("p (a b) -> p a b", b=128) for _ in range(2)]
        meanT, msqT, varT, rstdT = AR.f32(512), AR.f32(512), AR.f32(512), AR.f32(512)
        zb = [AR.f32(512) for _ in range(2)]
        sqb = [AR.bf16(512) for _ in range(2)]
        sdma(cw, din['cw'], w=['cw'])
        sdma(cpar, din['cpar'], w=['cpar'])
        for i in range(2):
            P.pool(lambda h, i=i: h.memset(hp[i][:, 0:30], 0.0), w=[f'hp{i}'])
        for cc in range(8):
            hpc, hk = hp[cc % 2], f'hp{cc % 2}'
            dgc, dk = Dg[cc % 2], f'Dg{cc % 2}'
            wb, wk = next_w(24 + cc)
            proj_fm(xT, 'xT', wb, wk, lambda tt, b: P.act(
                lambda h: h.activation(out=sgf[:, tt * 512:(tt + 1) * 512], in_=ps[b][:, :], func=AF.Sigmoid), r=[f'ps{b}'], w=['sgf']))
            wb, wk = next_w(16 + cc)
            proj_fm(xT, 'xT', wb, wk, lambda tt, b, hpc=hpc, hk=hk: P.dve(
                lambda h: h.tensor_tensor(out=hpc[:, 30 + tt * 512:30 + (tt + 1) * 512], in0=ps[b][:, :], in1=sgf[:, tt * 512:(tt + 1) * 512], op=ALU.mult),
                r=[f'ps{b}', 'sgf'], w=[hk]))
            P.pool(lambda h, dgc=dgc, cc=cc: h.tensor_tensor(out=dgc, in0=ident[:].unsqueeze(1).to_broadcast([128, 31, 128]),
                                                            in1=cw[:, cc, :].unsqueeze(2).to_broadcast([128, 31, 128]), op=ALU.mult),
                   r=['ident', 'cw'], w=[dk])
            for tt in range(4):
                b = nb()
                for jt in range(31):
                    mm(ps[b][:, :], dgc[:, jt, :], hpc[:, tt * 512 + jt:tt * 512 + jt + 512], jt == 0, jt == 30, r=[dk, hk], w=[f'ps{b}'])
                P.act(lambda h, b=b, cc=cc, tt=tt: h.activation(out=yT[:, 8 + cc, tt * 512:(tt + 1) * 512], in_=ps[b][:, :], func=AF.Identity,
                                                                bias=cpar[:, 0, cc:cc + 1], scale=1.0), r=[f'ps{b}', 'cpar'], w=[f'yd{cc}'])
        scnt = 0
        for tt in range(4):
            ts = slice(tt * 512, (tt + 1) * 512)
            b1 = nb()
            for cc in range(8):
                mm(ps[b1][:, :], ones_bf[:], yT[:, 8 + cc, ts], cc == 0, cc == 7, r=['ones_bf', f'yd{cc}'], w=[f'ps{b1}'])
            b2 = nb()
            for cc in range(8):
                sq_, sk_ = sqb[scnt % 2], f'sqb{scnt % 2}'
                scnt += 1
                P.pool(lambda h, sq_=sq_, cc=cc, ts=ts: h.tensor_tensor(out=sq_, in0=yT[:, 8 + cc, ts], in1=yT[:, 8 + cc, ts], op=ALU.mult),
                       r=[f'yd{cc}'], w=[sk_])
                mm(ps[b2][:, :], ones_bf[:], sq_, cc == 0, cc == 7, r=['ones_bf', sk_], w=[f'ps{b2}'])
            P.act(lambda h, b1=b1: h.mul(out=meanT, in_=ps[b1][:, :], mul=1.0 / 1024), r=[f'ps{b1}'], w=['meanT'])
            P.dve(lambda h: h.tensor_tensor(out=msqT, in0=meanT, in1=meanT, op=ALU.mult), r=['meanT'], w=['msqT'])
            P.dve(lambda h, b2=b2: h.scalar_tensor_tensor(out=varT, in0=ps[b2][:, :], scalar=1.0 / 1024, in1=msqT, op0=ALU.mult, op1=ALU.subtract),
                  r=[f'ps{b2}', 'msqT'], w=['varT'])
            P.act(lambda h: h.activation(out=rstdT, in_=varT, func=AF.Sqrt, bias=LN_EPS, scale=1.0), r=['varT'], w=['rstdT'])
            P.dve(lambda h: h.reciprocal(out=rstdT, in_=rstdT), r=['rstdT'], w=['rstdT'])
            for cc in range(8):
                z_, zk = zb[cc % 2], f'zb{cc % 2}'
                P.dve(lambda h, z_=z_, cc=cc, ts=ts: h.tensor_tensor(out=z_, in0=yT[:, 8 + cc, ts], in1=meanT, op=ALU.subtract),
                      r=[f'yd{cc}', 'meanT'], w=[zk])
                P.pool(lambda h, z_=z_: h.tensor_tensor(out=z_, in0=z_, in1=rstdT, op=ALU.mult), r=[zk, 'rstdT'], w=[zk])
                P.act(lambda h, z_=z_, cc=cc, ts=ts: h.activation(out=yT[:, 8 + cc, ts], in_=z_, func=AF.Silu,
                                                                  bias=cpar[:, 2, cc:cc + 1], scale=cpar[:, 1, cc:cc + 1]),
                      r=[zk, 'cpar'], w=[f'yd{cc}'])
        P.dve(lambda h: h.tensor_copy(out=meanT[:, 0:1], in_=meanT[:, 0:1]), r=['yTc'] + [f'yd{cc}' for cc in range(8)], w=['yT'])
        P.barrier()

    def dump_dram(src_d):
        AR.reset()
        st = [AR.f32(2048) for _ in range(2)]
        keys = []
        for j in range(NT):
            sdma(st[j % 2], src_d[j * 128:(j + 1) * 128, :], w=[f'dst{j % 2}'])
            kk = uid('dbg')
            sdma(dbg_d[j * 128:(j + 1) * 128, :], st[j % 2], r=[f'dst{j % 2}'], w=[kk])
            keys.append(kk)
        P.finish(keys)
        P.emit()
        return nc

    try:
        P.tag = 'phase_a0'
        phase_a0()
    except _Cut:
        pass
    if debug and stage == 0:
        AR.reset()
        st = AR.f32(2048)
        for fc in range(16):
            P.dve(lambda h, fc=fc: h.tensor_copy(out=st, in_=bigB[:, fc, :]), r=['yT'], w=['st'])
            sdma(dbg_d[fc * 128:(fc + 1) * 128, :], st, r=['st'], w=[f'dbg{fc}'])
        P.finish([f'dbg{fc}' for fc in range(16)])
        P.emit()
        return nc
    P.tag = 'phase_c_0'
    phase_c(0, din['x_tm'], x1_d)
    if debug and stage == 1:
        return dump_dram(x1_d)
    P.tag = 'phase_d_0'
    phase_d(0)
    P.tag = 'phase_e_0'
    phase_e(0, x1_d, x2_d, True)
    if debug and stage == 2:
        return dump_dram(x2_d)
    P.tag = 'phase_a1'
    phase_a1()
    if debug and stage == 3:
        AR.reset()
        st = AR.f32(2048)
        for fc in range(16):
            P.dve(lambda h, fc=fc: h.tensor_copy(out=st, in_=bigB[:, fc, :]), r=['yT'], w=['st'])
            sdma(dbg_d[fc * 128:(fc + 1) * 128, :], st, r=['st'], w=[f'dbg{fc}'])
        P.finish([f'dbg{fc}' for fc in range(16)])
        P.emit()
        return nc
    P.tag = 'phase_c_1'
    phase_c(1, x2_d, x3_d)
    P.tag = 'phase_d_1'
    phase_d(1)
    P.tag = 'phase_e_1'
    outs = phase_e(1, x3_d, out_d, False)
    P.finish(outs)
    P.emit()
    k.nops = len(P.ops)
    return nc


def kernel(**inputs):
    inp = {k: np.asarray(v) for k, v in inputs.items()}
    sh = prep_shared(inp)
    x = inp['x']
    in_maps = []
    for b in range(8):
        m = dict(sh)
        m['x_tm'] = np.ascontiguousarray(x[b])
        m['xT'] = np.ascontiguousarray(x[b].T)
        in_maps.append(m)
    nc = build()
    res = run_bass_kernel_spmd(nc, in_maps, core_ids=list(range(8)))
    return np.stack([np.asarray(r['out']) for r in res.results], axis=0).astype(np.float32)
```

```python
import numpy as np
from contextlib import ExitStack
import concourse.bass as bass
import concourse.mybir as mybir

F32 = mybir.dt.float32
BF16 = mybir.dt.bfloat16
I32 = mybir.dt.int32
U32 = mybir.dt.uint32
AF = mybir.ActivationFunctionType
ALU = mybir.AluOpType
AX = mybir.AxisListType

SEM_ROT = 20000
DMA_K = 6


class Prog:
    ENGS = ['pe', 'act', 'dve', 'pool', 'sp']

    def __init__(self, nc):
        self.nc = nc
        self.ops = []
        self.es = ExitStack()
        self.last_w = {}
        self.readers = {}
        self.ndma = {e: 0 for e in self.ENGS}
        self.dma_ops = {e: [] for e in self.ENGS}

    def sb(self, name, shape, dt):
        return self.es.enter_context(self.nc.sbuf_tensor("sb_" + name, list(shape), dt))

    def ps(self, name, shape, dt=F32):
        return self.es.enter_context(self.nc.psum_tensor("pp_" + name, list(shape), dt))

    def add(self, eng, fn, r=(), w=(), dma=False):
        i = len(self.ops)
        deps = set()
        psk = [k for k in list(r) + list(w) if isinstance(k, str) and k.startswith('ps')]
        r = [k for k in r if k not in psk] + ['phase']
        w = [k for k in w if k not in psk]
        for k in psk:
            lw = self.last_w.get(k)
            if lw is not None and self.ops[lw]['eng'] != eng:
                deps.add(lw)
        for k in r:
            lw = self.last_w.get(k)
            if lw is not None:
                deps.add(lw)
        for k in w:
            lw = self.last_w.get(k)
            if lw is not None:
                deps.add(lw)
            for rd in self.readers.get(k, ()):
                deps.add(rd)
        deps.discard(i)
        op = dict(i=i, eng=eng, fn=fn, deps=deps, dma=dma, sig=False, tag=getattr(self, 'tag', 'x'))
        if dma:
            j = self.ndma[eng]
            self.ndma[eng] += 1
            op['dj'] = j
            self.dma_ops[eng].append(i)
            if j >= DMA_K:
                deps.add(self.dma_ops[eng][j - DMA_K])
        self.ops.append(op)
        for k in psk:
            self.last_w[k] = i
        for k in r:
            self.readers.setdefault(k, []).append(i)
        for k in w:
            self.last_w[k] = i
            self.readers[k] = []
        return i

    def pe(self, fn, r=(), w=()): return self.add('pe', fn, r, w)
    def act(self, fn, r=(), w=()): return self.add('act', fn, r, w)
    def dve(self, fn, r=(), w=()): return self.add('dve', fn, r, w)
    def pool(self, fn, r=(), w=()): return self.add('pool', fn, r, w)
    def dma(self, eng, fn, r=(), w=()): return self.add(eng, fn, r, w, dma=True)

    def emit(self):
        nc = self.nc
        ops = self.ops
        for op in ops:
            if op['eng'] == 'pe' and not op['dma']:
                op['deps'] = {d for d in op['deps'] if not (ops[d]['eng'] == 'pe' and not ops[d]['dma'])}
        if getattr(self, 'prune_same_engine', False):
            for op in ops:
                if not op['dma']:
                    op['deps'] = {d for d in op['deps'] if not (ops[d]['eng'] == op['eng'] and not ops[d]['dma'])}
        cnt = {e: 0 for e in self.ENGS}
        needed = set()
        for op in ops:
            for d in op['deps']:
                needed.add(d)
        for op in ops:
            if op['dma']:
                continue
            if op['i'] in needed:
                cnt[op['eng']] += 1
                op['sidx'] = cnt[op['eng']]
        nsem_eng = {e: (cnt[e] + SEM_ROT - 1) // SEM_ROT for e in self.ENGS}
        sems = {}
        for e in self.ENGS:
            sems[e] = [self.es.enter_context(nc.semaphore(f"s_{e}_{k}")) for k in range(max(1, nsem_eng[e]))]
        dsems = {}
        for e in self.ENGS:
            if self.ndma[e]:
                dsems[e] = [self.es.enter_context(nc.semaphore(f"d_{e}_{k}")) for k in range(DMA_K)]
        know = {e: {x: 0 for x in self.ENGS} for e in self.ENGS}
        know_dma = {e: set() for e in self.ENGS}
        snap_sig = {}
        snap_dma = {}
        nwaits = 0

        def inherit(e, sn):
            ks, kd = sn
            for x in self.ENGS:
                if ks[x] > know[e][x]:
                    know[e][x] = ks[x]
            know_dma[e] |= kd

        for op in ops:
            e = op['eng']
            waits = []
            need_eng = {}
            need_dma = []
            for d in sorted(op['deps']):
                dop = ops[d]
                if dop['dma']:
                    need_dma.append((dop['eng'], dop['dj']))
                else:
                    need_eng[dop['eng']] = max(need_eng.get(dop['eng'], 0), dop['sidx'])
            for x, s in sorted(need_eng.items(), key=lambda t: -t[1]):
                if know[e][x] >= s:
                    continue
                waits.append(('eng', x, s))
                know[e][x] = s
                inherit(e, snap_sig[(x, s)])
            for key in need_dma:
                if key in know_dma[e]:
                    continue
                waits.append(('dma', key[0], key[1]))
                know_dma[e].add(key)
                inherit(e, snap_dma[key])
            op['waits'] = waits
            nwaits += len(waits)
            if op['dma']:
                snap_dma[(e, op['dj'])] = (dict(know[e]), set(know_dma[e]))
            elif 'sidx' in op:
                snap_sig[(e, op['sidx'])] = (dict(know[e]), set(know_dma[e]))
        self.nwaits = nwaits

        def sem_of(x, s):
            return sems[x][(s - 1) // SEM_ROT], (s - 1) % SEM_ROT + 1

        def dsem_of(x, j):
            return dsems[x][j % DMA_K], 16 * (j // DMA_K + 1)

        streams = {e: [op for op in ops if op['eng'] == e] for e in self.ENGS}

        scoped = getattr(self, 'use_scopes', False)

        def run_stream(e, h):
            import itertools
            for tag, grp in itertools.groupby(streams[e], key=lambda o: o.get('tag')):
                if scoped:
                    with nc.named_scope(str(tag)):
                        for op in grp:
                            self._emit_one(e, h, op, sem_of, dsem_of)
                else:
                    for op in grp:
                        self._emit_one(e, h, op, sem_of, dsem_of)

        def _unused(e, h):
            for op in streams[e]:
                for wt in op['waits']:
                    if wt[0] == 'eng':
                        s, v = sem_of(wt[1], wt[2])
                    else:
                        s, v = dsem_of(wt[1], wt[2])
                    h.wait_ge(s, v)
                ins = op['fn'](h)
                if op['dma']:
                    s, v = dsem_of(e, op['dj'])
                    ins.then_inc(s, 16)
                elif 'sidx' in op:
                    s, v = sem_of(e, op['sidx'])
                    ins.then_inc(s, 1)

        with nc.Block() as block:
            if streams['sp']:
                @block.sync
                def _(h):
                    run_stream('sp', h)
            if streams['pe']:
                @block.tensor
                def _(h):
                    run_stream('pe', h)
            if streams['act']:
                @block.scalar
                def _(h):
                    run_stream('act', h)
            if streams['dve']:
                @block.vector
                def _(h):
                    run_stream('dve', h)
            if streams['pool']:
                @block.gpsimd
                def _(h):
                    run_stream('pool', h)

    def _emit_one(self, e, h, op, sem_of, dsem_of):
        for wt in op['waits']:
            if wt[0] == 'eng':
                s, v = sem_of(wt[1], wt[2])
            else:
                s, v = dsem_of(wt[1], wt[2])
            h.wait_ge(s, v)
        ins = op['fn'](h)
        if op['dma']:
            s, v = dsem_of(e, op['dj'])
            ins.then_inc(s, 16)
        elif 'sidx' in op:
            s, v = sem_of(e, op['sidx'])
            ins.then_inc(s, 1)

    def barrier(self):
        self.add('sp', lambda h: h.nop(), r=(), w=['phase'])

    def finish(self, final_deps_keys):
        deps = [self.last_w[k] for k in final_deps_keys]
        i = len(self.ops)
        op = dict(i=i, eng='sp', fn=lambda h: h.nop(), deps=set(deps), dma=False, sig=False)
        self.ops.append(op)


class Arena:
    def __init__(self, P, nbytes):
        self.t = P.sb('arena', [128, nbytes // 4], F32)
        self.n = nbytes // 4
        self.off = 0

    def reset(self):
        self.off = 0

    def f32(self, n):
        assert self.off + n <= self.n, ("arena overflow", self.off, n, self.n)
        ap = self.t[:, self.off:self.off + n]
        self.off += n
        return ap

    def bf16(self, n):
        w = (n + 1) // 2
        return self.f32(w).bitcast(BF16)[:, 0:n]

    def i32(self, n):
        return self.f32(n).bitcast(I32)

import math
from concourse.bass_utils import run_bass_kernel_spmd

S = 2048
D = 2048
NT = 16
NE = 32
CAP = 384
NSLOT = NE * CAP
ALPHA = float((2 * 2) ** 0.25)
LN_EPS = 1e-5
RMS_EPS = 1e-6
NEG = -30000.0


def _t5_bucket(dist):
    max_exact = 16
    d = np.maximum(dist, 1).astype(np.float32)
    large = max_exact + (np.log(d / np.float32(max_exact)) / np.float32(math.log(128 / max_exact))
                         * np.float32(32 - max_exact)).astype(np.int32)
    large = np.minimum(large, 31)
    return np.where(dist < max_exact, dist, large)


def _bc(v, n=128):
    return np.ascontiguousarray(np.broadcast_to(np.asarray(v, np.float32)[None], (n,) + tuple(np.shape(v))))


def prep_shared(inp):
    f32 = np.float32
    sh = {}
    w = np.asarray(inp['w_in_ab'][0])
    cols = []
    for c in range(8):
        cols += list(range(c * 64, c * 64 + 64)) + list(range((8 + c) * 64, (8 + c) * 64 + 64))
    cols += list(range(1024, 1152)) + list(range(1152, 1280))
    for h in range(8):
        for part in range(4):
            b0 = 1280 + part * 1024 + h * 128
            cols += list(range(b0, b0 + 128))
    wp = w[:, cols]
    sh['w_in0'] = np.ascontiguousarray(wp.reshape(16, 128, 42, 128).transpose(2, 1, 0, 3))
    rows = []
    for c in range(8):
        rows += list(range(c * 64, c * 64 + 64)) + list(range((8 + c) * 64, (8 + c) * 64 + 64))
    rows += list(range(1024, 2048))
    wo = np.asarray(inp['w_out_ab'][0])[rows]
    sh['w_out0'] = np.ascontiguousarray(wo.reshape(16, 128, 2048).transpose(1, 0, 2))
    t_loc = np.arange(128, dtype=np.int32)[:, None]
    s_loc = np.arange(256, dtype=np.int32)[None, :]
    dist = t_loc + 128 - s_loc
    bucket = _t5_bucket(np.maximum(dist, 0))
    rb = np.asarray(inp['rel_bias'])
    bg = rb[bucket]
    hb = np.zeros((128, 8, 2, 256), f32)
    for c in range(8):
        hb[:, c, 0, :] = bg[:, :, c]
        hb[:, c, 1, :] = bg[:, :, 8 + c]
    sh['bias_g'] = hb
    valid = (dist >= 0) & (dist < 128)
    sh['maskneg'] = np.where(valid, 0.0, NEG).astype(f32)
    sk = np.asarray(inp['attn_sinks'][0])
    sk2 = np.stack([sk[0:8], sk[8:16]], axis=1)
    sh['sinks'] = _bc(sk2)
    lbl = np.asarray(inp['hgrn_lb_logits'])
    sh['lbl'] = np.ascontiguousarray(lbl.reshape(3, 8, 128).transpose(2, 1, 0))
    sh['normg'] = _bc(np.asarray(inp['hgrn_norm_g'][0]))
    sh['ident'] = np.eye(128, dtype=f32)
    ii = np.arange(128)
    sh['mask_bd'] = ((ii[:, None] <= ii[None, :]) & ((ii[:, None] // 64) == (ii[None, :] // 64))).astype(f32)
    sh['tri_strict'] = (ii[:, None] < ii[None, :]).astype(f32)
    sh['tril_st'] = (ii[:, None] <= ii[None, :]).astype(f32)
    sh['ecap'] = _bc(np.arange(NE, dtype=f32) * CAP)
    sh['pidx'] = (np.arange(128, dtype=f32) + NSLOT).reshape(128, 1)
    for l in range(2):
        sh[f'lnmix{l}'] = np.stack([_bc(inp['ln_mix_g'][l]), _bc(inp['ln_mix_b'][l])], axis=1)
        sh[f'lnffn{l}'] = np.stack([_bc(inp['ln_ffn_g'][l]), _bc(inp['ln_ffn_b'][l])], axis=1)
        wr = np.concatenate([np.asarray(inp['moe_w_group'][l]), np.asarray(inp['moe_w_router'][l])], axis=1)
        sh[f'wr{l}'] = np.ascontiguousarray(wr.reshape(16, 128, 36).transpose(1, 0, 2))
        sh[f'br{l}'] = _bc(np.concatenate([np.asarray(inp['moe_b_group'][l]), np.asarray(inp['moe_b_router'][l])]))
    w1 = np.asarray(inp['w_in_cd'][0])
    sh['w_in1'] = np.ascontiguousarray(w1.reshape(16, 128, 32, 128).transpose(2, 1, 0, 3))
    wo1 = np.asarray(inp['w_out_cd'][0])
    sh['w_out1'] = np.ascontiguousarray(wo1.reshape(16, 128, 2048).transpose(1, 0, 2))
    sh['wsT'] = np.ascontiguousarray(np.asarray(inp['gmlp_w_s'][0]).transpose(2, 0, 1))
    sh['bsT'] = np.ascontiguousarray(np.asarray(inp['gmlp_b_s'][0]).T)
    sh['glnp'] = np.stack([_bc(inp['gmlp_ln_g'][0]), _bc(inp['gmlp_ln_b'][0])], axis=1)
    sh['cw'] = np.ascontiguousarray(np.asarray(inp['conv_w'][0]).reshape(31, 8, 128).transpose(2, 1, 0))
    cp = np.stack([np.asarray(inp['conv_b'][0]), np.asarray(inp['conv_ln_g'][0]), np.asarray(inp['conv_ln_b'][0])], axis=0)
    sh['cpar'] = np.ascontiguousarray(cp.reshape(3, 8, 128).transpose(2, 0, 1))
    sh['moe_w1'] = np.asarray(inp['moe_w1'])
    sh['moe_w3'] = np.asarray(inp['moe_w3'])
    sh['moe_w2'] = np.asarray(inp['moe_w2'])
    return sh


IN_SPECS = dict(
    x_tm=([S, D], 'f'), xT=([D, S], 'f'),
    w_in0=([42, 128, 16, 128], 'f'), w_out0=([128, 16, 2048], 'f'),
    bias_g=([128, 8, 2, 256], 'f'), maskneg=([128, 256], 'f'), sinks=([128, 8, 2], 'f'),
    lbl=([128, 8, 3], 'f'), normg=([128, 1024], 'f'),
    ident=([128, 128], 'f'), mask_bd=([128, 128], 'f'), tri_strict=([128, 128], 'f'), tril_st=([128, 128], 'f'),
    ecap=([128, 32], 'f'), pidx=([128, 1], 'f'),
    lnmix0=([128, 2, 2048], 'f'), lnffn0=([128, 2, 2048], 'f'), lnmix1=([128, 2, 2048], 'f'), lnffn1=([128, 2, 2048], 'f'),
    wr0=([128, 16, 36], 'f'), wr1=([128, 16, 36], 'f'), br0=([128, 36], 'f'), br1=([128, 36], 'f'),
    w_in1=([32, 128, 16, 128], 'f'), w_out1=([128, 16, 2048], 'f'),
    wsT=([128, 8, 128], 'f'), bsT=([128, 8], 'f'), glnp=([128, 2, 1024], 'f'),
    cw=([128, 8, 31], 'f'), cpar=([128, 3, 8], 'f'),
    moe_w1=([2, 32, 2048, 512], 'f'), moe_w3=([2, 32, 2048, 512], 'f'), moe_w2=([2, 32, 512, 2048], 'f'),
)


class K:
    pass


class _Cut(Exception):
    pass


def build(stage=99, debug=None, cut=None, scopes=False, prune_same=False):
    nc = bass.Bass("TRN2", target_bir_lowering=False)
    P = Prog(nc)
    P.use_scopes = scopes
    P.prune_same_engine = prune_same
    P.tag = 'init'
    din = {}
    for name, (shape, _) in IN_SPECS.items():
        din[name] = nc.dram_tensor(name, list(shape), F32, kind="ExternalInput").ap()
    out_d = nc.dram_tensor("out", [S, D], F32, kind="ExternalOutput").ap()
    dbg_d = None
    if debug:
        dbg_d = nc.dram_tensor("dbg", list(debug), F32, kind="ExternalOutput").ap()
    x1_d = nc.dram_tensor("x1_scr", [S, D], F32, kind="Internal").ap()
    x2_d = nc.dram_tensor("x2_scr", [S, D], F32, kind="Internal").ap()
    x3_d = nc.dram_tensor("x3_scr", [S, D], F32, kind="Internal").ap()
    xg_d = nc.dram_tensor("xg_scr", [NSLOT + 128, D], BF16, kind="Internal").ap()
    yb_d = nc.dram_tensor("yb_scr", [NSLOT + 128, D], BF16, kind="Internal").ap()
    gu_d = nc.dram_tensor("gu_scr", [S, 1024], BF16, kind="Internal").ap()
    x1b_d = nc.dram_tensor("x1b_scr", [S, D], BF16, kind="Internal").ap()

    bigA = P.sb("bigA", [128, 16, 2048], BF16)
    bigB = P.sb("bigB", [128, 16, 2048], BF16)
    AR = Arena(P, 76 * 1024)
    ps = [P.ps(f"ps{i}", [128, 512]) for i in range(8)]
    ident = P.sb("ident", [128, 128], BF16)
    mask_bd = P.sb("mask_bd", [128, 128], BF16)
    tri_strict = P.sb("tri_strict", [128, 128], BF16)
    tril_st = P.sb("tril_st", [128, 128], F32)
    ones_bf = P.sb("ones_bf", [128, 128], BF16)
    ones_f = P.sb("ones_f", [128, 128], F32)
    ecap = P.sb("ecap", [128, 32], F32)
    pidx = P.sb("pidx", [128, 1], F32)
    dests = P.sb("dests", [128, 16, 2], I32)
    wts = P.sb("wts", [128, 16, 2], F32)
    zt = P.sb("zt", [128, 512], BF16)

    k = K()
    k.nc, k.P, k.din, k.ps, k.AR = nc, P, din, ps, AR
    k.bank = 0

    def nb():
        b = k.bank
        k.bank = (k.bank + 1) % 8
        return b
    k.nb = nb
    k.cnt = 0

    def uid(s):
        k.cnt += 1
        return f"{s}#{k.cnt}"

    pdma = lambda out, in_, r=(), w=(): P.dma('pool', lambda h: h.dma_start(out=out, in_=in_), r=r, w=w)
    sdma = lambda out, in_, r=(), w=(): P.dma('sp', lambda h: h.dma_start(out=out, in_=in_), r=r, w=w)

    def mm(out, lhsT, rhs, start, stop, r, w):
        P.pe(lambda h: h.matmul(out, lhsT=lhsT, rhs=rhs, start=start, stop=stop), r=r, w=w)

    pdma(ident[:], din['ident'], w=['ident'])
    pdma(mask_bd[:], din['mask_bd'], w=['mask_bd'])
    pdma(tri_strict[:], din['tri_strict'], w=['tri_strict'])
    sdma(tril_st[:], din['tril_st'], w=['tril_st'])
    sdma(ecap[:], din['ecap'], w=['ecap'])
    sdma(pidx[:], din['pidx'], w=['pidx'])
    P.dve(lambda h: h.memset(ones_bf[:], 1.0), w=['ones_bf'])
    P.dve(lambda h: h.memset(ones_f[:], 1.0), w=['ones_f'])
    zrow = AR.f32(2048)
    P.dve(lambda h: h.memset(zrow, 0.0), w=['zrow'])
    sdma(yb_d[NSLOT:NSLOT + 128, :], zrow.bitcast(BF16)[:, 0:2048], r=['zrow'], w=['yb_trash'])
    P.barrier()

    def load_wblk(dst, src_ap, key):
        pdma(dst, src_ap, w=[key])

    def proj_fm(xT, xkey, wb, wkey, evac):
        for tt in range(4):
            b = nb()
            for kc in range(16):
                mm(ps[b][:, :], wb[:, kc, :], xT[:, kc, tt * 512:(tt + 1) * 512], kc == 0, kc == 15,
                   r=[xkey, wkey], w=[f'ps{b}'])
            evac(tt, b)

    def proj_tm(xT, xkey, wb, wkey, ncol, evac):
        for g in range(4):
            b = nb()
            for jj in range(4):
                j = g * 4 + jj
                for kc in range(16):
                    mm(ps[b][:, jj * 128:jj * 128 + ncol], xT[:, kc, j * 128:(j + 1) * 128], wb[:, kc, 0:ncol],
                       kc == 0, kc == 15, r=[xkey, wkey], w=[f'ps{b}'])
            evac(g, b)

    def phase_a0():
        AR.reset()
        xT, yT = bigA, bigB
        if cut == 'const':
            raise _Cut()
        wblk = [AR.bf16(2048).rearrange("p (a b) -> p a b", b=128) for _ in range(2)]
        wcnt = [0]

        def next_w(blk):
            i = wcnt[0] % 2
            wcnt[0] += 1
            load_wblk(wblk[i], din['w_in0'][blk], f'wblk{i}')
            return wblk[i], f'wblk{i}'
        xsrc = din['xT'].rearrange("(kc p) t -> p kc t", p=128)
        for q in range(4):
            pdma(xT[:, 4 * q:4 * q + 4, :], xsrc[:, 4 * q:4 * q + 4, :], w=[f'xT'])
        if cut == 'xT':
            raise _Cut()
        mark = AR.off
        bm = AR.f32(8 * 512).rearrange("p (c x) -> p c x", x=512)
        kT = AR.bf16(128 + 2048)
        V0 = AR.bf16(2048).rearrange("p (a b) -> p a b", b=128)
        V1 = AR.bf16(2048).rearrange("p (a b) -> p a b", b=128)
        q0 = AR.bf16(2048)
        q1 = AR.bf16(2048)
        tb = [AR.f32(512) for _ in range(4)]
        eb = [AR.f32(512) for _ in range(4)]
        pb = [AR.bf16(512) for _ in range(4)]
        pTb = [AR.bf16(512) for _ in range(4)]
        sinks = AR.f32(16).rearrange("p (c x) -> p c x", x=2)
        mskn = AR.f32(256)
        sm = [AR.f32(16) for _ in range(4)]
        sdma(bm, din['bias_g'].rearrange("p c s k -> p c (s k)"), w=['bm'])
        sdma(mskn, din['maskneg'], w=['mskn'])
        sdma(sinks, din['sinks'], w=['sinks'])
        P.pool(lambda h: h.memset(zt[:], 0.0), w=['zt'])
        k.zkeys = []
        for q in range(NSLOT // 128 + 1):
            zk = uid('xgz')
            sdma(xg_d[q * 128:(q + 1) * 128, :].rearrange("p (a c) -> p a c", c=512), zt[:].unsqueeze(1).to_broadcast([128, 4, 512]), r=['zt'], w=[zk])
            k.zkeys.append(zk)
        P.dve(lambda h: h.tensor_tensor(out=bm.rearrange("p c (s k) -> p (c s) k", k=256),
                                        in0=bm.rearrange("p c (s k) -> p (c s) k", k=256),
                                        in1=mskn.unsqueeze(1).to_broadcast([128, 16, 256]), op=ALU.add),
              r=['bm', 'mskn'], w=['bm'])
        P.pool(lambda h: h.memset(kT[:, 0:128], 0.0), w=['kT'])
        P.pool(lambda h: h.memset(V0, 0.0), w=['V0'])
        P.pool(lambda h: h.memset(V1, 0.0), w=['V1'])
        P.pool(lambda h: h.memset(q0, 0.0), w=['q0'])
        P.pool(lambda h: h.memset(q1, 0.0), w=['q1'])
        if cut == 'setup':
            raise _Cut()
        wb, wk = next_w(8)
        proj_fm(xT, 'xT', wb, wk, lambda tt, b: P.act(
            lambda h: h.copy(out=kT[:, 128 + tt * 512:128 + (tt + 1) * 512], in_=ps[b][:, :]), r=[f'ps{b}'], w=['kT']))
        if cut == 'k':
            raise _Cut()
        wb, wk = next_w(9)

        def ev_v(g, b):
            for jj in range(4):
                j = 4 * g + jj
                P.act(lambda h, j=j, jj=jj: h.copy(out=V0[:, j, 0:64], in_=ps[b][:, jj * 128:jj * 128 + 64]), r=[f'ps{b}'], w=['V0'])
                P.dve(lambda h, j=j, jj=jj: h.tensor_copy(out=V1[:, j, 64:128], in_=ps[b][:, jj * 128 + 64:jj * 128 + 128]), r=[f'ps{b}'], w=['V1'])
        proj_tm(xT, 'xT', wb, wk, 128, ev_v)
        if cut == 'kv':
            raise _Cut()
        for c in range(8):
            if cut == 'att1' and c == 1:
                raise _Cut()
            wb, wk = next_w(c)

            def ev_q(tt, b):
                P.act(lambda h: h.copy(out=q0[0:64, tt * 512:(tt + 1) * 512], in_=ps[b][0:64, :]), r=[f'ps{b}'], w=['q0'])
                P.dve(lambda h: h.tensor_copy(out=q1[64:128, tt * 512:(tt + 1) * 512], in_=ps[b][64:128, :]), r=[f'ps{b}'], w=['q1'])
            proj_fm(xT, 'xT', wb, wk, ev_q)
            for n0 in range(0, 16, 4):
                by = (n0 // 4) % 2
                sbank = [2, 3, 4, 5]
                tbank = [6, 7, 2, 3]
                ctx = []
                for i in range(4):
                    n = n0 + i
                    t, e, p_bf, pT, s = tb[i], eb[i], pb[i], pTb[i], sm[i]
                    d = dict(n=n, t=t, e=e, p=p_bf, pT=pT, tk=f't{i}', ek=f'e{i}', pk=f'p{i}', ptk=f'pT{i}', sk=f'sm{i}',
                             mx=s[:, 0:2], nmx=s[:, 2:4], rs=s[:, 4:6], es=s[:, 6:8], sd=s[:, 8:10], rinv=s[:, 10:12],
                             t3=t.rearrange("p (s k) -> p s k", k=256), e3=e.rearrange("p (s k) -> p s k", k=256),
                             p3=p_bf.rearrange("p (s k) -> p s k", k=256), pT3=pT.rearrange("p (a b) -> p a b", b=128),
                             b=sbank[i], bT=tbank[i], halves=([1] if n == 0 else [0, 1]))
                    ctx.append(d)
                for d in ctx:
                    n, bq = d['n'], d['b']
                    qs = slice(n * 128, (n + 1) * 128)
                    mm(ps[bq][:, 0:256], q0[:, qs], kT[:, n * 128:n * 128 + 256], True, True, r=['q0', 'kT'], w=[f'ps{bq}'])
                    mm(ps[bq][:, 256:512], q1[:, qs], kT[:, n * 128:n * 128 + 256], True, True, r=['q1', 'kT'], w=[f'ps{bq}'])
                for d in ctx:
                    P.dve(lambda h, d=d, c=c: h.scalar_tensor_tensor(out=d['t'], in0=ps[d['b']][:, :], scalar=0.125, in1=bm[:, c, :],
                                                                    op0=ALU.mult, op1=ALU.add), r=[f"ps{d['b']}", 'bm'], w=[d['tk']])
                    if d['n'] == 0:
                        P.pool(lambda h, d=d: h.memset(d['t3'][:, :, 0:128], NEG), w=[d['tk']])
                for d in ctx:
                    P.dve(lambda h, d=d: h.tensor_reduce(out=d['mx'], in_=d['t3'], axis=AX.X, op=ALU.max), r=[d['tk']], w=[d['sk']])
                for d in ctx:
                    P.dve(lambda h, d=d, c=c: h.tensor_tensor(out=d['mx'], in0=d['mx'], in1=sinks[:, c, :], op=ALU.max), r=[d['sk'], 'sinks'], w=[d['sk']])
                for d in ctx:
                    P.dve(lambda h, d=d: h.tensor_scalar(out=d['nmx'], in0=d['mx'], scalar1=-1.0, scalar2=None, op0=ALU.mult), r=[d['sk']], w=[d['sk']])
                for d in ctx:
                    P.dve(lambda h, d=d, c=c: h.tensor_tensor(out=d['sd'], in0=sinks[:, c, :], in1=d['nmx'], op=ALU.add), r=[d['sk'], 'sinks'], w=[d['sk']])
                for d in ctx:
                    for hs in range(2):
                        P.act(lambda h, hs=hs, d=d: h.activation(
                            out=d['e3'][:, hs, :], in_=d['t3'][:, hs, :], func=AF.Exp, bias=d['nmx'][:, hs:hs + 1], scale=1.0,
                            accum_out=d['rs'][:, hs:hs + 1]), r=[d['tk'], d['sk']], w=[f"{d['ek']}{hs}", f"{d['sk']}r{hs}"])
                    P.act(lambda h, d=d: h.activation(out=d['es'], in_=d['sd'], func=AF.Exp), r=[d['sk']], w=[d['sk'] + 'e'])
                for d in ctx:
                    sk = d['sk']
                    P.dve(lambda h, d=d: h.tensor_tensor(out=d['rs'], in0=d['rs'], in1=d['es'], op=ALU.add), r=[sk + 'r0', sk + 'r1', sk + 'e'], w=[sk + 'r0', sk + 'r1'])
                for d in ctx:
                    sk = d['sk']
                    P.dve(lambda h, d=d: h.reciprocal(out=d['rinv'], in_=d['rs']), r=[sk + 'r0', sk + 'r1'], w=[sk + 'i'])
                for d in ctx:
                    P.pool(lambda h, d=d: h.tensor_tensor(
                        out=d['p3'], in0=d['e3'], in1=d['rinv'].unsqueeze(2).to_broadcast([128, 2, 256]), op=ALU.mult),
                        r=[d['ek'] + '0', d['ek'] + '1', d['sk'] + 'i'], w=[d['pk']])
                for d in ctx:
                    bT = d['bT']
                    for hs in range(2):
                        for hf in d['halves']:
                            mm(ps[bT][:, (hs * 2 + hf) * 128:(hs * 2 + hf + 1) * 128], d['p3'][:, hs, hf * 128:(hf + 1) * 128], ident[:],
                               True, True, r=[d['pk'], 'ident'], w=[f'ps{bT}'])
                for i, d in enumerate(ctx):
                    if i % 2 == 0:
                        P.act(lambda h, d=d: h.copy(out=d['pT'], in_=ps[d['bT']][:, :]), r=[f"ps{d['bT']}"], w=[d['ptk']])
                    else:
                        P.dve(lambda h, d=d: h.tensor_copy(out=d['pT'], in_=ps[d['bT']][:, :]), r=[f"ps{d['bT']}"], w=[d['ptk']])
                for d in ctx:
                    n = d['n']
                    items = [(hs, hf) for hs in range(2) for hf in d['halves']]
                    for ii, (hs, hf) in enumerate(items):
                        Vx, vk = (V0, 'V0') if hs == 0 else (V1, 'V1')
                        mm(ps[by][:, (n % 4) * 128:(n % 4 + 1) * 128], Vx[:, n - 1 + hf, :], d['pT3'][:, hs * 2 + hf, :],
                           ii == 0, ii == len(items) - 1, r=[vk, d['ptk']], w=[f'ps{by}'])
                P.dve(lambda h, by=by, c=c, n0=n0: h.tensor_copy(out=yT[:, c, n0 * 128:(n0 + 4) * 128], in_=ps[by][:, :]),
                      r=[f'ps{by}'], w=['yT'])
        if cut == 'att':
            raise _Cut()
        P.barrier()
        P.tag = 'phase_a0_hgrn'
        AR.off = mark
        sq = AR.bf16(2048)
        omf = AR.bf16(2048)
        fA = AR.f32(2048)
        bt = AR.f32(2048)
        tmp1 = AR.f32(2048)
        qd2 = AR.bf16(2048)
        qd = AR.bf16(2048)
        kd = AR.bf16(2048)
        kd2 = AR.bf16(2048)
        kdx = AR.bf16(2048).rearrange("p (j s) -> p j s", s=128)
        Vh = AR.bf16(2048).rearrange("p (j s) -> p j s", s=128)
        gs = AR.bf16(2048).rearrange("p (j s) -> p j s", s=128)
        lbt = AR.f32(24).rearrange("p (h l) -> p h l", l=3)
        lb = AR.f32(8)
        oml = AR.f32(8)
        lsum = AR.f32(8)
        ebl = AR.f32(16)
        Sst = AR.f32(128)
        y_sbs = [AR.bf16(128) for _ in range(2)]
        ss_all = AR.f32(48).rearrange("p (j s) -> p j s", s=3)
        normg = AR.f32(1024)
        tmp2 = fA
        sdma(lbt, din['lbl'], w=['lbt'])
        sdma(normg, din['normg'], w=['normg'])
        P.act(lambda h: h.activation(out=lbt, in_=lbt, func=AF.Exp), r=['lbt'], w=['lbt'])
        P.dve(lambda h: h.tensor_reduce(out=lsum, in_=lbt, axis=AX.X, op=ALU.add), r=['lbt'], w=['lsum'])
        P.dve(lambda h: h.reciprocal(out=lsum, in_=lsum), r=['lsum'], w=['lsum'])
        P.dve(lambda h: h.tensor_tensor(out=lb, in0=lbt[:, :, 0], in1=lsum, op=ALU.mult), r=['lbt', 'lsum'], w=['lb'])
        P.dve(lambda h: h.tensor_scalar(out=oml, in0=lb, scalar1=-1.0, scalar2=1.0, op0=ALU.mult, op1=ALU.add), r=['lb'], w=['oml'])
        P.pool(lambda h: h.memset(kdx, 0.0), w=['kdx'])
        def projqf(hd):
            wb, wk = next_w(10 + 4 * hd + 0)
            proj_fm(xT, 'xT', wb, wk, lambda tt, b: P.act(
                lambda h: h.activation(out=sq[:, tt * 512:(tt + 1) * 512], in_=ps[b][:, :], func=AF.Silu), r=[f'ps{b}'], w=['sq']))
            wb, wk = next_w(10 + 4 * hd + 1)
            proj_fm(xT, 'xT', wb, wk, lambda tt, b: P.act(
                lambda h: h.activation(out=fA[:, tt * 512:(tt + 1) * 512], in_=ps[b][:, :], func=AF.Sigmoid), r=[f'ps{b}'], w=['fA']))
        sqj = AR.f32(256)
        projqf(0)
        for hd in range(8):
            P.dve(lambda h, hd=hd: h.tensor_scalar(out=fA, in0=fA, scalar1=oml[:, hd:hd + 1], scalar2=lb[:, hd:hd + 1],
                                                  op0=ALU.mult, op1=ALU.add), r=['fA', 'oml', 'lb'], w=['fA'])
            P.act(lambda h: h.activation(out=tmp1, in_=fA, func=AF.Ln), r=['fA'], w=['tmp1'])
            for j in range(16):
                P.dve(lambda h, j=j: h.tensor_tensor_scan(out=bt[:, j * 128:(j + 1) * 128], data0=ones_f[:, :],
                                                         data1=tmp1[:, j * 128:(j + 1) * 128], initial=0.0, op0=ALU.mult, op1=ALU.add),
                      r=['tmp1', 'ones_f'], w=['bt'])
            P.pool(lambda h: h.tensor_scalar(out=omf, in0=fA, scalar1=-1.0, scalar2=1.0, op0=ALU.mult, op1=ALU.add), r=['fA'], w=['omf'])
            P.act(lambda h: h.activation(out=tmp1, in_=bt, func=AF.Exp), r=['bt'], w=['tmp1'])
            P.pool(lambda h: h.tensor_tensor(out=qd2, in0=sq, in1=tmp1, op=ALU.mult), r=['sq', 'tmp1'], w=['qd2'])
            bt64 = bt.rearrange("p (c s) -> p c s", s=64)
            P.dve(lambda h: h.tensor_tensor(out=tmp2.rearrange("p (c s) -> p c s", s=64), in0=bt64,
                                            in1=bt64[:, :, 31:32].to_broadcast([128, 32, 64]), op=ALU.subtract), r=['bt', 'omf'], w=['fA'])
            P.act(lambda h: h.activation(out=tmp1, in_=tmp2, func=AF.Exp), r=['fA', 'qd2'], w=['tmp1'])
            P.dve(lambda h: h.tensor_tensor(out=qd, in0=sq, in1=tmp1, op=ALU.mult), r=['sq', 'tmp1'], w=['qd'])
            P.act(lambda h: h.activation(out=tmp1, in_=tmp2, func=AF.Exp, scale=-1.0), r=['fA', 'qd'], w=['tmp1'])
            P.dve(lambda h: h.tensor_tensor(out=kd, in0=omf, in1=tmp1, op=ALU.mult), r=['omf', 'tmp1'], w=['kd'])
            bt128 = bt.rearrange("p (j s) -> p j s", s=128)
            P.dve(lambda h: h.tensor_tensor(out=tmp2.rearrange("p (j s) -> p j s", s=128), in0=bt128,
                                            in1=bt128[:, :, 127:128].to_broadcast([128, 16, 128]), op=ALU.subtract), r=['bt', 'kd'], w=['fA'])
            P.act(lambda h: h.activation(out=tmp1, in_=tmp2, func=AF.Exp, scale=-1.0), r=['fA', 'kd'], w=['tmp1'])
            P.dve(lambda h: h.tensor_tensor(out=kd2, in0=omf, in1=tmp1, op=ALU.mult), r=['omf', 'tmp1'], w=['kd2'])
            P.dve(lambda h: h.tensor_tensor(out=tmp2.rearrange("p (j s) -> p j s", s=128)[:, :, 0:64], in0=bt128[:, :, 0:64],
                                            in1=bt128[:, :, 95:96].to_broadcast([128, 16, 64]), op=ALU.subtract), r=['bt', 'kd2'], w=['fA'])
            P.act(lambda h: h.activation(out=tmp1.rearrange("p (j s) -> p j s", s=128)[:, :, 0:64],
                                         in_=tmp2.rearrange("p (j s) -> p j s", s=128)[:, :, 0:64], func=AF.Exp, scale=-1.0),
                  r=['fA', 'kd2'], w=['tmp1'])
            P.dve(lambda h: h.tensor_tensor(out=kdx[:, :, 0:64], in0=omf.rearrange("p (j s) -> p j s", s=128)[:, :, 0:64],
                                            in1=tmp1.rearrange("p (j s) -> p j s", s=128)[:, :, 0:64], op=ALU.mult),
                  r=['omf', 'tmp1'], w=['kdx'])
            P.act(lambda h: h.activation(out=ebl, in_=bt128[:, :, 127], func=AF.Exp), r=['bt'], w=['ebl'])
            wb, wk = next_w(10 + 4 * hd + 2)
            proj_tm(xT, 'xT', wb, wk, 128, lambda g, b: P.act(
                lambda h: h.copy(out=Vh[:, 4 * g:4 * g + 4, :], in_=ps[b][:, :].rearrange("p (j c) -> p j c", c=128)),
                r=[f'ps{b}'], w=['Vh']))
            wb, wk = next_w(10 + 4 * hd + 3)

            def ev_g(g, b, hd=hd):
                t4 = tmp1[:, 0:512].rearrange("p (j c) -> p j c", c=128)
                P.act(lambda h: h.activation(out=tmp1[:, 0:512], in_=ps[b][:, :], func=AF.Silu), r=[f'ps{b}', 'kdx'], w=['tmp1'])
                P.pool(lambda h: h.tensor_tensor(out=gs[:, 4 * g:4 * g + 4, :], in0=t4,
                                                 in1=normg[:, hd * 128:(hd + 1) * 128].unsqueeze(1).to_broadcast([128, 4, 128]),
                                                 op=ALU.mult), r=['tmp1', 'normg'], w=['gs'])
            proj_tm(xT, 'xT', wb, wk, 128, ev_g)
            A_all = tmp1.bitcast(BF16)[:, 0:2048].rearrange("p (j s) -> p j s", s=128)
            kd2T_all = tmp1.bitcast(BF16)[:, 2048:4096].rearrange("p (j s) -> p j s", s=128)
            Sbf_all = bt.bitcast(BF16)[:, 0:17 * 128].rearrange("p (j s) -> p j s", s=128)
            for j in range(16):
                js = slice(j * 128, (j + 1) * 128)
                bA = nb()
                mm(ps[bA][:, 0:128], kd[:, js], qd[:, js], True, True, r=['kd', 'qd'], w=[f'ps{bA}'])
                mm(ps[bA][:, 128:192], kdx[:, j, :], qd[:, j * 128 + 64:(j + 1) * 128], True, True, r=['kdx', 'qd'], w=[f'ps{bA}'])
                mm(ps[bA][:, 256:384], kd2[:, js], ident[:], True, True, r=['kd2', 'ident'], w=[f'ps{bA}'])
                P.dve(lambda h, bA=bA, j=j: h.tensor_tensor(out=A_all[:, j, :], in0=ps[bA][:, 0:128], in1=mask_bd[:], op=ALU.mult),
                      r=[f'ps{bA}', 'mask_bd', 'ebl', 'gs'], w=['tmp1'])
                P.dve(lambda h, bA=bA, j=j: h.tensor_copy(out=A_all[0:64, j, 64:128], in_=ps[bA][0:64, 128:192]), r=[f'ps{bA}'], w=['tmp1'])
                P.act(lambda h, bA=bA, j=j: h.copy(out=kd2T_all[:, j, :], in_=ps[bA][:, 256:384]), r=[f'ps{bA}', 'tmp1'], w=['tmp1k'])
            if hd + 1 < 8:
                projqf(hd + 1)
            P.dve(lambda h: h.memset(Sst, 0.0), w=['S'])
            P.pool(lambda h: h.memset(Sbf_all[:, 0, :], 0.0), r=['ebl'], w=['bt'])
            bU = None
            for j in range(16):
                if j % 4 == 0:
                    bU = nb()
                mm(ps[bU][:, (j % 4) * 128:(j % 4 + 1) * 128], kd2T_all[:, j, :], Vh[:, j, :], True, True, r=['tmp1k', 'Vh'], w=[f'ps{bU}'])
                P.dve(lambda h, bU=bU, j=j: h.scalar_tensor_tensor(out=Sst, in0=Sst, scalar=ebl[:, j:j + 1], in1=ps[bU][:, (j % 4) * 128:(j % 4 + 1) * 128],
                                                                  op0=ALU.mult, op1=ALU.add), r=['S', 'ebl', f'ps{bU}'], w=['S'])
                P.act(lambda h, j=j: h.copy(out=Sbf_all[:, j + 1, :], in_=Sst), r=['S'], w=['bt'])
            bTr = None
            for j in range(16):
                js = slice(j * 128, (j + 1) * 128)
                bO = nb()
                ysb = y_sbs[j % 2]
                yk = f'y_sb{j % 2}'
                mm(ps[bO][:, 0:128], A_all[:, j, :], Vh[:, j, :], True, False, r=['tmp1', 'Vh'], w=[f'ps{bO}'])
                mm(ps[bO][:, 0:128], qd2[:, js], Sbf_all[:, j, :], False, True, r=['qd2', 'bt'], w=[f'ps{bO}'])
                P.act(lambda h, bO=bO, j=j: h.activation(out=sqj[:, (j % 2) * 128:(j % 2 + 1) * 128], in_=ps[bO][:, 0:128], func=AF.Square,
                                                          accum_out=ss_all[:, j, 0:1]), r=[f'ps{bO}'], w=[f'ss{j}', f'sqj{j % 2}'])
                P.act(lambda h, j=j: h.activation(out=ss_all[:, j, 1:2], in_=ss_all[:, j, 0:1], func=AF.Sqrt, bias=RMS_EPS, scale=1.0 / 128),
                      r=[f'ss{j}'], w=[f'ss1{j}'])
                P.dve(lambda h, j=j: h.reciprocal(out=ss_all[:, j, 2:3], in_=ss_all[:, j, 1:2]), r=[f'ss1{j}'], w=[f'ss2{j}'])
                P.dve(lambda h, bO=bO, j=j, ysb=ysb: h.scalar_tensor_tensor(out=ysb, in0=ps[bO][:, 0:128], scalar=ss_all[:, j, 2:3], in1=gs[:, j, :],
                                                                           op0=ALU.mult, op1=ALU.mult), r=[f'ps{bO}', f'ss2{j}', 'gs'], w=[yk])
                if j % 4 == 0:
                    bTr = nb()
                mm(ps[bTr][:, (j % 4) * 128:(j % 4 + 1) * 128], ysb, ident[:], True, True, r=[yk, 'ident'], w=[f'ps{bTr}'])
                if j % 4 == 3:
                    P.act(lambda h, bTr=bTr, j=j, hd=hd: h.copy(out=yT[:, 8 + hd, (j - 3) * 128:(j + 1) * 128], in_=ps[bTr][:, :]),
                          r=[f'ps{bTr}'], w=['yT'])
        P.barrier()


    def layer_norm_gen(src, skey, dst, dkey, lnp, lnkey, junk, st, jk='junk', sx=''):
        P.act(lambda h: h.activation(out=junk, in_=src, func=AF.Copy, accum_out=st[:, 0:1]), r=[skey], w=[jk, 'st0' + sx])
        yield
        P.act(lambda h: h.activation(out=junk, in_=src, func=AF.Square, accum_out=st[:, 1:2]), r=[skey], w=[jk, 'st1' + sx])
        yield
        P.dve(lambda h: h.tensor_scalar(out=st[:, 2:3], in0=st[:, 0:1], scalar1=1.0 / D, scalar2=None, op0=ALU.mult), r=['st0' + sx], w=['st2' + sx])
        yield
        P.dve(lambda h: h.tensor_tensor(out=st[:, 3:4], in0=st[:, 2:3], in1=st[:, 2:3], op=ALU.mult), r=['st2' + sx], w=['st3' + sx])
        yield
        P.dve(lambda h: h.scalar_tensor_tensor(out=st[:, 4:5], in0=st[:, 1:2], scalar=1.0 / D, in1=st[:, 3:4], op0=ALU.mult, op1=ALU.subtract),
              r=['st1' + sx, 'st3' + sx], w=['st4' + sx])
        yield
        P.act(lambda h: h.activation(out=st[:, 5:6], in_=st[:, 4:5], func=AF.Sqrt, bias=LN_EPS, scale=1.0), r=['st4' + sx], w=['st5' + sx])
        yield
        P.dve(lambda h: h.reciprocal(out=st[:, 6:7], in_=st[:, 5:6]), r=['st5' + sx], w=['st6' + sx])
        yield
        P.dve(lambda h: h.tensor_scalar(out=dst, in0=src, scalar1=st[:, 2:3], scalar2=st[:, 6:7], op0=ALU.subtract, op1=ALU.mult),
              r=[skey, 'st2' + sx, 'st6' + sx], w=[dkey])
        yield
        P.dve(lambda h: h.tensor_tensor(out=dst, in0=dst, in1=lnp[:, 0, :], op=ALU.mult), r=[dkey, lnkey], w=[dkey])
        yield
        P.dve(lambda h: h.tensor_tensor(out=dst, in0=dst, in1=lnp[:, 1, :], op=ALU.add), r=[dkey, lnkey], w=[dkey])
        yield


    def layer_norm_tile(src, skey, dst, dkey, lnp, lnkey, junk, st):
        for _ in layer_norm_gen(src, skey, dst, dkey, lnp, lnkey, junk, st):
            pass

    def zip_run(gens):
        gens = list(gens)
        while gens:
            for g in list(gens):
                try:
                    next(g)
                except StopIteration:
                    gens.remove(g)

    def phase_c(l, xin_d, xout_d):
        AR.reset()
        Wout, yT = bigA, bigB
        wsrc = din[f'w_out{l}']
        for q in range(4):
            pdma(Wout[:, 4 * q:4 * q + 4, :], wsrc[:, 4 * q:4 * q + 4, :], w=['xT'])
        lnp = AR.f32(4096).rearrange("p (a b) -> p a b", b=2048)
        sdma(lnp, din[f'lnmix{l}'], w=['lnp'])
        wr_bf = AR.bf16(16 * 36).rearrange("p (a b) -> p a b", b=36)
        pdma(wr_bf, din[f'wr{l}'], w=['wr'])
        brt = AR.f32(36)
        sdma(brt, din[f'br{l}'], w=['brt'])
        zkeys = k.zkeys if l == 0 else []
        lg_all = AR.f32(16 * 36).rearrange("p (j c) -> p j c", c=36)
        markc = AR.off
        xs = [AR.f32(2048) for _ in range(2)]
        rbufs = [AR.f32(2048) for _ in range(2)]
        x1Ts = [AR.bf16(2048).rearrange("p (a b) -> p a b", b=128) for _ in range(2)]
        sts = [AR.f32(8) for _ in range(2)]
        BIGV = 1000.0

        def load_x(j):
            sdma(xs[j % 2], xin_d[j * 128:(j + 1) * 128, :], w=[f'xs{j % 2}'])

        def tile_c(j):
            pr = j % 2
            x_s, xk = xs[pr], f'xs{pr}'
            rbuf, rk = rbufs[pr], f'rbuf{pr}'
            x1T, st = x1Ts[pr], sts[pr]
            junk_j = x_s.bitcast(BF16)[:, 0:2048]
            xb = x_s.bitcast(BF16)[:, 2048:4096]
            for db in range(4):
                b = nb()
                for fc in range(16):
                    mm(ps[b][:, :], yT[:, fc, j * 128:(j + 1) * 128], Wout[:, fc, db * 512:(db + 1) * 512], fc == 0, fc == 15,
                       r=['yT', 'xT'], w=[f'ps{b}'])
                P.dve(lambda h, b=b, db=db: h.scalar_tensor_tensor(
                    out=rbuf[:, db * 512:(db + 1) * 512], in0=x_s[:, db * 512:(db + 1) * 512], scalar=ALPHA, in1=ps[b][:, :],
                    op0=ALU.mult, op1=ALU.add), r=[xk, f'ps{b}'], w=[f'{rk}_{db}'])
                yield
            P.dve(lambda h: h.tensor_copy(out=st[:, 7:8], in_=st[:, 7:8]), r=[f'{rk}_{d_}' for d_ in range(4)], w=[rk] + [f'{rk}_{d_}' for d_ in range(4)])
            yield
            yield from layer_norm_gen(rbuf, rk, rbuf, rk, lnp, 'lnp', junk_j, st, jk=xk, sx=f'_{pr}')
            sdma(xout_d[j * 128:(j + 1) * 128, :], rbuf, r=[rk], w=[uid('xout'), rk])
            yield
            P.act(lambda h: h.copy(out=xb, in_=rbuf), r=[rk], w=[xk])
            yield
            sdma(x1b_d[j * 128:(j + 1) * 128, :], xb, r=[xk], w=[uid('x1bd'), xk])
            yield
            for g4 in range(4):
                b = nb()
                for q in range(4):
                    kc = g4 * 4 + q
                    mm(ps[b][:, q * 128:(q + 1) * 128], xb[:, kc * 128:(kc + 1) * 128], ident[:], True, True, r=[xk, 'ident'], w=[f'ps{b}'])
                if g4 % 2 == 0:
                    P.act(lambda h, b=b, g4=g4: h.copy(out=x1T[:, 4 * g4:4 * g4 + 4, :], in_=ps[b][:, :].rearrange("p (a b) -> p a b", b=128)),
                          r=[f'ps{b}'], w=[f'x1T{pr}_{g4}'])
                else:
                    P.dve(lambda h, b=b, g4=g4: h.tensor_copy(out=x1T[:, 4 * g4:4 * g4 + 4, :], in_=ps[b][:, :].rearrange("p (a b) -> p a b", b=128)),
                          r=[f'ps{b}'], w=[f'x1T{pr}_{g4}'])
                yield
            b = nb()
            for kc in range(16):
                mm(ps[b][:, 0:36], x1T[:, kc, :], wr_bf[:, kc, :], kc == 0, kc == 15, r=[f'x1T{pr}_{kc // 4}', 'wr'], w=[f'ps{b}'])
            P.dve(lambda h, b=b: h.tensor_tensor(out=lg_all[:, j, :], in0=ps[b][:, 0:36], in1=brt, op=ALU.add), r=[f'ps{b}', 'brt'], w=[uid('lg')])
            yield
        load_x(0)
        load_x(1)
        for j in range(0, NT, 2):
            zip_run([tile_c(j), tile_c(j + 1)])
            if j + 2 < NT:
                load_x(j + 2)
                load_x(j + 3)
        P.barrier()
        AR.off = markc
        V = P.dve
        T3 = lambda n: AR.f32(16 * n).rearrange("p (j c) -> p j c", c=n)
        gmask, eg, pen = T3(4), T3(4), T3(4)
        em, em2, mask1, mask2, tmpe, rank, slot, okm, cs, base = [T3(32) for _ in range(10)]
        m12b = AR.bf16(512).rearrange("p (j c) -> p j c", c=32)
        gmax, gsum, gw, m1, m2, dd, e2, den, w1, w2 = [AR.f32(16) for _ in range(10)]
        dstk, okk, dfin = AR.f32(16), AR.f32(16), AR.f32(16)
        G = lg_all[:, :, 0:4]
        L = lg_all[:, :, 4:36]
        bc = lambda a, n: a.unsqueeze(2).to_broadcast([128, 16, n])
        V(lambda h: h.tensor_reduce(out=gmax, in_=G, axis=AX.X, op=ALU.max), r=['lg'], w=['gmax'])
        V(lambda h: h.tensor_tensor(out=gmask, in0=G, in1=bc(gmax, 4), op=ALU.is_equal), r=['lg', 'gmax'], w=['gmask'])
        V(lambda h: h.tensor_tensor(out=eg, in0=G, in1=bc(gmax, 4), op=ALU.subtract), r=['lg', 'gmax'], w=['eg'])
        P.act(lambda h: h.activation(out=eg, in_=eg, func=AF.Exp), r=['eg'], w=['eg'])
        V(lambda h: h.tensor_reduce(out=gsum, in_=eg, axis=AX.X, op=ALU.add), r=['eg'], w=['gsum'])
        V(lambda h: h.reciprocal(out=gw, in_=gsum), r=['gsum'], w=['gw'])
        V(lambda h: h.tensor_scalar(out=pen, in0=gmask, scalar1=BIGV, scalar2=-BIGV, op0=ALU.mult, op1=ALU.add), r=['gmask'], w=['pen'])
        em64 = em.rearrange("p j (g e) -> p (j g) e", e=8)
        pen64 = pen.rearrange("p j g -> p (j g)")
        V(lambda h: h.tensor_copy(out=em, in_=L), r=['lg'], w=['em'])
        V(lambda h: h.tensor_tensor(out=em64, in0=em64, in1=pen64.unsqueeze(2).to_broadcast([128, 64, 8]), op=ALU.add), r=['em', 'pen'], w=['em'])
        V(lambda h: h.tensor_reduce(out=m1, in_=em, axis=AX.X, op=ALU.max), r=['em'], w=['m1'])
        V(lambda h: h.tensor_tensor(out=mask1, in0=em, in1=bc(m1, 32), op=ALU.is_equal), r=['em', 'm1'], w=['mask1'])
        V(lambda h: h.scalar_tensor_tensor(out=em2, in0=mask1, scalar=-BIGV, in1=em, op0=ALU.mult, op1=ALU.add), r=['mask1', 'em'], w=['em2'])
        V(lambda h: h.tensor_reduce(out=m2, in_=em2, axis=AX.X, op=ALU.max), r=['em2'], w=['m2'])
        V(lambda h: h.tensor_tensor(out=mask2, in0=em2, in1=bc(m2, 32), op=ALU.is_equal), r=['em2', 'm2'], w=['mask2'])
        V(lambda h: h.tensor_tensor(out=dd, in0=m2, in1=m1, op=ALU.subtract), r=['m1', 'm2'], w=['dd'])
        P.act(lambda h: h.activation(out=e2, in_=dd, func=AF.Exp), r=['dd'], w=['e2'])
        V(lambda h: h.tensor_scalar(out=den, in0=e2, scalar1=1.0, scalar2=None, op0=ALU.add), r=['e2'], w=['den'])
        V(lambda h: h.reciprocal(out=den, in_=den), r=['den'], w=['den'])
        V(lambda h: h.tensor_tensor(out=w1, in0=den, in1=gw, op=ALU.mult), r=['den', 'gw'], w=['w1'])
        V(lambda h: h.tensor_tensor(out=w2, in0=w1, in1=e2, op=ALU.mult), r=['w1', 'e2'], w=['w2'])
        V(lambda h: h.tensor_tensor(out=tmpe, in0=mask1, in1=mask2, op=ALU.add), r=['mask1', 'mask2'], w=['tmpe'])
        V(lambda h: h.tensor_copy(out=m12b, in_=tmpe), r=['tmpe'], w=['m12b'])
        bR, bC = nb(), nb()
        for j in range(NT):
            mm(ps[bR][:, j * 32:(j + 1) * 32], tri_strict[:], m12b[:, j, :], True, True, r=['tri_strict', 'm12b'], w=[f'ps{bR}'])
        for j in range(NT):
            mm(ps[bC][:, j * 32:(j + 1) * 32], ones_bf[:], m12b[:, j, :], True, True, r=['ones_bf', 'm12b'], w=[f'ps{bC}'])
        V(lambda h: h.tensor_copy(out=cs, in_=ps[bC][:, :].rearrange("p (j c) -> p j c", c=32)), r=[f'ps{bC}'], w=['cs'])
        V(lambda h: h.memset(base[:, 0, :], 0.0), w=['base'])
        for j in range(1, NT):
            V(lambda h, j=j: h.tensor_tensor(out=base[:, j, :], in0=base[:, j - 1, :], in1=cs[:, j - 1, :], op=ALU.add), r=['base', 'cs'], w=['base'])
        V(lambda h: h.tensor_tensor(out=rank, in0=ps[bR][:, :].rearrange("p (j c) -> p j c", c=32), in1=base, op=ALU.add), r=[f'ps{bR}', 'base'], w=['rank'])
        V(lambda h: h.tensor_tensor(out=slot, in0=rank, in1=ecap[:].unsqueeze(1).to_broadcast([128, 16, 32]), op=ALU.add), r=['rank', 'ecap'], w=['slot'])
        V(lambda h: h.tensor_single_scalar(out=okm, in_=rank, scalar=float(CAP), op=ALU.is_lt), r=['rank'], w=['okm'])
        for kk, (mk, mkey, wk_, wkey) in enumerate([(mask1, 'mask1', w1, 'w1'), (mask2, 'mask2', w2, 'w2')]):
            V(lambda h, mk=mk: h.tensor_tensor(out=tmpe, in0=mk, in1=slot, op=ALU.mult), r=[mkey, 'slot', 'm12b'], w=['tmpe'])
            V(lambda h: h.tensor_reduce(out=dstk, in_=tmpe, axis=AX.X, op=ALU.add), r=['tmpe'], w=['dstk'])
            V(lambda h, mk=mk: h.tensor_tensor(out=tmpe, in0=mk, in1=okm, op=ALU.mult), r=[mkey, 'okm', 'dstk'], w=['tmpe'])
            V(lambda h: h.tensor_reduce(out=okk, in_=tmpe, axis=AX.X, op=ALU.add), r=['tmpe'], w=['okk'])
            V(lambda h: h.tensor_scalar(out=dfin, in0=dstk, scalar1=pidx[:, 0:1], scalar2=None, op0=ALU.subtract), r=['dstk', 'pidx'], w=['dfin'])
            V(lambda h: h.tensor_tensor(out=dfin, in0=dfin, in1=okk, op=ALU.mult), r=['dfin', 'okk'], w=['dfin'])
            V(lambda h: h.tensor_scalar(out=dfin, in0=dfin, scalar1=pidx[:, 0:1], scalar2=None, op0=ALU.add), r=['dfin', 'pidx'], w=['dfin'])
            V(lambda h, kk=kk: h.tensor_copy(out=dests[:, :, kk], in_=dfin), r=['dfin'], w=[f'dests{kk}'])
            V(lambda h, kk=kk, wk_=wk_: h.tensor_tensor(out=wts[:, :, kk], in0=wk_, in1=okk, op=ALU.mult), r=[wkey, 'okk'], w=[f'wts{kk}'])
        xsc = [AR.bf16(2048) for _ in range(2)]
        for j in range(NT):
            xb = xsc[j % 2]
            xbk = f'xsc{j % 2}'
            sdma(xb, x1b_d[j * 128:(j + 1) * 128, :], w=[xbk])
            for kk in range(2):
                P.dma('pool', lambda h, kk=kk, j=j, xb=xb: h.indirect_dma_start(
                    out=xg_d, out_offset=bass.IndirectOffsetOnAxis(ap=dests[:, j, kk:kk + 1], axis=0), in_=xb, in_offset=None),
                    r=[xbk, f'dests{kk}'] + (zkeys if j == 0 else []), w=[uid('xg'), xbk])
        P.barrier()

    def phase_d(l):
        AR.reset()
        NSC = CAP // 128
        xg = [AR.bf16(NSC * 2048).rearrange("p (a b) -> p a b", b=2048) for _ in range(2)]
        xgT = AR.bf16(16 * CAP).rearrange("p (a b) -> p a b", b=CAP)
        hT = AR.bf16(4 * CAP).rearrange("p (a b) -> p a b", b=CAP)
        sa = [AR.f32(CAP) for _ in range(2)]
        ybs = [AR.bf16(2048) for _ in range(4)]
        ycnt = 0

        def wviews(e):
            big = bigA if e % 2 == 0 else bigB
            return (big[:, 0:4, :].rearrange("p a (b c) -> p (a b) c", c=512),
                    big[:, 4:8, :].rearrange("p a (b c) -> p (a b) c", c=512),
                    big[:, 8:12, :])

        def prefetch(e):
            w1v, w3v, w2v = wviews(e)
            wk = f'wset{e % 2}'
            pdma(w1v, din['moe_w1'][l, e].rearrange("(kc p) f -> p kc f", p=128), w=[wk + 'a'])
            pdma(w3v, din['moe_w3'][l, e].rearrange("(kc p) f -> p kc f", p=128), w=[wk + 'b'])
            pdma(w2v, din['moe_w2'][l, e].rearrange("(fc p) d -> p fc d", p=128), w=[wk + 'c'])
            sdma(xg[e % 2], xg_d[e * CAP:(e + 1) * CAP, :].rearrange("(sc p) d -> p sc d", p=128), w=[f'xg{e % 2}'])
        prefetch(0)
        for e in range(NE):
            if e + 1 < NE:
                prefetch(e + 1)
            wk = f'wset{e % 2}'
            w1v, w3v, w2v = wviews(e)
            xge = xg[e % 2]
            xgk = f'xg{e % 2}'
            ec = 0
            for sc in range(NSC):
                for g4 in range(4):
                    b = nb()
                    for q in range(4):
                        kc = g4 * 4 + q
                        mm(ps[b][:, q * 128:(q + 1) * 128], xge[:, sc, kc * 128:(kc + 1) * 128], ident[:], True, True, r=[xgk, 'ident'], w=[f'ps{b}'])
                    src = ps[b][:, :].rearrange("p (a b) -> p a b", b=128)
                    dstv = xgT[:, 4 * g4:4 * g4 + 4, sc * 128:(sc + 1) * 128]
                    if ec % 2 == 0:
                        P.act(lambda h, src=src, dstv=dstv: h.copy(out=dstv, in_=src), r=[f'ps{b}'], w=[uid('xgT')])
                    else:
                        P.dve(lambda h, src=src, dstv=dstv: h.tensor_copy(out=dstv, in_=src), r=[f'ps{b}'], w=[uid('xgT')])
                    ec += 1
            P.dve(lambda h: h.tensor_copy(out=sa[0][:, 0:1], in_=sa[0][:, 0:1]), r=[f'xgT#{k.cnt - i_}' for i_ in range(NSC * 4)], w=['xgT'])
            for fc in range(4):
                ba = nb()
                for kc in range(16):
                    mm(ps[ba][:, 0:CAP], w1v[:, kc, fc * 128:(fc + 1) * 128], xgT[:, kc, :], kc == 0, kc == 15, r=[wk + 'a', 'xgT'], w=[f'ps{ba}'])
                bb = nb()
                for kc in range(16):
                    mm(ps[bb][:, 0:CAP], w3v[:, kc, fc * 128:(fc + 1) * 128], xgT[:, kc, :], kc == 0, kc == 15, r=[wk + 'b', 'xgT'], w=[f'ps{bb}'])
                s_ = sa[fc % 2]
                P.act(lambda h, ba=ba, s_=s_: h.activation(out=s_, in_=ps[ba][:, 0:CAP], func=AF.Silu), r=[f'ps{ba}'], w=[f'sa{fc % 2}'])
                P.dve(lambda h, bb=bb, s_=s_, fc=fc: h.tensor_tensor(out=hT[:, fc, :], in0=s_, in1=ps[bb][:, 0:CAP], op=ALU.mult),
                      r=[f'sa{fc % 2}', f'ps{bb}'], w=[f'hT{fc}'])
            for sc in range(NSC):
                yb_ = ybs[ycnt % 4]
                ybk = f'ybs{ycnt % 4}'
                ycnt += 1
                for db in range(4):
                    b = nb()
                    for fc in range(4):
                        mm(ps[b][:, :], hT[:, fc, sc * 128:(sc + 1) * 128], w2v[:, fc, db * 512:(db + 1) * 512], fc == 0, fc == 3,
                           r=[f'hT{fc}', wk + 'c'], w=[f'ps{b}'])
                    if db % 2 == 0:
                        P.act(lambda h, b=b, db=db, yb_=yb_: h.copy(out=yb_[:, db * 512:(db + 1) * 512], in_=ps[b][:, :]), r=[f'ps{b}'], w=[f'{ybk}_{db}'])
                    else:
                        P.dve(lambda h, b=b, db=db, yb_=yb_: h.tensor_copy(out=yb_[:, db * 512:(db + 1) * 512], in_=ps[b][:, :]), r=[f'ps{b}'], w=[f'{ybk}_{db}'])
                sdma(yb_d[e * CAP + sc * 128:e * CAP + (sc + 1) * 128, :], yb_, r=[f'{ybk}_{d_}' for d_ in range(4)], w=[uid('ybd')] + [f'{ybk}_{d_}' for d_ in range(4)])
        P.barrier()

    def phase_e(l, xin_d, xout_d, make_xT):
        AR.reset()
        lnp = AR.f32(4096).rearrange("p (a b) -> p a b", b=2048)
        sdma(lnp, din[f'lnffn{l}'], w=['lnp'])
        r0 = [AR.f32(2048) for _ in range(2)]
        r1 = [AR.f32(2048) for _ in range(2)]
        g0 = [AR.bf16(2048) for _ in range(2)]
        g1 = [r1[i].bitcast(BF16)[:, 2048:4096] for i in range(2)]
        xs = [AR.f32(2048) for _ in range(2)]
        st = AR.f32(8)
        outs = []
        def loads(j):
            a0, a1, x_s = r0[j % 2], r1[j % 2], xs[j % 2]
            k0, k1, xk = f'r0{j % 2}', f'r1{j % 2}', f'xs{j % 2}'
            P.dma('pool', lambda h, j=j: h.indirect_dma_start(
                out=g0[j % 2], out_offset=None, in_=yb_d, in_offset=bass.IndirectOffsetOnAxis(ap=dests[:, j, 0:1], axis=0)), w=[f'g0{j % 2}'])
            P.dma('pool', lambda h, j=j: h.indirect_dma_start(
                out=g1[j % 2], out_offset=None, in_=yb_d, in_offset=bass.IndirectOffsetOnAxis(ap=dests[:, j, 1:2], axis=0)), w=[k1])
            sdma(x_s, xin_d[j * 128:(j + 1) * 128, :], w=[xk])
        def tile_gen(j):
            a0, a1, x_s = r0[j % 2], r1[j % 2], xs[j % 2]
            k0, k1, xk = f'r0{j % 2}', f'r1{j % 2}', f'xs{j % 2}'
            sx = f'_{j % 2}'
            stj = st2[j % 2]
            junk_j = a1.bitcast(BF16)[:, 0:2048]
            x2b_j = x_s.bitcast(BF16)[:, 0:2048]
            P.dve(lambda h: h.tensor_scalar(out=a0, in0=g0[j % 2], scalar1=wts[:, j, 0:1], scalar2=None, op0=ALU.mult), r=[f'g0{j % 2}'], w=[k0])
            yield
            P.dve(lambda h: h.scalar_tensor_tensor(out=a0, in0=g1[j % 2], scalar=wts[:, j, 1:2], in1=a0, op0=ALU.mult, op1=ALU.add), r=[k0, k1], w=[k0])
            yield
            P.dve(lambda h: h.scalar_tensor_tensor(out=a0, in0=x_s, scalar=ALPHA, in1=a0, op0=ALU.mult, op1=ALU.add), r=[k0, xk], w=[k0])
            yield
            yield from layer_norm_gen(a0, k0, a0, k0, lnp, 'lnp', junk_j, stj, jk=k1, sx=sx)
            ok_ = uid('xout')
            sdma(xout_d[j * 128:(j + 1) * 128, :], a0, r=[k0], w=[ok_, k0])
            outs.append(ok_)
            yield
            if make_xT:
                P.act(lambda h: h.copy(out=x2b_j, in_=a0), r=[k0], w=[xk])
                yield
                for g4 in range(4):
                    b = nb()
                    for q in range(4):
                        kc = g4 * 4 + q
                        mm(ps[b][:, q * 128:(q + 1) * 128], x2b_j[:, kc * 128:(kc + 1) * 128], ident[:], True, True, r=[xk, 'ident'], w=[f'ps{b}'])
                    if g4 % 2 == 0:
                        P.act(lambda h, b=b, g4=g4: h.copy(out=bigA[:, 4 * g4:4 * g4 + 4, j * 128:(j + 1) * 128],
                                                           in_=ps[b][:, :].rearrange("p (a b) -> p a b", b=128)), r=[f'ps{b}'], w=['xT'])
                    else:
                        P.dve(lambda h, b=b, g4=g4: h.tensor_copy(out=bigA[:, 4 * g4:4 * g4 + 4, j * 128:(j + 1) * 128],
                                                                  in_=ps[b][:, :].rearrange("p (a b) -> p a b", b=128)), r=[f'ps{b}'], w=['xT'])
                    yield
        st2 = [st, AR.f32(8)]
        if make_xT:
            loads(0)
            loads(1)
            for j in range(0, NT, 2):
                zip_run([tile_gen(j), tile_gen(j + 1)])
                if j + 2 < NT:
                    loads(j + 2)
                    loads(j + 3)
        else:
            loads(0)
            for j in range(NT):
                if j + 1 < NT:
                    loads(j + 1)
                zip_run([tile_gen(j)])
        P.barrier()
        return outs

    def phase_a1():
        AR.reset()
        xT, yT = bigA, bigB
        wblk = [AR.bf16(2048).rearrange("p (a b) -> p a b", b=128) for _ in range(2)]
        wcnt = [0]

        def next_w(blk):
            i = wcnt[0] % 2
            wcnt[0] += 1
            load_wblk(wblk[i], din['w_in1'][blk], f'wblk{i}')
            return wblk[i], f'wblk{i}'
        mark = AR.off
        gu = bigB[:, 8:16, :].rearrange("p a (b c) -> p (a b) c", c=1024)
        gv = AR.bf16(16 * 1024).rearrange("p (a b) -> p a b", b=1024)
        glnp = AR.f32(2048).rearrange("p (a b) -> p a b", b=1024)
        wsT = AR.f32(1024).rearrange("p (a b) -> p a b", b=128)
        wsTm = AR.bf16(1024).rearrange("p (a b) -> p a b", b=128)
        bsT = AR.f32(8)
        s1 = AR.f32(128).rearrange("p (a b) -> p a b", b=8)
        s2 = AR.f32(128).rearrange("p (a b) -> p a b", b=8)
        mean, msq, var, rstd = AR.f32(16), AR.f32(16), AR.f32(16), AR.f32(16)
        vn = AR.f32(1024)
        vnb = AR.bf16(1024)
        tmp = AR.f32(1024)
        ycb = AR.bf16(1024)
        tmpf = [AR.f32(128) for _ in range(2)]
        junkb = AR.bf16(128)
        sdma(glnp, din['glnp'], w=['glnp'])
        sdma(wsT, din['wsT'], w=['wsT'])
        sdma(bsT, din['bsT'], w=['bsT'])
        P.dve(lambda h: h.tensor_tensor(out=wsTm, in0=wsT, in1=tril_st[:].unsqueeze(1).to_broadcast([128, 8, 128]), op=ALU.mult),
              r=['wsT', 'tril_st'], w=['wsTm'])
        for ub in range(8):
            wb, wk = next_w(ub)
            proj_tm(xT, 'xT', wb, wk, 128, lambda g, b, ub=ub: P.act(
                lambda h: h.activation(out=gu[:, 4 * g:4 * g + 4, ub * 128:(ub + 1) * 128],
                                       in_=ps[b][:, :].rearrange("p (j c) -> p j c", c=128), func=AF.Gelu), r=[f'ps{b}'], w=['gu']))
        tcnt = [0]
        for vb in range(8):
            wb, wk = next_w(8 + vb)

            def ev_v(g, b, vb=vb):
                for jj in range(4):
                    j = 4 * g + jj
                    tf = tmpf[tcnt[0] % 2]
                    tk = f'tmpf{tcnt[0] % 2}'
                    tcnt[0] += 1
                    P.act(lambda h, jj=jj, j=j, tf=tf: h.activation(out=tf, in_=ps[b][:, jj * 128:(jj + 1) * 128], func=AF.Gelu,
                                                                    accum_out=s1[:, j, vb:vb + 1]), r=[f'ps{b}'], w=[tk, uid('s1')])
                    P.act(lambda h, j=j, tf=tf: h.activation(out=junkb, in_=tf, func=AF.Square, accum_out=s2[:, j, vb:vb + 1]),
                          r=[tk], w=['junkb', uid('s2')])
                    P.dve(lambda h, j=j, tf=tf: h.tensor_copy(out=gv[:, j, vb * 128:(vb + 1) * 128], in_=tf), r=[tk], w=['gv'])
            proj_tm(xT, 'xT', wb, wk, 128, ev_v)
        P.barrier()
        P.dve(lambda h: h.tensor_reduce(out=mean, in_=s1, axis=AX.X, op=ALU.add), w=['mean'])
        P.dve(lambda h: h.tensor_reduce(out=var, in_=s2, axis=AX.X, op=ALU.add), w=['var'])
        P.dve(lambda h: h.tensor_scalar(out=mean, in0=mean, scalar1=1.0 / 1024, scalar2=None, op0=ALU.mult), r=['mean'], w=['mean'])
        P.dve(lambda h: h.tensor_tensor(out=msq, in0=mean, in1=mean, op=ALU.mult), r=['mean'], w=['msq'])
        P.dve(lambda h: h.scalar_tensor_tensor(out=var, in0=var, scalar=1.0 / 1024, in1=msq, op0=ALU.mult, op1=ALU.subtract), r=['var', 'msq'], w=['var'])
        P.act(lambda h: h.activation(out=rstd, in_=var, func=AF.Sqrt, bias=LN_EPS, scale=1.0), r=['var'], w=['rstd'])
        P.dve(lambda h: h.reciprocal(out=rstd, in_=rstd), r=['rstd'], w=['rstd'])
        for j in range(NT):
            P.dve(lambda h, j=j: h.tensor_scalar(out=vn, in0=gv[:, j, :], scalar1=mean[:, j:j + 1], scalar2=rstd[:, j:j + 1],
                                                op0=ALU.subtract, op1=ALU.mult), r=['gv', 'mean', 'rstd'], w=['vn'])
            P.dve(lambda h: h.tensor_tensor(out=vn, in0=vn, in1=glnp[:, 0, :], op=ALU.mult), r=['vn', 'glnp'], w=['vn'])
            P.pool(lambda h: h.tensor_tensor(out=vnb, in0=vn, in1=glnp[:, 1, :], op=ALU.add), r=['vn', 'glnp'], w=['vnb'])
            for half in range(2):
                b = nb()
                for q in range(4):
                    g = half * 4 + q
                    mm(ps[b][:, q * 128:(q + 1) * 128], wsTm[:, g, :], vnb[:, g * 128:(g + 1) * 128], True, True, r=['wsTm', 'vnb'], w=[f'ps{b}'])
                P.dve(lambda h, b=b, half=half: h.tensor_tensor(
                    out=tmp[:, half * 512:(half + 1) * 512].rearrange("p (g c) -> p g c", c=128),
                    in0=ps[b][:, :].rearrange("p (g c) -> p g c", c=128),
                    in1=bsT[:, half * 4:half * 4 + 4].unsqueeze(2).to_broadcast([128, 4, 128]), op=ALU.add), r=[f'ps{b}', 'bsT'], w=[f'tmp{half}'])
                P.pool(lambda h, half=half, j=j: h.tensor_tensor(out=ycb[:, half * 512:(half + 1) * 512], in0=tmp[:, half * 512:(half + 1) * 512],
                                                                in1=gu[:, j, half * 512:(half + 1) * 512], op=ALU.mult), r=[f'tmp{half}', 'gu'], w=[f'ycb{half}'])
            for half in range(2):
                b = nb()
                for q in range(4):
                    g = half * 4 + q
                    mm(ps[b][:, q * 128:(q + 1) * 128], ycb[:, g * 128:(g + 1) * 128], ident[:], True, True, r=[f'ycb{half}', 'ident'], w=[f'ps{b}'])
                if half == 0:
                    P.act(lambda h, b=b, half=half, j=j: h.copy(out=yT[:, 4 * half:4 * half + 4, j * 128:(j + 1) * 128],
                                                                in_=ps[b][:, :].rearrange("p (a b) -> p a b", b=128)), r=[f'ps{b}'], w=['yTc'])
                else:
                    P.dve(lambda h, b=b, half=half, j=j: h.tensor_copy(out=yT[:, 4 * half:4 * half + 4, j * 128:(j + 1) * 128],
                                                                       in_=ps[b][:, :].rearrange("p (a b) -> p a b", b=128)), r=[f'ps{b}'], w=['yTc'])
        P.barrier()
        P.tag = 'phase_a1_conv'
        AR.off = mark
        cw = AR.f32(8 * 31).rearrange("p (a b) -> p a b", b=31)
        cpar = AR.f32(24).rearrange("p (a b) -> p a b", b=8)
        sgf = AR.f32(2048)
        hp = [AR.bf16(30 + 2048) for _ in range(2)]
        Dg = [AR.bf16(31 * 128).rearrange("p (a b) -> p a b", b=128) for _ in range(2)]
        meanT, msqT, varT, rstdT = AR.f32(512), AR.f32(512), AR.f32(512), AR.f32(512)
        zb = [AR.f32(512) for _ in range(2)]
        sqb = [AR.bf16(512) for _ in range(2)]
        sdma(cw, din['cw'], w=['cw'])
        sdma(cpar, din['cpar'], w=['cpar'])
        for i in range(2):
            P.pool(lambda h, i=i: h.memset(hp[i][:, 0:30], 0.0), w=[f'hp{i}'])
        for cc in range(8):
            hpc, hk = hp[cc % 2], f'hp{cc % 2}'
            dgc, dk = Dg[cc % 2], f'Dg{cc % 2}'
            wb, wk = next_w(24 + cc)
            proj_fm(xT, 'xT', wb, wk, lambda tt, b: P.act(
                lambda h: h.activation(out=sgf[:, tt * 512:(tt + 1) * 512], in_=ps[b][:, :], func=AF.Sigmoid), r=[f'ps{b}'], w=['sgf']))
            wb, wk = next_w(16 + cc)
            proj_fm(xT, 'xT', wb, wk, lambda tt, b, hpc=hpc, hk=hk: P.dve(
                lambda h: h.tensor_tensor(out=hpc[:, 30 + tt * 512:30 + (tt + 1) * 512], in0=ps[b][:, :], in1=sgf[:, tt * 512:(tt + 1) * 512], op=ALU.mult),
                r=[f'ps{b}', 'sgf'], w=[hk]))
            P.pool(lambda h, dgc=dgc, cc=cc: h.tensor_tensor(out=dgc, in0=ident[:].unsqueeze(1).to_broadcast([128, 31, 128]),
                                                            in1=cw[:, cc, :].unsqueeze(2).to_broadcast([128, 31, 128]), op=ALU.mult),
                   r=['ident', 'cw'], w=[dk])
            for tt in range(4):
                b = nb()
                for jt in range(31):
                    mm(ps[b][:, :], dgc[:, jt, :], hpc[:, tt * 512 + jt:tt * 512 + jt + 512], jt == 0, jt == 30, r=[dk, hk], w=[f'ps{b}'])
                P.act(lambda h, b=b, cc=cc, tt=tt: h.activation(out=yT[:, 8 + cc, tt * 512:(tt + 1) * 512], in_=ps[b][:, :], func=AF.Identity,
                                                                bias=cpar[:, 0, cc:cc + 1], scale=1.0), r=[f'ps{b}', 'cpar'], w=[f'yd{cc}'])
        scnt = 0
        for tt in range(4):
            ts = slice(tt * 512, (tt + 1) * 512)
            b1 = nb()
            for cc in range(8):
                mm(ps[b1][:, :], ones_bf[:], yT[:, 8 + cc, ts], cc == 0, cc == 7, r=['ones_bf', f'yd{cc}'], w=[f'ps{b1}'])
            b2 = nb()
            for cc in range(8):
                sq_, sk_ = sqb[scnt % 2], f'sqb{scnt % 2}'
                scnt += 1
                P.pool(lambda h, sq_=sq_, cc=cc, ts=ts: h.tensor_tensor(out=sq_, in0=yT[:, 8 + cc, ts], in1=yT[:, 8 + cc, ts], op=ALU.mult),
                       r=[f'yd{cc}'], w=[sk_])
                mm(ps[b2][:, :], ones_bf[:], sq_, cc == 0, cc == 7, r=['ones_bf', sk_], w=[f'ps{b2}'])
            P.act(lambda h, b1=b1: h.mul(out=meanT, in_=ps[b1][:, :], mul=1.0 / 1024), r=[f'ps{b1}'], w=['meanT'])
            P.dve(lambda h: h.tensor_tensor(out=msqT, in0=meanT, in1=meanT, op=ALU.mult), r=['meanT'], w=['msqT'])
            P.dve(lambda h, b2=b2: h.scalar_tensor_tensor(out=varT, in0=ps[b2][:, :], scalar=1.0 / 1024, in1=msqT, op0=ALU.mult, op1=ALU.subtract),
                  r=[f'ps{b2}', 'msqT'], w=['varT'])
            P.act(lambda h: h.activation(out=rstdT, in_=varT, func=AF.Sqrt, bias=LN_EPS, scale=1.0), r=['varT'], w=['rstdT'])
            P.dve(lambda h: h.reciprocal(out=rstdT, in_=rstdT), r=['rstdT'], w=['rstdT'])
            for cc in range(8):
                z_, zk = zb[cc % 2], f'zb{cc % 2}'
                P.dve(lambda h, z_=z_, cc=cc, ts=ts: h.tensor_tensor(out=z_, in0=yT[:, 8 + cc, ts], in1=meanT, op=ALU.subtract),
                      r=[f'yd{cc}', 'meanT'], w=[zk])
                P.dve(lambda h, z_=z_: h.tensor_tensor(out=z_, in0=z_, in1=rstdT, op=ALU.mult), r=[zk, 'rstdT'], w=[zk])
                P.act(lambda h, z_=z_, cc=cc, ts=ts: h.activation(out=yT[:, 8 + cc, ts], in_=z_, func=AF.Silu,
                                                                  bias=cpar[:, 2, cc:cc + 1], scale=cpar[:, 1, cc:cc + 1]),
                      r=[zk, 'cpar'], w=[f'yd{cc}'])
        P.dve(lambda h: h.tensor_copy(out=meanT[:, 0:1], in_=meanT[:, 0:1]), r=['yTc'] + [f'yd{cc}' for cc in range(8)], w=['yT'])
        P.barrier()

    def dump_dram(src_d):
        AR.reset()
        st = [AR.f32(2048) for _ in range(2)]
        keys = []
        for j in range(NT):
            sdma(st[j % 2], src_d[j * 128:(j + 1) * 128, :], w=[f'dst{j % 2}'])
            kk = uid('dbg')
            sdma(dbg_d[j * 128:(j + 1) * 128, :], st[j % 2], r=[f'dst{j % 2}'], w=[kk])
            keys.append(kk)
        P.finish(keys)
        P.emit()
        return nc

    try:
        P.tag = 'phase_a0'
        phase_a0()
    except _Cut:
        pass
    if debug and stage == 0:
        AR.reset()
        st = AR.f32(2048)
        for fc in range(16):
            P.dve(lambda h, fc=fc: h.tensor_copy(out=st, in_=bigB[:, fc, :]), r=['yT'], w=['st'])
            sdma(dbg_d[fc * 128:(fc + 1) * 128, :], st, r=['st'], w=[f'dbg{fc}'])
        P.finish([f'dbg{fc}' for fc in range(16)])
        P.emit()
        return nc
    P.tag = 'phase_c_0'
    phase_c(0, din['x_tm'], x1_d)
    if debug and stage == 1:
        return dump_dram(x1_d)
    P.tag = 'phase_d_0'
    phase_d(0)
    P.tag = 'phase_e_0'
    phase_e(0, x1_d, x2_d, True)
    if debug and stage == 2:
        return dump_dram(x2_d)
    P.tag = 'phase_a1'
    phase_a1()
    if debug and stage == 3:
        AR.reset()
        st = AR.f32(2048)
        for fc in range(16):
            P.dve(lambda h, fc=fc: h.tensor_copy(out=st, in_=bigB[:, fc, :]), r=['yT'], w=['st'])
            sdma(dbg_d[fc * 128:(fc + 1) * 128, :], st, r=['st'], w=[f'dbg{fc}'])
        P.finish([f'dbg{fc}' for fc in range(16)])
        P.emit()
        return nc
    P.tag = 'phase_c_1'
    phase_c(1, x2_d, x3_d)
    P.tag = 'phase_d_1'
    phase_d(1)
    P.tag = 'phase_e_1'
    outs = phase_e(1, x3_d, out_d, False)
    P.finish(outs)
    P.emit()
    k.nops = len(P.ops)
    return nc


def kernel(**inputs):
    inp = {k: np.asarray(v) for k, v in inputs.items()}
    sh = prep_shared(inp)
    x = inp['x']
    in_maps = []
    for b in range(8):
        m = dict(sh)
        m['x_tm'] = np.ascontiguousarray(x[b])
        m['xT'] = np.ascontiguousarray(x[b].T)
        in_maps.append(m)
    nc = build()
    res = run_bass_kernel_spmd(nc, in_maps, core_ids=list(range(8)))
    return np.stack([np.asarray(r['out']) for r in res.results], axis=0).astype(np.float32)
```

```python
import numpy as np
from contextlib import ExitStack
import concourse.bass as bass
import concourse.mybir as mybir

F32 = mybir.dt.float32
BF16 = mybir.dt.bfloat16
I32 = mybir.dt.int32
U32 = mybir.dt.uint32
AF = mybir.ActivationFunctionType
ALU = mybir.AluOpType
AX = mybir.AxisListType

SEM_ROT = 20000
DMA_K = 6


class Prog:
    ENGS = ['pe', 'act', 'dve', 'pool', 'sp']

    def __init__(self, nc):
        self.nc = nc
        self.ops = []
        self.es = ExitStack()
        self.last_w = {}
        self.readers = {}
        self.ndma = {e: 0 for e in self.ENGS}
        self.dma_ops = {e: [] for e in self.ENGS}

    def sb(self, name, shape, dt):
        return self.es.enter_context(self.nc.sbuf_tensor("sb_" + name, list(shape), dt))

    def ps(self, name, shape, dt=F32):
        return self.es.enter_context(self.nc.psum_tensor("pp_" + name, list(shape), dt))

    def add(self, eng, fn, r=(), w=(), dma=False):
        i = len(self.ops)
        deps = set()
        psk = [k for k in list(r) + list(w) if isinstance(k, str) and k.startswith('ps')]
        r = [k for k in r if k not in psk] + ['phase']
        w = [k for k in w if k not in psk]
        for k in psk:
            lw = self.last_w.get(k)
            if lw is not None and self.ops[lw]['eng'] != eng:
                deps.add(lw)
        for k in r:
            lw = self.last_w.get(k)
            if lw is not None:
                deps.add(lw)
        for k in w:
            lw = self.last_w.get(k)
            if lw is not None:
                deps.add(lw)
            for rd in self.readers.get(k, ()):
                deps.add(rd)
        deps.discard(i)
        op = dict(i=i, eng=eng, fn=fn, deps=deps, dma=dma, sig=False, tag=getattr(self, 'tag', 'x'))
        if dma:
            j = self.ndma[eng]
            self.ndma[eng] += 1
            op['dj'] = j
            self.dma_ops[eng].append(i)
            if j >= DMA_K:
                deps.add(self.dma_ops[eng][j - DMA_K])
        self.ops.append(op)
        for k in psk:
            self.last_w[k] = i
        for k in r:
            self.readers.setdefault(k, []).append(i)
        for k in w:
            self.last_w[k] = i
            self.readers[k] = []
        return i

    def pe(self, fn, r=(), w=()): return self.add('pe', fn, r, w)
    def act(self, fn, r=(), w=()): return self.add('act', fn, r, w)
    def dve(self, fn, r=(), w=()): return self.add('dve', fn, r, w)
    def pool(self, fn, r=(), w=()): return self.add('pool', fn, r, w)
    def dma(self, eng, fn, r=(), w=()): return self.add(eng, fn, r, w, dma=True)

    def emit(self):
        nc = self.nc
        ops = self.ops
        for op in ops:
            if op['eng'] == 'pe' and not op['dma']:
                op['deps'] = {d for d in op['deps'] if not (ops[d]['eng'] == 'pe' and not ops[d]['dma'])}
        if getattr(self, 'prune_same_engine', False):
            for op in ops:
                if not op['dma']:
                    op['deps'] = {d for d in op['deps'] if not (ops[d]['eng'] == op['eng'] and not ops[d]['dma'])}
        cnt = {e: 0 for e in self.ENGS}
        needed = set()
        for op in ops:
            for d in op['deps']:
                needed.add(d)
        for op in ops:
            if op['dma']:
                continue
            if op['i'] in needed:
                cnt[op['eng']] += 1
                op['sidx'] = cnt[op['eng']]
        nsem_eng = {e: (cnt[e] + SEM_ROT - 1) // SEM_ROT for e in self.ENGS}
        sems = {}
        for e in self.ENGS:
            sems[e] = [self.es.enter_context(nc.semaphore(f"s_{e}_{k}")) for k in range(max(1, nsem_eng[e]))]
        dsems = {}
        for e in self.ENGS:
            if self.ndma[e]:
                dsems[e] = [self.es.enter_context(nc.semaphore(f"d_{e}_{k}")) for k in range(DMA_K)]
        know = {e: {x: 0 for x in self.ENGS} for e in self.ENGS}
        know_dma = {e: set() for e in self.ENGS}
        snap_sig = {}
        snap_dma = {}
        nwaits = 0

        def inherit(e, sn):
            ks, kd = sn
            for x in self.ENGS:
                if ks[x] > know[e][x]:
                    know[e][x] = ks[x]
            know_dma[e] |= kd

        for op in ops:
            e = op['eng']
            waits = []
            need_eng = {}
            need_dma = []
            for d in sorted(op['deps']):
                dop = ops[d]
                if dop['dma']:
                    need_dma.append((dop['eng'], dop['dj']))
                else:
                    need_eng[dop['eng']] = max(need_eng.get(dop['eng'], 0), dop['sidx'])
            for x, s in sorted(need_eng.items(), key=lambda t: -t[1]):
                if know[e][x] >= s:
                    continue
                waits.append(('eng', x, s))
                know[e][x] = s
                inherit(e, snap_sig[(x, s)])
            for key in need_dma:
                if key in know_dma[e]:
                    continue
                waits.append(('dma', key[0], key[1]))
                know_dma[e].add(key)
                inherit(e, snap_dma[key])
            op['waits'] = waits
            nwaits += len(waits)
            if op['dma']:
                snap_dma[(e, op['dj'])] = (dict(know[e]), set(know_dma[e]))
            elif 'sidx' in op:
                snap_sig[(e, op['sidx'])] = (dict(know[e]), set(know_dma[e]))
        self.nwaits = nwaits

        def sem_of(x, s):
            return sems[x][(s - 1) // SEM_ROT], (s - 1) % SEM_ROT + 1

        def dsem_of(x, j):
            return dsems[x][j % DMA_K], 16 * (j // DMA_K + 1)

        streams = {e: [op for op in ops if op['eng'] == e] for e in self.ENGS}

        scoped = getattr(self, 'use_scopes', False)

        def run_stream(e, h):
            import itertools
            for tag, grp in itertools.groupby(streams[e], key=lambda o: o.get('tag')):
                if scoped:
                    with nc.named_scope(str(tag)):
                        for op in grp:
                            self._emit_one(e, h, op, sem_of, dsem_of)
                else:
                    for op in grp:
                        self._emit_one(e, h, op, sem_of, dsem_of)

        def _unused(e, h):
            for op in streams[e]:
                for wt in op['waits']:
                    if wt[0] == 'eng':
                        s, v = sem_of(wt[1], wt[2])
                    else:
                        s, v = dsem_of(wt[1], wt[2])
                    h.wait_ge(s, v)
                ins = op['fn'](h)
                if op['dma']:
                    s, v = dsem_of(e, op['dj'])
                    ins.then_inc(s, 16)
                elif 'sidx' in op:
                    s, v = sem_of(e, op['sidx'])
                    ins.then_inc(s, 1)

        with nc.Block() as block:
            if streams['sp']:
                @block.sync
                def _(h):
                    run_stream('sp', h)
            if streams['pe']:
                @block.tensor
                def _(h):
                    run_stream('pe', h)
            if streams['act']:
                @block.scalar
                def _(h):
                    run_stream('act', h)
            if streams['dve']:
                @block.vector
                def _(h):
                    run_stream('dve', h)
            if streams['pool']:
                @block.gpsimd
                def _(h):
                    run_stream('pool', h)

    def _emit_one(self, e, h, op, sem_of, dsem_of):
        for wt in op['waits']:
            if wt[0] == 'eng':
                s, v = sem_of(wt[1], wt[2])
            else:
                s, v = dsem_of(wt[1], wt[2])
            h.wait_ge(s, v)
        ins = op['fn'](h)
        if op['dma']:
            s, v = dsem_of(e, op['dj'])
            ins.then_inc(s, 16)
        elif 'sidx' in op:
            s, v = sem_of(e, op['sidx'])
            ins.then_inc(s, 1)

    def barrier(self):
        self.add('sp', lambda h: h.nop(), r=(), w=['phase'])

    def finish(self, final_deps_keys):
        deps = [self.last_w[k] for k in final_deps_keys]
        i = len(self.ops)
        op = dict(i=i, eng='sp', fn=lambda h: h.nop(), deps=set(deps), dma=False, sig=False)
        self.ops.append(op)


class Arena:
    def __init__(self, P, nbytes):
        self.t = P.sb('arena', [128, nbytes // 4], F32)
        self.n = nbytes // 4
        self.off = 0

    def reset(self):
        self.off = 0

    def f32(self, n):
        assert self.off + n <= self.n, ("arena overflow", self.off, n, self.n)
        ap = self.t[:, self.off:self.off + n]
        self.off += n
        return ap

    def bf16(self, n):
        w = (n + 1) // 2
        return self.f32(w).bitcast(BF16)[:, 0:n]

    def i32(self, n):
        return self.f32(n).bitcast(I32)

import math
from concourse.bass_utils import run_bass_kernel_spmd

S = 2048
D = 2048
NT = 16
NE = 32
CAP = 384
NSLOT = NE * CAP
ALPHA = float((2 * 2) ** 0.25)
LN_EPS = 1e-5
RMS_EPS = 1e-6
NEG = -30000.0


def _t5_bucket(dist):
    max_exact = 16
    d = np.maximum(dist, 1).astype(np.float32)
    large = max_exact + (np.log(d / np.float32(max_exact)) / np.float32(math.log(128 / max_exact))
                         * np.float32(32 - max_exact)).astype(np.int32)
    large = np.minimum(large, 31)
    return np.where(dist < max_exact, dist, large)


def _bc(v, n=128):
    return np.ascontiguousarray(np.broadcast_to(np.asarray(v, np.float32)[None], (n,) + tuple(np.shape(v))))


def prep_shared(inp):
    f32 = np.float32
    sh = {}
    w = np.asarray(inp['w_in_ab'][0])
    cols = []
    for c in range(8):
        cols += list(range(c * 64, c * 64 + 64)) + list(range((8 + c) * 64, (8 + c) * 64 + 64))
    cols += list(range(1024, 1152)) + list(range(1152, 1280))
    for h in range(8):
        for part in range(4):
            b0 = 1280 + part * 1024 + h * 128
            cols += list(range(b0, b0 + 128))
    wp = w[:, cols]
    sh['w_in0'] = np.ascontiguousarray(wp.reshape(16, 128, 42, 128).transpose(2, 1, 0, 3))
    rows = []
    for c in range(8):
        rows += list(range(c * 64, c * 64 + 64)) + list(range((8 + c) * 64, (8 + c) * 64 + 64))
    rows += list(range(1024, 2048))
    wo = np.asarray(inp['w_out_ab'][0])[rows]
    sh['w_out0'] = np.ascontiguousarray(wo.reshape(16, 128, 2048).transpose(1, 0, 2))
    t_loc = np.arange(128, dtype=np.int32)[:, None]
    s_loc = np.arange(256, dtype=np.int32)[None, :]
    dist = t_loc + 128 - s_loc
    bucket = _t5_bucket(np.maximum(dist, 0))
    rb = np.asarray(inp['rel_bias'])
    bg = rb[bucket]
    hb = np.zeros((128, 8, 2, 256), f32)
    for c in range(8):
        hb[:, c, 0, :] = bg[:, :, c]
        hb[:, c, 1, :] = bg[:, :, 8 + c]
    sh['bias_g'] = hb
    valid = (dist >= 0) & (dist < 128)
    sh['maskneg'] = np.where(valid, 0.0, NEG).astype(f32)
    sk = np.asarray(inp['attn_sinks'][0])
    sk2 = np.stack([sk[0:8], sk[8:16]], axis=1)
    sh['sinks'] = _bc(sk2)
    lbl = np.asarray(inp['hgrn_lb_logits'])
    sh['lbl'] = np.ascontiguousarray(lbl.reshape(3, 8, 128).transpose(2, 1, 0))
    sh['normg'] = _bc(np.asarray(inp['hgrn_norm_g'][0]))
    sh['ident'] = np.eye(128, dtype=f32)
    ii = np.arange(128)
    sh['mask_bd'] = ((ii[:, None] <= ii[None, :]) & ((ii[:, None] // 64) == (ii[None, :] // 64))).astype(f32)
    sh['tri_strict'] = (ii[:, None] < ii[None, :]).astype(f32)
    sh['tril_st'] = (ii[:, None] <= ii[None, :]).astype(f32)
    sh['ecap'] = _bc(np.arange(NE, dtype=f32) * CAP)
    sh['pidx'] = (np.arange(128, dtype=f32) + NSLOT).reshape(128, 1)
    for l in range(2):
        sh[f'lnmix{l}'] = np.stack([_bc(inp['ln_mix_g'][l]), _bc(inp['ln_mix_b'][l])], axis=1)
        sh[f'lnffn{l}'] = np.stack([_bc(inp['ln_ffn_g'][l]), _bc(inp['ln_ffn_b'][l])], axis=1)
        wr = np.concatenate([np.asarray(inp['moe_w_group'][l]), np.asarray(inp['moe_w_router'][l])], axis=1)
        sh[f'wr{l}'] = np.ascontiguousarray(wr.reshape(16, 128, 36).transpose(1, 0, 2))
        sh[f'br{l}'] = _bc(np.concatenate([np.asarray(inp['moe_b_group'][l]), np.asarray(inp['moe_b_router'][l])]))
    w1 = np.asarray(inp['w_in_cd'][0])
    sh['w_in1'] = np.ascontiguousarray(w1.reshape(16, 128, 32, 128).transpose(2, 1, 0, 3))
    wo1 = np.asarray(inp['w_out_cd'][0])
    sh['w_out1'] = np.ascontiguousarray(wo1.reshape(16, 128, 2048).transpose(1, 0, 2))
    sh['wsT'] = np.ascontiguousarray(np.asarray(inp['gmlp_w_s'][0]).transpose(2, 0, 1))
    sh['bsT'] = np.ascontiguousarray(np.asarray(inp['gmlp_b_s'][0]).T)
    sh['glnp'] = np.stack([_bc(inp['gmlp_ln_g'][0]), _bc(inp['gmlp_ln_b'][0])], axis=1)
    sh['cw'] = np.ascontiguousarray(np.asarray(inp['conv_w'][0]).reshape(31, 8, 128).transpose(2, 1, 0))
    cp = np.stack([np.asarray(inp['conv_b'][0]), np.asarray(inp['conv_ln_g'][0]), np.asarray(inp['conv_ln_b'][0])], axis=0)
    sh['cpar'] = np.ascontiguousarray(cp.reshape(3, 8, 128).transpose(2, 0, 1))
    sh['moe_w1'] = np.asarray(inp['moe_w1'])
    sh['moe_w3'] = np.asarray(inp['moe_w3'])
    sh['moe_w2'] = np.asarray(inp['moe_w2'])
    return sh


IN_SPECS = dict(
    x_tm=([S, D], 'f'), xT=([D, S], 'f'),
    w_in0=([42, 128, 16, 128], 'f'), w_out0=([128, 16, 2048], 'f'),
    bias_g=([128, 8, 2, 256], 'f'), maskneg=([128, 256], 'f'), sinks=([128, 8, 2], 'f'),
    lbl=([128, 8, 3], 'f'), normg=([128, 1024], 'f'),
    ident=([128, 128], 'f'), mask_bd=([128, 128], 'f'), tri_strict=([128, 128], 'f'), tril_st=([128, 128], 'f'),
    ecap=([128, 32], 'f'), pidx=([128, 1], 'f'),
    lnmix0=([128, 2, 2048], 'f'), lnffn0=([128, 2, 2048], 'f'), lnmix1=([128, 2, 2048], 'f'), lnffn1=([128, 2, 2048], 'f'),
    wr0=([128, 16, 36], 'f'), wr1=([128, 16, 36], 'f'), br0=([128, 36], 'f'), br1=([128, 36], 'f'),
    w_in1=([32, 128, 16, 128], 'f'), w_out1=([128, 16, 2048], 'f'),
    wsT=([128, 8, 128], 'f'), bsT=([128, 8], 'f'), glnp=([128, 2, 1024], 'f'),
    cw=([128, 8, 31], 'f'), cpar=([128, 3, 8], 'f'),
    moe_w1=([2, 32, 2048, 512], 'f'), moe_w3=([2, 32, 2048, 512], 'f'), moe_w2=([2, 32, 512, 2048], 'f'),
)


class K:
    pass


class _Cut(Exception):
    pass


def build(stage=99, debug=None, cut=None, scopes=False, prune_same=False):
    nc = bass.Bass("TRN2", target_bir_lowering=False)
    P = Prog(nc)
    P.use_scopes = scopes
    P.prune_same_engine = prune_same
    P.tag = 'init'
    din = {}
    for name, (shape, _) in IN_SPECS.items():
        din[name] = nc.dram_tensor(name, list(shape), F32, kind="ExternalInput").ap()
    out_d = nc.dram_tensor("out", [S, D], F32, kind="ExternalOutput").ap()
    dbg_d = None
    if debug:
        dbg_d = nc.dram_tensor("dbg", list(debug), F32, kind="ExternalOutput").ap()
    x1_d = nc.dram_tensor("x1_scr", [S, D], F32, kind="Internal").ap()
    x2_d = nc.dram_tensor("x2_scr", [S, D], F32, kind="Internal").ap()
    x3_d = nc.dram_tensor("x3_scr", [S, D], F32, kind="Internal").ap()
    xg_d = nc.dram_tensor("xg_scr", [NSLOT + 128, D], BF16, kind="Internal").ap()
    yb_d = nc.dram_tensor("yb_scr", [NSLOT + 128, D], BF16, kind="Internal").ap()
    gu_d = nc.dram_tensor("gu_scr", [S, 1024], BF16, kind="Internal").ap()
    x1b_d = nc.dram_tensor("x1b_scr", [S, D], BF16, kind="Internal").ap()

    bigA = P.sb("bigA", [128, 16, 2048], BF16)
    bigB = P.sb("bigB", [128, 16, 2048], BF16)
    AR = Arena(P, 76 * 1024)
    ps = [P.ps(f"ps{i}", [128, 512]) for i in range(8)]
    ident = P.sb("ident", [128, 128], BF16)
    mask_bd = P.sb("mask_bd", [128, 128], BF16)
    tri_strict = P.sb("tri_strict", [128, 128], BF16)
    tril_st = P.sb("tril_st", [128, 128], F32)
    ones_bf = P.sb("ones_bf", [128, 128], BF16)
    ones_f = P.sb("ones_f", [128, 128], F32)
    ecap = P.sb("ecap", [128, 32], F32)
    pidx = P.sb("pidx", [128, 1], F32)
    dests = P.sb("dests", [128, 16, 2], I32)
    wts = P.sb("wts", [128, 16, 2], F32)
    zt = P.sb("zt", [128, 512], BF16)

    k = K()
    k.nc, k.P, k.din, k.ps, k.AR = nc, P, din, ps, AR
    k.bank = 0

    def nb():
        b = k.bank
        k.bank = (k.bank + 1) % 8
        return b
    k.nb = nb
    k.cnt = 0

    def uid(s):
        k.cnt += 1
        return f"{s}#{k.cnt}"

    pdma = lambda out, in_, r=(), w=(): P.dma('pool', lambda h: h.dma_start(out=out, in_=in_), r=r, w=w)
    sdma = lambda out, in_, r=(), w=(): P.dma('sp', lambda h: h.dma_start(out=out, in_=in_), r=r, w=w)

    def mm(out, lhsT, rhs, start, stop, r, w):
        P.pe(lambda h: h.matmul(out, lhsT=lhsT, rhs=rhs, start=start, stop=stop), r=r, w=w)

    pdma(ident[:], din['ident'], w=['ident'])
    pdma(mask_bd[:], din['mask_bd'], w=['mask_bd'])
    pdma(tri_strict[:], din['tri_strict'], w=['tri_strict'])
    sdma(tril_st[:], din['tril_st'], w=['tril_st'])
    sdma(ecap[:], din['ecap'], w=['ecap'])
    sdma(pidx[:], din['pidx'], w=['pidx'])
    P.dve(lambda h: h.memset(ones_bf[:], 1.0), w=['ones_bf'])
    P.dve(lambda h: h.memset(ones_f[:], 1.0), w=['ones_f'])
    zrow = AR.f32(2048)
    P.dve(lambda h: h.memset(zrow, 0.0), w=['zrow'])
    sdma(yb_d[NSLOT:NSLOT + 128, :], zrow.bitcast(BF16)[:, 0:2048], r=['zrow'], w=['yb_trash'])
    P.barrier()

    def load_wblk(dst, src_ap, key):
        pdma(dst, src_ap, w=[key])

    def proj_fm(xT, xkey, wb, wkey, evac):
        for tt in range(4):
            b = nb()
            for kc in range(16):
                mm(ps[b][:, :], wb[:, kc, :], xT[:, kc, tt * 512:(tt + 1) * 512], kc == 0, kc == 15,
                   r=[xkey, wkey], w=[f'ps{b}'])
            evac(tt, b)

    def proj_tm(xT, xkey, wb, wkey, ncol, evac):
        for g in range(4):
            b = nb()
            for jj in range(4):
                j = g * 4 + jj
                for kc in range(16):
                    mm(ps[b][:, jj * 128:jj * 128 + ncol], xT[:, kc, j * 128:(j + 1) * 128], wb[:, kc, 0:ncol],
                       kc == 0, kc == 15, r=[xkey, wkey], w=[f'ps{b}'])
            evac(g, b)

    def phase_a0():
        AR.reset()
        xT, yT = bigA, bigB
        if cut == 'const':
            raise _Cut()
        wblk = [AR.bf16(2048).rearrange("p (a b) -> p a b", b=128) for _ in range(2)]
        wcnt = [0]

        def next_w(blk):
            i = wcnt[0] % 2
            wcnt[0] += 1
            load_wblk(wblk[i], din['w_in0'][blk], f'wblk{i}')
            return wblk[i], f'wblk{i}'
        xsrc = din['xT'].rearrange("(kc p) t -> p kc t", p=128)
        for q in range(4):
            pdma(xT[:, 4 * q:4 * q + 4, :], xsrc[:, 4 * q:4 * q + 4, :], w=[f'xT'])
        if cut == 'xT':
            raise _Cut()
        mark = AR.off
        bm = AR.f32(8 * 512).rearrange("p (c x) -> p c x", x=512)
        kT = AR.bf16(128 + 2048)
        V0 = AR.bf16(2048).rearrange("p (a b) -> p a b", b=128)
        V1 = AR.bf16(2048).rearrange("p (a b) -> p a b", b=128)
        q0 = AR.bf16(2048)
        q1 = AR.bf16(2048)
        tb = [AR.f32(512) for _ in range(4)]
        eb = [AR.f32(512) for _ in range(4)]
        pb = [AR.bf16(512) for _ in range(4)]
        pTb = [AR.bf16(512) for _ in range(4)]
        sinks = AR.f32(16).rearrange("p (c x) -> p c x", x=2)
        mskn = AR.f32(256)
        sm = [AR.f32(16) for _ in range(4)]
        sdma(bm, din['bias_g'].rearrange("p c s k -> p c (s k)"), w=['bm'])
        sdma(mskn, din['maskneg'], w=['mskn'])
        sdma(sinks, din['sinks'], w=['sinks'])
        P.pool(lambda h: h.memset(zt[:], 0.0), w=['zt'])
        k.zkeys = []
        for q in range(NSLOT // 128 + 1):
            zk = uid('xgz')
            sdma(xg_d[q * 128:(q + 1) * 128, :].rearrange("p (a c) -> p a c", c=512), zt[:].unsqueeze(1).to_broadcast([128, 4, 512]), r=['zt'], w=[zk])
            k.zkeys.append(zk)
        P.dve(lambda h: h.tensor_tensor(out=bm.rearrange("p c (s k) -> p (c s) k", k=256),
                                        in0=bm.rearrange("p c (s k) -> p (c s) k", k=256),
                                        in1=mskn.unsqueeze(1).to_broadcast([128, 16, 256]), op=ALU.add),
              r=['bm', 'mskn'], w=['bm'])
        P.pool(lambda h: h.memset(kT[:, 0:128], 0.0), w=['kT'])
        P.pool(lambda h: h.memset(V0, 0.0), w=['V0'])
        P.pool(lambda h: h.memset(V1, 0.0), w=['V1'])
        P.pool(lambda h: h.memset(q0, 0.0), w=['q0'])
        P.pool(lambda h: h.memset(q1, 0.0), w=['q1'])
        if cut == 'setup':
            raise _Cut()
        wb, wk = next_w(8)
        proj_fm(xT, 'xT', wb, wk, lambda tt, b: P.act(
            lambda h: h.copy(out=kT[:, 128 + tt * 512:128 + (tt + 1) * 512], in_=ps[b][:, :]), r=[f'ps{b}'], w=['kT']))
        if cut == 'k':
            raise _Cut()
        wb, wk = next_w(9)

        def ev_v(g, b):
            for jj in range(4):
                j = 4 * g + jj
                P.act(lambda h, j=j, jj=jj: h.copy(out=V0[:, j, 0:64], in_=ps[b][:, jj * 128:jj * 128 + 64]), r=[f'ps{b}'], w=['V0'])
                P.dve(lambda h, j=j, jj=jj: h.tensor_copy(out=V1[:, j, 64:128], in_=ps[b][:, jj * 128 + 64:jj * 128 + 128]), r=[f'ps{b}'], w=['V1'])
        proj_tm(xT, 'xT', wb, wk, 128, ev_v)
        if cut == 'kv':
            raise _Cut()
        for c in range(8):
            if cut == 'att1' and c == 1:
                raise _Cut()
            wb, wk = next_w(c)

            def ev_q(tt, b):
                P.act(lambda h: h.copy(out=q0[0:64, tt * 512:(tt + 1) * 512], in_=ps[b][0:64, :]), r=[f'ps{b}'], w=['q0'])
                P.dve(lambda h: h.tensor_copy(out=q1[64:128, tt * 512:(tt + 1) * 512], in_=ps[b][64:128, :]), r=[f'ps{b}'], w=['q1'])
            proj_fm(xT, 'xT', wb, wk, ev_q)
            for n0 in range(0, 16, 4):
                by = (n0 // 4) % 2
                sbank = [2, 3, 4, 5]
                tbank = [6, 7, 2, 3]
                ctx = []
                for i in range(4):
                    n = n0 + i
                    t, e, p_bf, pT, s = tb[i], eb[i], pb[i], pTb[i], sm[i]
                    d = dict(n=n, t=t, e=e, p=p_bf, pT=pT, tk=f't{i}', ek=f'e{i}', pk=f'p{i}', ptk=f'pT{i}', sk=f'sm{i}',
                             mx=s[:, 0:2], nmx=s[:, 2:4], rs=s[:, 4:6], es=s[:, 6:8], sd=s[:, 8:10], rinv=s[:, 10:12],
                             t3=t.rearrange("p (s k) -> p s k", k=256), e3=e.rearrange("p (s k) -> p s k", k=256),
                             p3=p_bf.rearrange("p (s k) -> p s k", k=256), pT3=pT.rearrange("p (a b) -> p a b", b=128),
                             b=sbank[i], bT=tbank[i], halves=([1] if n == 0 else [0, 1]))
                    ctx.append(d)
                for d in ctx:
                    n, bq = d['n'], d['b']
                    qs = slice(n * 128, (n + 1) * 128)
                    mm(ps[bq][:, 0:256], q0[:, qs], kT[:, n * 128:n * 128 + 256], True, True, r=['q0', 'kT'], w=[f'ps{bq}'])
                    mm(ps[bq][:, 256:512], q1[:, qs], kT[:, n * 128:n * 128 + 256], True, True, r=['q1', 'kT'], w=[f'ps{bq}'])
                for d in ctx:
                    P.dve(lambda h, d=d, c=c: h.scalar_tensor_tensor(out=d['t'], in0=ps[d['b']][:, :], scalar=0.125, in1=bm[:, c, :],
                                                                    op0=ALU.mult, op1=ALU.add), r=[f"ps{d['b']}", 'bm'], w=[d['tk']])
                    if d['n'] == 0:
                        P.pool(lambda h, d=d: h.memset(d['t3'][:, :, 0:128], NEG), w=[d['tk']])
                for d in ctx:
                    P.dve(lambda h, d=d: h.tensor_reduce(out=d['mx'], in_=d['t3'], axis=AX.X, op=ALU.max), r=[d['tk']], w=[d['sk']])
                for d in ctx:
                    P.dve(lambda h, d=d, c=c: h.tensor_tensor(out=d['mx'], in0=d['mx'], in1=sinks[:, c, :], op=ALU.max), r=[d['sk'], 'sinks'], w=[d['sk']])
                for d in ctx:
                    P.dve(lambda h, d=d: h.tensor_scalar(out=d['nmx'], in0=d['mx'], scalar1=-1.0, scalar2=None, op0=ALU.mult), r=[d['sk']], w=[d['sk']])
                for d in ctx:
                    P.dve(lambda h, d=d, c=c: h.tensor_tensor(out=d['sd'], in0=sinks[:, c, :], in1=d['nmx'], op=ALU.add), r=[d['sk'], 'sinks'], w=[d['sk']])
                for d in ctx:
                    for hs in range(2):
                        P.act(lambda h, hs=hs, d=d: h.activation(
                            out=d['e3'][:, hs, :], in_=d['t3'][:, hs, :], func=AF.Exp, bias=d['nmx'][:, hs:hs + 1], scale=1.0,
                            accum_out=d['rs'][:, hs:hs + 1]), r=[d['tk'], d['sk']], w=[f"{d['ek']}{hs}", f"{d['sk']}r{hs}"])
                    P.act(lambda h, d=d: h.activation(out=d['es'], in_=d['sd'], func=AF.Exp), r=[d['sk']], w=[d['sk'] + 'e'])
                for d in ctx:
                    sk = d['sk']
                    P.dve(lambda h, d=d: h.tensor_tensor(out=d['rs'], in0=d['rs'], in1=d['es'], op=ALU.add), r=[sk + 'r0', sk + 'r1', sk + 'e'], w=[sk + 'r0', sk + 'r1'])
                for d in ctx:
                    sk = d['sk']
                    P.dve(lambda h, d=d: h.reciprocal(out=d['rinv'], in_=d['rs']), r=[sk + 'r0', sk + 'r1'], w=[sk + 'i'])
                for d in ctx:
                    P.pool(lambda h, d=d: h.tensor_tensor(
                        out=d['p3'], in0=d['e3'], in1=d['rinv'].unsqueeze(2).to_broadcast([128, 2, 256]), op=ALU.mult),
                        r=[d['ek'] + '0', d['ek'] + '1', d['sk'] + 'i'], w=[d['pk']])
                for d in ctx:
                    bT = d['bT']
                    for hs in range(2):
                        for hf in d['halves']:
                            mm(ps[bT][:, (hs * 2 + hf) * 128:(hs * 2 + hf + 1) * 128], d['p3'][:, hs, hf * 128:(hf + 1) * 128], ident[:],
                               True, True, r=[d['pk'], 'ident'], w=[f'ps{bT}'])
                for i, d in enumerate(ctx):
                    if i % 2 == 0:
                        P.act(lambda h, d=d: h.copy(out=d['pT'], in_=ps[d['bT']][:, :]), r=[f"ps{d['bT']}"], w=[d['ptk']])
                    else:
                        P.dve(lambda h, d=d: h.tensor_copy(out=d['pT'], in_=ps[d['bT']][:, :]), r=[f"ps{d['bT']}"], w=[d['ptk']])
                for d in ctx:
                    n = d['n']
                    items = [(hs, hf) for hs in range(2) for hf in d['halves']]
                    for ii, (hs, hf) in enumerate(items):
                        Vx, vk = (V0, 'V0') if hs == 0 else (V1, 'V1')
                        mm(ps[by][:, (n % 4) * 128:(n % 4 + 1) * 128], Vx[:, n - 1 + hf, :], d['pT3'][:, hs * 2 + hf, :],
                           ii == 0, ii == len(items) - 1, r=[vk, d['ptk']], w=[f'ps{by}'])
                P.dve(lambda h, by=by, c=c, n0=n0: h.tensor_copy(out=yT[:, c, n0 * 128:(n0 + 4) * 128], in_=ps[by][:, :]),
                      r=[f'ps{by}'], w=['yT'])
        if cut == 'att':
            raise _Cut()
        P.barrier()
        P.tag = 'phase_a0_hgrn'
        AR.off = mark
        sq = AR.bf16(2048)
        omf = AR.bf16(2048)
        fA = AR.f32(2048)
        bt = AR.f32(2048)
        tmp1 = AR.f32(2048)
        qd2 = AR.bf16(2048)
        qd = AR.bf16(2048)
        kd = AR.bf16(2048)
        kd2 = AR.bf16(2048)
        kdx = AR.bf16(2048).rearrange("p (j s) -> p j s", s=128)
        Vh = AR.bf16(2048).rearrange("p (j s) -> p j s", s=128)
        gs = AR.bf16(2048).rearrange("p (j s) -> p j s", s=128)
        lbt = AR.f32(24).rearrange("p (h l) -> p h l", l=3)
        lb = AR.f32(8)
        oml = AR.f32(8)
        lsum = AR.f32(8)
        ebl = AR.f32(16)
        Sst = AR.f32(128)
        y_sbs = [AR.bf16(128) for _ in range(2)]
        ss_all = AR.f32(48).rearrange("p (j s) -> p j s", s=3)
        normg = AR.f32(1024)
        tmp2 = fA
        sdma(lbt, din['lbl'], w=['lbt'])
        sdma(normg, din['normg'], w=['normg'])
        P.act(lambda h: h.activation(out=lbt, in_=lbt, func=AF.Exp), r=['lbt'], w=['lbt'])
        P.dve(lambda h: h.tensor_reduce(out=lsum, in_=lbt, axis=AX.X, op=ALU.add), r=['lbt'], w=['lsum'])
        P.dve(lambda h: h.reciprocal(out=lsum, in_=lsum), r=['lsum'], w=['lsum'])
        P.dve(lambda h: h.tensor_tensor(out=lb, in0=lbt[:, :, 0], in1=lsum, op=ALU.mult), r=['lbt', 'lsum'], w=['lb'])
        P.dve(lambda h: h.tensor_scalar(out=oml, in0=lb, scalar1=-1.0, scalar2=1.0, op0=ALU.mult, op1=ALU.add), r=['lb'], w=['oml'])
        P.pool(lambda h: h.memset(kdx, 0.0), w=['kdx'])
        def projqf(hd):
            wb, wk = next_w(10 + 4 * hd + 0)
            proj_fm(xT, 'xT', wb, wk, lambda tt, b: P.act(
                lambda h: h.activation(out=sq[:, tt * 512:(tt + 1) * 512], in_=ps[b][:, :], func=AF.Silu), r=[f'ps{b}'], w=['sq']))
            wb, wk = next_w(10 + 4 * hd + 1)
            proj_fm(xT, 'xT', wb, wk, lambda tt, b: P.act(
                lambda h: h.activation(out=fA[:, tt * 512:(tt + 1) * 512], in_=ps[b][:, :], func=AF.Sigmoid), r=[f'ps{b}'], w=['fA']))
        sqj = AR.f32(256)
        projqf(0)
        for hd in range(8):
            P.dve(lambda h, hd=hd: h.tensor_scalar(out=fA, in0=fA, scalar1=oml[:, hd:hd + 1], scalar2=lb[:, hd:hd + 1],
                                                  op0=ALU.mult, op1=ALU.add), r=['fA', 'oml', 'lb'], w=['fA'])
            P.act(lambda h: h.activation(out=tmp1, in_=fA, func=AF.Ln), r=['fA'], w=['tmp1'])
            for j in range(16):
                P.dve(lambda h, j=j: h.tensor_tensor_scan(out=bt[:, j * 128:(j + 1) * 128], data0=ones_f[:, :],
                                                         data1=tmp1[:, j * 128:(j + 1) * 128], initial=0.0, op0=ALU.mult, op1=ALU.add),
                      r=['tmp1', 'ones_f'], w=['bt'])
            P.dve(lambda h: h.tensor_scalar(out=omf, in0=fA, scalar1=-1.0, scalar2=1.0, op0=ALU.mult, op1=ALU.add), r=['fA'], w=['omf'])
            P.act(lambda h: h.activation(out=tmp1, in_=bt, func=AF.Exp), r=['bt'], w=['tmp1'])
            P.dve(lambda h: h.tensor_tensor(out=qd2, in0=sq, in1=tmp1, op=ALU.mult), r=['sq', 'tmp1'], w=['qd2'])
            bt64 = bt.rearrange("p (c s) -> p c s", s=64)
            P.dve(lambda h: h.tensor_tensor(out=tmp2.rearrange("p (c s) -> p c s", s=64), in0=bt64,
                                            in1=bt64[:, :, 31:32].to_broadcast([128, 32, 64]), op=ALU.subtract), r=['bt', 'omf'], w=['fA'])
            P.act(lambda h: h.activation(out=tmp1, in_=tmp2, func=AF.Exp), r=['fA', 'qd2'], w=['tmp1'])
            P.dve(lambda h: h.tensor_tensor(out=qd, in0=sq, in1=tmp1, op=ALU.mult), r=['sq', 'tmp1'], w=['qd'])
            P.act(lambda h: h.activation(out=tmp1, in_=tmp2, func=AF.Exp, scale=-1.0), r=['fA', 'qd'], w=['tmp1'])
            P.dve(lambda h: h.tensor_tensor(out=kd, in0=omf, in1=tmp1, op=ALU.mult), r=['omf', 'tmp1'], w=['kd'])
            bt128 = bt.rearrange("p (j s) -> p j s", s=128)
            P.dve(lambda h: h.tensor_tensor(out=tmp2.rearrange("p (j s) -> p j s", s=128), in0=bt128,
                                            in1=bt128[:, :, 127:128].to_broadcast([128, 16, 128]), op=ALU.subtract), r=['bt', 'kd'], w=['fA'])
            P.act(lambda h: h.activation(out=tmp1, in_=tmp2, func=AF.Exp, scale=-1.0), r=['fA', 'kd'], w=['tmp1'])
            P.dve(lambda h: h.tensor_tensor(out=kd2, in0=omf, in1=tmp1, op=ALU.mult), r=['omf', 'tmp1'], w=['kd2'])
            P.dve(lambda h: h.tensor_tensor(out=tmp2.rearrange("p (j s) -> p j s", s=128)[:, :, 0:64], in0=bt128[:, :, 0:64],
                                            in1=bt128[:, :, 95:96].to_broadcast([128, 16, 64]), op=ALU.subtract), r=['bt', 'kd2'], w=['fA'])
            P.act(lambda h: h.activation(out=tmp1.rearrange("p (j s) -> p j s", s=128)[:, :, 0:64],
                                         in_=tmp2.rearrange("p (j s) -> p j s", s=128)[:, :, 0:64], func=AF.Exp, scale=-1.0),
                  r=['fA', 'kd2'], w=['tmp1'])
            P.dve(lambda h: h.tensor_tensor(out=kdx[:, :, 0:64], in0=omf.rearrange("p (j s) -> p j s", s=128)[:, :, 0:64],
                                            in1=tmp1.rearrange("p (j s) -> p j s", s=128)[:, :, 0:64], op=ALU.mult),
                  r=['omf', 'tmp1'], w=['kdx'])
            P.act(lambda h: h.activation(out=ebl, in_=bt128[:, :, 127], func=AF.Exp), r=['bt'], w=['ebl'])
            wb, wk = next_w(10 + 4 * hd + 2)
            proj_tm(xT, 'xT', wb, wk, 128, lambda g, b: P.act(
                lambda h: h.copy(out=Vh[:, 4 * g:4 * g + 4, :], in_=ps[b][:, :].rearrange("p (j c) -> p j c", c=128)),
                r=[f'ps{b}'], w=['Vh']))
            wb, wk = next_w(10 + 4 * hd + 3)

            def ev_g(g, b, hd=hd):
                t4 = tmp1[:, 0:512].rearrange("p (j c) -> p j c", c=128)
                P.act(lambda h: h.activation(out=tmp1[:, 0:512], in_=ps[b][:, :], func=AF.Silu), r=[f'ps{b}', 'kdx'], w=['tmp1'])
                P.pool(lambda h: h.tensor_tensor(out=gs[:, 4 * g:4 * g + 4, :], in0=t4,
                                                 in1=normg[:, hd * 128:(hd + 1) * 128].unsqueeze(1).to_broadcast([128, 4, 128]),
                                                 op=ALU.mult), r=['tmp1', 'normg'], w=['gs'])
            proj_tm(xT, 'xT', wb, wk, 128, ev_g)
            A_all = tmp1.bitcast(BF16)[:, 0:2048].rearrange("p (j s) -> p j s", s=128)
            kd2T_all = tmp1.bitcast(BF16)[:, 2048:4096].rearrange("p (j s) -> p j s", s=128)
            Sbf_all = bt.bitcast(BF16)[:, 0:17 * 128].rearrange("p (j s) -> p j s", s=128)
            for j in range(16):
                js = slice(j * 128, (j + 1) * 128)
                bA = nb()
                mm(ps[bA][:, 0:128], kd[:, js], qd[:, js], True, True, r=['kd', 'qd'], w=[f'ps{bA}'])
                mm(ps[bA][:, 128:192], kdx[:, j, :], qd[:, j * 128 + 64:(j + 1) * 128], True, True, r=['kdx', 'qd'], w=[f'ps{bA}'])
                mm(ps[bA][:, 256:384], kd2[:, js], ident[:], True, True, r=['kd2', 'ident'], w=[f'ps{bA}'])
                P.dve(lambda h, bA=bA, j=j: h.tensor_tensor(out=A_all[:, j, :], in0=ps[bA][:, 0:128], in1=mask_bd[:], op=ALU.mult),
                      r=[f'ps{bA}', 'mask_bd', 'ebl', 'gs'], w=['tmp1'])
                P.dve(lambda h, bA=bA, j=j: h.tensor_copy(out=A_all[0:64, j, 64:128], in_=ps[bA][0:64, 128:192]), r=[f'ps{bA}'], w=['tmp1'])
                P.act(lambda h, bA=bA, j=j: h.copy(out=kd2T_all[:, j, :], in_=ps[bA][:, 256:384]), r=[f'ps{bA}', 'tmp1'], w=['tmp1k'])
            if hd + 1 < 8:
                projqf(hd + 1)
            P.dve(lambda h: h.memset(Sst, 0.0), w=['S'])
            P.pool(lambda h: h.memset(Sbf_all[:, 0, :], 0.0), r=['ebl'], w=['bt'])
            bU = None
            for j in range(16):
                if j % 4 == 0:
                    bU = nb()
                mm(ps[bU][:, (j % 4) * 128:(j % 4 + 1) * 128], kd2T_all[:, j, :], Vh[:, j, :], True, True, r=['tmp1k', 'Vh'], w=[f'ps{bU}'])
                P.dve(lambda h, bU=bU, j=j: h.scalar_tensor_tensor(out=Sst, in0=Sst, scalar=ebl[:, j:j + 1], in1=ps[bU][:, (j % 4) * 128:(j % 4 + 1) * 128],
                                                                  op0=ALU.mult, op1=ALU.add), r=['S', 'ebl', f'ps{bU}'], w=['S'])
                P.act(lambda h, j=j: h.copy(out=Sbf_all[:, j + 1, :], in_=Sst), r=['S'], w=['bt'])
            bTr = None
            for j in range(16):
                js = slice(j * 128, (j + 1) * 128)
                bO = nb()
                ysb = y_sbs[j % 2]
                yk = f'y_sb{j % 2}'
                mm(ps[bO][:, 0:128], A_all[:, j, :], Vh[:, j, :], True, False, r=['tmp1', 'Vh'], w=[f'ps{bO}'])
                mm(ps[bO][:, 0:128], qd2[:, js], Sbf_all[:, j, :], False, True, r=['qd2', 'bt'], w=[f'ps{bO}'])
                P.act(lambda h, bO=bO, j=j: h.activation(out=sqj[:, (j % 2) * 128:(j % 2 + 1) * 128], in_=ps[bO][:, 0:128], func=AF.Square,
                                                          accum_out=ss_all[:, j, 0:1]), r=[f'ps{bO}'], w=[f'ss{j}', f'sqj{j % 2}'])
                P.act(lambda h, j=j: h.activation(out=ss_all[:, j, 1:2], in_=ss_all[:, j, 0:1], func=AF.Sqrt, bias=RMS_EPS, scale=1.0 / 128),
                      r=[f'ss{j}'], w=[f'ss1{j}'])
                P.dve(lambda h, j=j: h.reciprocal(out=ss_all[:, j, 2:3], in_=ss_all[:, j, 1:2]), r=[f'ss1{j}'], w=[f'ss2{j}'])
                P.dve(lambda h, bO=bO, j=j, ysb=ysb: h.scalar_tensor_tensor(out=ysb, in0=ps[bO][:, 0:128], scalar=ss_all[:, j, 2:3], in1=gs[:, j, :],
                                                                           op0=ALU.mult, op1=ALU.mult), r=[f'ps{bO}', f'ss2{j}', 'gs'], w=[yk])
                if j % 4 == 0:
                    bTr = nb()
                mm(ps[bTr][:, (j % 4) * 128:(j % 4 + 1) * 128], ysb, ident[:], True, True, r=[yk, 'ident'], w=[f'ps{bTr}'])
                if j % 4 == 3:
                    P.act(lambda h, bTr=bTr, j=j, hd=hd: h.copy(out=yT[:, 8 + hd, (j - 3) * 128:(j + 1) * 128], in_=ps[bTr][:, :]),
                          r=[f'ps{bTr}'], w=['yT'])
        P.barrier()


    def layer_norm_gen(src, skey, dst, dkey, lnp, lnkey, junk, st, jk='junk', sx=''):
        P.act(lambda h: h.activation(out=junk, in_=src, func=AF.Copy, accum_out=st[:, 0:1]), r=[skey], w=[jk, 'st0' + sx])
        yield
        P.act(lambda h: h.activation(out=junk, in_=src, func=AF.Square, accum_out=st[:, 1:2]), r=[skey], w=[jk, 'st1' + sx])
        yield
        P.dve(lambda h: h.tensor_scalar(out=st[:, 2:3], in0=st[:, 0:1], scalar1=1.0 / D, scalar2=None, op0=ALU.mult), r=['st0' + sx], w=['st2' + sx])
        yield
        P.dve(lambda h: h.tensor_tensor(out=st[:, 3:4], in0=st[:, 2:3], in1=st[:, 2:3], op=ALU.mult), r=['st2' + sx], w=['st3' + sx])
        yield
        P.dve(lambda h: h.scalar_tensor_tensor(out=st[:, 4:5], in0=st[:, 1:2], scalar=1.0 / D, in1=st[:, 3:4], op0=ALU.mult, op1=ALU.subtract),
              r=['st1' + sx, 'st3' + sx], w=['st4' + sx])
        yield
        P.act(lambda h: h.activation(out=st[:, 5:6], in_=st[:, 4:5], func=AF.Sqrt, bias=LN_EPS, scale=1.0), r=['st4' + sx], w=['st5' + sx])
        yield
        P.dve(lambda h: h.reciprocal(out=st[:, 6:7], in_=st[:, 5:6]), r=['st5' + sx], w=['st6' + sx])
        yield
        P.dve(lambda h: h.tensor_scalar(out=dst, in0=src, scalar1=st[:, 2:3], scalar2=st[:, 6:7], op0=ALU.subtract, op1=ALU.mult),
              r=[skey, 'st2' + sx, 'st6' + sx], w=[dkey])
        yield
        P.dve(lambda h: h.tensor_tensor(out=dst, in0=dst, in1=lnp[:, 0, :], op=ALU.mult), r=[dkey, lnkey], w=[dkey])
        yield
        P.dve(lambda h: h.tensor_tensor(out=dst, in0=dst, in1=lnp[:, 1, :], op=ALU.add), r=[dkey, lnkey], w=[dkey])
        yield


    def layer_norm_tile(src, skey, dst, dkey, lnp, lnkey, junk, st):
        for _ in layer_norm_gen(src, skey, dst, dkey, lnp, lnkey, junk, st):
            pass

    def zip_run(gens):
        gens = list(gens)
        while gens:
            for g in list(gens):
                try:
                    next(g)
                except StopIteration:
                    gens.remove(g)

    def phase_c(l, xin_d, xout_d):
        AR.reset()
        Wout, yT = bigA, bigB
        wsrc = din[f'w_out{l}']
        for q in range(4):
            pdma(Wout[:, 4 * q:4 * q + 4, :], wsrc[:, 4 * q:4 * q + 4, :], w=['xT'])
        lnp = AR.f32(4096).rearrange("p (a b) -> p a b", b=2048)
        sdma(lnp, din[f'lnmix{l}'], w=['lnp'])
        wr_bf = AR.bf16(16 * 36).rearrange("p (a b) -> p a b", b=36)
        pdma(wr_bf, din[f'wr{l}'], w=['wr'])
        brt = AR.f32(36)
        sdma(brt, din[f'br{l}'], w=['brt'])
        zkeys = k.zkeys if l == 0 else []
        lg_all = AR.f32(16 * 36).rearrange("p (j c) -> p j c", c=36)
        markc = AR.off
        xs = [AR.f32(2048) for _ in range(2)]
        rbufs = [AR.f32(2048) for _ in range(2)]
        x1Ts = [AR.bf16(2048).rearrange("p (a b) -> p a b", b=128) for _ in range(2)]
        sts = [AR.f32(8) for _ in range(2)]
        BIGV = 1000.0

        def load_x(j):
            sdma(xs[j % 2], xin_d[j * 128:(j + 1) * 128, :], w=[f'xs{j % 2}'])

        def tile_c(j):
            pr = j % 2
            x_s, xk = xs[pr], f'xs{pr}'
            rbuf, rk = rbufs[pr], f'rbuf{pr}'
            x1T, st = x1Ts[pr], sts[pr]
            junk_j = x_s.bitcast(BF16)[:, 0:2048]
            xb = x_s.bitcast(BF16)[:, 2048:4096]
            for db in range(4):
                b = nb()
                for fc in range(16):
                    mm(ps[b][:, :], yT[:, fc, j * 128:(j + 1) * 128], Wout[:, fc, db * 512:(db + 1) * 512], fc == 0, fc == 15,
                       r=['yT', 'xT'], w=[f'ps{b}'])
                P.dve(lambda h, b=b, db=db: h.scalar_tensor_tensor(
                    out=rbuf[:, db * 512:(db + 1) * 512], in0=x_s[:, db * 512:(db + 1) * 512], scalar=ALPHA, in1=ps[b][:, :],
                    op0=ALU.mult, op1=ALU.add), r=[xk, f'ps{b}'], w=[f'{rk}_{db}'])
                yield
            P.dve(lambda h: h.tensor_copy(out=st[:, 7:8], in_=st[:, 7:8]), r=[f'{rk}_{d_}' for d_ in range(4)], w=[rk] + [f'{rk}_{d_}' for d_ in range(4)])
            yield
            yield from layer_norm_gen(rbuf, rk, rbuf, rk, lnp, 'lnp', junk_j, st, jk=xk, sx=f'_{pr}')
            sdma(xout_d[j * 128:(j + 1) * 128, :], rbuf, r=[rk], w=[uid('xout'), rk])
            yield
            P.act(lambda h: h.copy(out=xb, in_=rbuf), r=[rk], w=[xk])
            yield
            sdma(x1b_d[j * 128:(j + 1) * 128, :], xb, r=[xk], w=[uid('x1bd'), xk])
            yield
            for g4 in range(4):
                b = nb()
                for q in range(4):
                    kc = g4 * 4 + q
                    mm(ps[b][:, q * 128:(q + 1) * 128], xb[:, kc * 128:(kc + 1) * 128], ident[:], True, True, r=[xk, 'ident'], w=[f'ps{b}'])
                if g4 % 2 == 0:
                    P.act(lambda h, b=b, g4=g4: h.copy(out=x1T[:, 4 * g4:4 * g4 + 4, :], in_=ps[b][:, :].rearrange("p (a b) -> p a b", b=128)),
                          r=[f'ps{b}'], w=[f'x1T{pr}_{g4}'])
                else:
                    P.dve(lambda h, b=b, g4=g4: h.tensor_copy(out=x1T[:, 4 * g4:4 * g4 + 4, :], in_=ps[b][:, :].rearrange("p (a b) -> p a b", b=128)),
                          r=[f'ps{b}'], w=[f'x1T{pr}_{g4}'])
                yield
            b = nb()
            for kc in range(16):
                mm(ps[b][:, 0:36], x1T[:, kc, :], wr_bf[:, kc, :], kc == 0, kc == 15, r=[f'x1T{pr}_{kc // 4}', 'wr'], w=[f'ps{b}'])
            P.dve(lambda h, b=b: h.tensor_tensor(out=lg_all[:, j, :], in0=ps[b][:, 0:36], in1=brt, op=ALU.add), r=[f'ps{b}', 'brt'], w=[uid('lg')])
            yield
        load_x(0)
        load_x(1)
        for j in range(0, NT, 2):
            zip_run([tile_c(j), tile_c(j + 1)])
            if j + 2 < NT:
                load_x(j + 2)
                load_x(j + 3)
        P.barrier()
        AR.off = markc
        V = P.dve
        T3 = lambda n: AR.f32(16 * n).rearrange("p (j c) -> p j c", c=n)
        gmask, eg, pen = T3(4), T3(4), T3(4)
        em, em2, mask1, mask2, tmpe, rank, slot, okm, cs, base = [T3(32) for _ in range(10)]
        m12b = AR.bf16(512).rearrange("p (j c) -> p j c", c=32)
        gmax, gsum, gw, m1, m2, dd, e2, den, w1, w2 = [AR.f32(16) for _ in range(10)]
        dstk, okk, dfin = AR.f32(16), AR.f32(16), AR.f32(16)
        G = lg_all[:, :, 0:4]
        L = lg_all[:, :, 4:36]
        bc = lambda a, n: a.unsqueeze(2).to_broadcast([128, 16, n])
        V(lambda h: h.tensor_reduce(out=gmax, in_=G, axis=AX.X, op=ALU.max), r=['lg'], w=['gmax'])
        V(lambda h: h.tensor_tensor(out=gmask, in0=G, in1=bc(gmax, 4), op=ALU.is_equal), r=['lg', 'gmax'], w=['gmask'])
        V(lambda h: h.tensor_tensor(out=eg, in0=G, in1=bc(gmax, 4), op=ALU.subtract), r=['lg', 'gmax'], w=['eg'])
        P.act(lambda h: h.activation(out=eg, in_=eg, func=AF.Exp), r=['eg'], w=['eg'])
        V(lambda h: h.tensor_reduce(out=gsum, in_=eg, axis=AX.X, op=ALU.add), r=['eg'], w=['gsum'])
        V(lambda h: h.reciprocal(out=gw, in_=gsum), r=['gsum'], w=['gw'])
        V(lambda h: h.tensor_scalar(out=pen, in0=gmask, scalar1=BIGV, scalar2=-BIGV, op0=ALU.mult, op1=ALU.add), r=['gmask'], w=['pen'])
        em64 = em.rearrange("p j (g e) -> p (j g) e", e=8)
        pen64 = pen.rearrange("p j g -> p (j g)")
        V(lambda h: h.tensor_copy(out=em, in_=L), r=['lg'], w=['em'])
        V(lambda h: h.tensor_tensor(out=em64, in0=em64, in1=pen64.unsqueeze(2).to_broadcast([128, 64, 8]), op=ALU.add), r=['em', 'pen'], w=['em'])
        V(lambda h: h.tensor_reduce(out=m1, in_=em, axis=AX.X, op=ALU.max), r=['em'], w=['m1'])
        V(lambda h: h.tensor_tensor(out=mask1, in0=em, in1=bc(m1, 32), op=ALU.is_equal), r=['em', 'm1'], w=['mask1'])
        V(lambda h: h.scalar_tensor_tensor(out=em2, in0=mask1, scalar=-BIGV, in1=em, op0=ALU.mult, op1=ALU.add), r=['mask1', 'em'], w=['em2'])
        V(lambda h: h.tensor_reduce(out=m2, in_=em2, axis=AX.X, op=ALU.max), r=['em2'], w=['m2'])
        V(lambda h: h.tensor_tensor(out=mask2, in0=em2, in1=bc(m2, 32), op=ALU.is_equal), r=['em2', 'm2'], w=['mask2'])
        V(lambda h: h.tensor_tensor(out=dd, in0=m2, in1=m1, op=ALU.subtract), r=['m1', 'm2'], w=['dd'])
        P.act(lambda h: h.activation(out=e2, in_=dd, func=AF.Exp), r=['dd'], w=['e2'])
        V(lambda h: h.tensor_scalar(out=den, in0=e2, scalar1=1.0, scalar2=None, op0=ALU.add), r=['e2'], w=['den'])
        V(lambda h: h.reciprocal(out=den, in_=den), r=['den'], w=['den'])
        V(lambda h: h.tensor_tensor(out=w1, in0=den, in1=gw, op=ALU.mult), r=['den', 'gw'], w=['w1'])
        V(lambda h: h.tensor_tensor(out=w2, in0=w1, in1=e2, op=ALU.mult), r=['w1', 'e2'], w=['w2'])
        V(lambda h: h.tensor_tensor(out=tmpe, in0=mask1, in1=mask2, op=ALU.add), r=['mask1', 'mask2'], w=['tmpe'])
        V(lambda h: h.tensor_copy(out=m12b, in_=tmpe), r=['tmpe'], w=['m12b'])
        bR, bC = nb(), nb()
        for j in range(NT):
            mm(ps[bR][:, j * 32:(j + 1) * 32], tri_strict[:], m12b[:, j, :], True, True, r=['tri_strict', 'm12b'], w=[f'ps{bR}'])
        for j in range(NT):
            mm(ps[bC][:, j * 32:(j + 1) * 32], ones_bf[:], m12b[:, j, :], True, True, r=['ones_bf', 'm12b'], w=[f'ps{bC}'])
        V(lambda h: h.tensor_copy(out=cs, in_=ps[bC][:, :].rearrange("p (j c) -> p j c", c=32)), r=[f'ps{bC}'], w=['cs'])
        V(lambda h: h.memset(base[:, 0, :], 0.0), w=['base'])
        for j in range(1, NT):
            V(lambda h, j=j: h.tensor_tensor(out=base[:, j, :], in0=base[:, j - 1, :], in1=cs[:, j - 1, :], op=ALU.add), r=['base', 'cs'], w=['base'])
        V(lambda h: h.tensor_tensor(out=rank, in0=ps[bR][:, :].rearrange("p (j c) -> p j c", c=32), in1=base, op=ALU.add), r=[f'ps{bR}', 'base'], w=['rank'])
        V(lambda h: h.tensor_tensor(out=slot, in0=rank, in1=ecap[:].unsqueeze(1).to_broadcast([128, 16, 32]), op=ALU.add), r=['rank', 'ecap'], w=['slot'])
        V(lambda h: h.tensor_single_scalar(out=okm, in_=rank, scalar=float(CAP), op=ALU.is_lt), r=['rank'], w=['okm'])
        for kk, (mk, mkey, wk_, wkey) in enumerate([(mask1, 'mask1', w1, 'w1'), (mask2, 'mask2', w2, 'w2')]):
            V(lambda h, mk=mk: h.tensor_tensor(out=tmpe, in0=mk, in1=slot, op=ALU.mult), r=[mkey, 'slot', 'm12b'], w=['tmpe'])
            V(lambda h: h.tensor_reduce(out=dstk, in_=tmpe, axis=AX.X, op=ALU.add), r=['tmpe'], w=['dstk'])
            V(lambda h, mk=mk: h.tensor_tensor(out=tmpe, in0=mk, in1=okm, op=ALU.mult), r=[mkey, 'okm', 'dstk'], w=['tmpe'])
            V(lambda h: h.tensor_reduce(out=okk, in_=tmpe, axis=AX.X, op=ALU.add), r=['tmpe'], w=['okk'])
            V(lambda h: h.tensor_scalar(out=dfin, in0=dstk, scalar1=pidx[:, 0:1], scalar2=None, op0=ALU.subtract), r=['dstk', 'pidx'], w=['dfin'])
            V(lambda h: h.tensor_tensor(out=dfin, in0=dfin, in1=okk, op=ALU.mult), r=['dfin', 'okk'], w=['dfin'])
            V(lambda h: h.tensor_scalar(out=dfin, in0=dfin, scalar1=pidx[:, 0:1], scalar2=None, op0=ALU.add), r=['dfin', 'pidx'], w=['dfin'])
            V(lambda h, kk=kk: h.tensor_copy(out=dests[:, :, kk], in_=dfin), r=['dfin'], w=[f'dests{kk}'])
            V(lambda h, kk=kk, wk_=wk_: h.tensor_tensor(out=wts[:, :, kk], in0=wk_, in1=okk, op=ALU.mult), r=[wkey, 'okk'], w=[f'wts{kk}'])
        xsc = [AR.bf16(2048) for _ in range(2)]
        for j in range(NT):
            xb = xsc[j % 2]
            xbk = f'xsc{j % 2}'
            sdma(xb, x1b_d[j * 128:(j + 1) * 128, :], w=[xbk])
            for kk in range(2):
                P.dma('pool', lambda h, kk=kk, j=j, xb=xb: h.indirect_dma_start(
                    out=xg_d, out_offset=bass.IndirectOffsetOnAxis(ap=dests[:, j, kk:kk + 1], axis=0), in_=xb, in_offset=None),
                    r=[xbk, f'dests{kk}'] + (zkeys if j == 0 else []), w=[uid('xg'), xbk])
        P.barrier()

    def phase_d(l):
        AR.reset()
        NSC = CAP // 128
        xg = [AR.bf16(NSC * 2048).rearrange("p (a b) -> p a b", b=2048) for _ in range(2)]
        xgT = AR.bf16(16 * CAP).rearrange("p (a b) -> p a b", b=CAP)
        hT = AR.bf16(4 * CAP).rearrange("p (a b) -> p a b", b=CAP)
        sa = [AR.f32(CAP) for _ in range(2)]
        ybs = [AR.bf16(2048) for _ in range(4)]
        ycnt = 0

        def wviews(e):
            big = bigA if e % 2 == 0 else bigB
            return (big[:, 0:4, :].rearrange("p a (b c) -> p (a b) c", c=512),
                    big[:, 4:8, :].rearrange("p a (b c) -> p (a b) c", c=512),
                    big[:, 8:12, :])

        def prefetch(e):
            w1v, w3v, w2v = wviews(e)
            wk = f'wset{e % 2}'
            pdma(w1v, din['moe_w1'][l, e].rearrange("(kc p) f -> p kc f", p=128), w=[wk + 'a'])
            pdma(w3v, din['moe_w3'][l, e].rearrange("(kc p) f -> p kc f", p=128), w=[wk + 'b'])
            pdma(w2v, din['moe_w2'][l, e].rearrange("(fc p) d -> p fc d", p=128), w=[wk + 'c'])
            sdma(xg[e % 2], xg_d[e * CAP:(e + 1) * CAP, :].rearrange("(sc p) d -> p sc d", p=128), w=[f'xg{e % 2}'])
        prefetch(0)
        for e in range(NE):
            if e + 1 < NE:
                prefetch(e + 1)
            wk = f'wset{e % 2}'
            w1v, w3v, w2v = wviews(e)
            xge = xg[e % 2]
            xgk = f'xg{e % 2}'
            ec = 0
            for sc in range(NSC):
                for g4 in range(4):
                    b = nb()
                    for q in range(4):
                        kc = g4 * 4 + q
                        mm(ps[b][:, q * 128:(q + 1) * 128], xge[:, sc, kc * 128:(kc + 1) * 128], ident[:], True, True, r=[xgk, 'ident'], w=[f'ps{b}'])
                    src = ps[b][:, :].rearrange("p (a b) -> p a b", b=128)
                    dstv = xgT[:, 4 * g4:4 * g4 + 4, sc * 128:(sc + 1) * 128]
                    if ec % 2 == 0:
                        P.act(lambda h, src=src, dstv=dstv: h.copy(out=dstv, in_=src), r=[f'ps{b}'], w=[uid('xgT')])
                    else:
                        P.dve(lambda h, src=src, dstv=dstv: h.tensor_copy(out=dstv, in_=src), r=[f'ps{b}'], w=[uid('xgT')])
                    ec += 1
            P.dve(lambda h: h.tensor_copy(out=sa[0][:, 0:1], in_=sa[0][:, 0:1]), r=[f'xgT#{k.cnt - i_}' for i_ in range(NSC * 4)], w=['xgT'])
            for fc in range(4):
                ba = nb()
                for kc in range(16):
                    mm(ps[ba][:, 0:CAP], w1v[:, kc, fc * 128:(fc + 1) * 128], xgT[:, kc, :], kc == 0, kc == 15, r=[wk + 'a', 'xgT'], w=[f'ps{ba}'])
                bb = nb()
                for kc in range(16):
                    mm(ps[bb][:, 0:CAP], w3v[:, kc, fc * 128:(fc + 1) * 128], xgT[:, kc, :], kc == 0, kc == 15, r=[wk + 'b', 'xgT'], w=[f'ps{bb}'])
                s_ = sa[fc % 2]
                P.act(lambda h, ba=ba, s_=s_: h.activation(out=s_, in_=ps[ba][:, 0:CAP], func=AF.Silu), r=[f'ps{ba}'], w=[f'sa{fc % 2}'])
                P.dve(lambda h, bb=bb, s_=s_, fc=fc: h.tensor_tensor(out=hT[:, fc, :], in0=s_, in1=ps[bb][:, 0:CAP], op=ALU.mult),
                      r=[f'sa{fc % 2}', f'ps{bb}'], w=[f'hT{fc}'])
            for sc in range(NSC):
                yb_ = ybs[ycnt % 4]
                ybk = f'ybs{ycnt % 4}'
                ycnt += 1
                for db in range(4):
                    b = nb()
                    for fc in range(4):
                        mm(ps[b][:, :], hT[:, fc, sc * 128:(sc + 1) * 128], w2v[:, fc, db * 512:(db + 1) * 512], fc == 0, fc == 3,
                           r=[f'hT{fc}', wk + 'c'], w=[f'ps{b}'])
                    if db % 2 == 0:
                        P.act(lambda h, b=b, db=db, yb_=yb_: h.copy(out=yb_[:, db * 512:(db + 1) * 512], in_=ps[b][:, :]), r=[f'ps{b}'], w=[f'{ybk}_{db}'])
                    else:
                        P.dve(lambda h, b=b, db=db, yb_=yb_: h.tensor_copy(out=yb_[:, db * 512:(db + 1) * 512], in_=ps[b][:, :]), r=[f'ps{b}'], w=[f'{ybk}_{db}'])
                sdma(yb_d[e * CAP + sc * 128:e * CAP + (sc + 1) * 128, :], yb_, r=[f'{ybk}_{d_}' for d_ in range(4)], w=[uid('ybd')] + [f'{ybk}_{d_}' for d_ in range(4)])
        P.barrier()

    def phase_e(l, xin_d, xout_d, make_xT):
        AR.reset()
        lnp = AR.f32(4096).rearrange("p (a b) -> p a b", b=2048)
        sdma(lnp, din[f'lnffn{l}'], w=['lnp'])
        r0 = [AR.f32(2048) for _ in range(2)]
        r1 = [AR.f32(2048) for _ in range(2)]
        g0 = [AR.bf16(2048) for _ in range(2)]
        g1 = [r1[i].bitcast(BF16)[:, 2048:4096] for i in range(2)]
        xs = [AR.f32(2048) for _ in range(2)]
        st = AR.f32(8)
        outs = []
        def loads(j):
            a0, a1, x_s = r0[j % 2], r1[j % 2], xs[j % 2]
            k0, k1, xk = f'r0{j % 2}', f'r1{j % 2}', f'xs{j % 2}'
            P.dma('pool', lambda h, j=j: h.indirect_dma_start(
                out=g0[j % 2], out_offset=None, in_=yb_d, in_offset=bass.IndirectOffsetOnAxis(ap=dests[:, j, 0:1], axis=0)), w=[f'g0{j % 2}'])
            P.dma('pool', lambda h, j=j: h.indirect_dma_start(
                out=g1[j % 2], out_offset=None, in_=yb_d, in_offset=bass.IndirectOffsetOnAxis(ap=dests[:, j, 1:2], axis=0)), w=[k1])
            sdma(x_s, xin_d[j * 128:(j + 1) * 128, :], w=[xk])
        def tile_gen(j):
            a0, a1, x_s = r0[j % 2], r1[j % 2], xs[j % 2]
            k0, k1, xk = f'r0{j % 2}', f'r1{j % 2}', f'xs{j % 2}'
            sx = f'_{j % 2}'
            stj = st2[j % 2]
            junk_j = a1.bitcast(BF16)[:, 0:2048]
            x2b_j = x_s.bitcast(BF16)[:, 0:2048]
            P.dve(lambda h: h.tensor_scalar(out=a0, in0=g0[j % 2], scalar1=wts[:, j, 0:1], scalar2=None, op0=ALU.mult), r=[f'g0{j % 2}'], w=[k0])
            yield
            P.dve(lambda h: h.scalar_tensor_tensor(out=a0, in0=g1[j % 2], scalar=wts[:, j, 1:2], in1=a0, op0=ALU.mult, op1=ALU.add), r=[k0, k1], w=[k0])
            yield
            P.dve(lambda h: h.scalar_tensor_tensor(out=a0, in0=x_s, scalar=ALPHA, in1=a0, op0=ALU.mult, op1=ALU.add), r=[k0, xk], w=[k0])
            yield
            yield from layer_norm_gen(a0, k0, a0, k0, lnp, 'lnp', junk_j, stj, jk=k1, sx=sx)
            ok_ = uid('xout')
            sdma(xout_d[j * 128:(j + 1) * 128, :], a0, r=[k0], w=[ok_, k0])
            outs.append(ok_)
            yield
            if make_xT:
                P.act(lambda h: h.copy(out=x2b_j, in_=a0), r=[k0], w=[xk])
                yield
                for g4 in range(4):
                    b = nb()
                    for q in range(4):
                        kc = g4 * 4 + q
                        mm(ps[b][:, q * 128:(q + 1) * 128], x2b_j[:, kc * 128:(kc + 1) * 128], ident[:], True, True, r=[xk, 'ident'], w=[f'ps{b}'])
                    if g4 % 2 == 0:
                        P.act(lambda h, b=b, g4=g4: h.copy(out=bigA[:, 4 * g4:4 * g4 + 4, j * 128:(j + 1) * 128],
                                                           in_=ps[b][:, :].rearrange("p (a b) -> p a b", b=128)), r=[f'ps{b}'], w=['xT'])
                    else:
                        P.dve(lambda h, b=b, g4=g4: h.tensor_copy(out=bigA[:, 4 * g4:4 * g4 + 4, j * 128:(j + 1) * 128],
                                                                  in_=ps[b][:, :].rearrange("p (a b) -> p a b", b=128)), r=[f'ps{b}'], w=['xT'])
                    yield
        st2 = [st, AR.f32(8)]
        if make_xT:
            loads(0)
            loads(1)
            for j in range(0, NT, 2):
                zip_run([tile_gen(j), tile_gen(j + 1)])
                if j + 2 < NT:
                    loads(j + 2)
                    loads(j + 3)
        else:
            loads(0)
            for j in range(NT):
                if j + 1 < NT:
                    loads(j + 1)
                zip_run([tile_gen(j)])
        P.barrier()
        return outs

    def phase_a1():
        AR.reset()
        xT, yT = bigA, bigB
        wblk = [AR.bf16(2048).rearrange("p (a b) -> p a b", b=128) for _ in range(2)]
        wcnt = [0]

        def next_w(blk):
            i = wcnt[0] % 2
            wcnt[0] += 1
            load_wblk(wblk[i], din['w_in1'][blk], f'wblk{i}')
            return wblk[i], f'wblk{i}'
        mark = AR.off
        gu = bigB[:, 8:16, :].rearrange("p a (b c) -> p (a b) c", c=1024)
        gv = AR.bf16(16 * 1024).rearrange("p (a b) -> p a b", b=1024)
        glnp = AR.f32(2048).rearrange("p (a b) -> p a b", b=1024)
        wsT = AR.f32(1024).rearrange("p (a b) -> p a b", b=128)
        wsTm = AR.bf16(1024).rearrange("p (a b) -> p a b", b=128)
        bsT = AR.f32(8)
        s1 = AR.f32(128).rearrange("p (a b) -> p a b", b=8)
        s2 = AR.f32(128).rearrange("p (a b) -> p a b", b=8)
        mean, msq, var, rstd = AR.f32(16), AR.f32(16), AR.f32(16), AR.f32(16)
        vn = AR.f32(1024)
        vnb = AR.bf16(1024)
        tmp = AR.f32(1024)
        ycb = AR.bf16(1024)
        tmpf = [AR.f32(128) for _ in range(2)]
        junkb = AR.bf16(128)
        sdma(glnp, din['glnp'], w=['glnp'])
        sdma(wsT, din['wsT'], w=['wsT'])
        sdma(bsT, din['bsT'], w=['bsT'])
        P.dve(lambda h: h.tensor_tensor(out=wsTm, in0=wsT, in1=tril_st[:].unsqueeze(1).to_broadcast([128, 8, 128]), op=ALU.mult),
              r=['wsT', 'tril_st'], w=['wsTm'])
        for ub in range(8):
            wb, wk = next_w(ub)
            proj_tm(xT, 'xT', wb, wk, 128, lambda g, b, ub=ub: P.act(
                lambda h: h.activation(out=gu[:, 4 * g:4 * g + 4, ub * 128:(ub + 1) * 128],
                                       in_=ps[b][:, :].rearrange("p (j c) -> p j c", c=128), func=AF.Gelu), r=[f'ps{b}'], w=['gu']))
        tcnt = [0]
        for vb in range(8):
            wb, wk = next_w(8 + vb)

            def ev_v(g, b, vb=vb):
                for jj in range(4):
                    j = 4 * g + jj
                    tf = tmpf[tcnt[0] % 2]
                    tk = f'tmpf{tcnt[0] % 2}'
                    tcnt[0] += 1
                    P.act(lambda h, jj=jj, j=j, tf=tf: h.activation(out=tf, in_=ps[b][:, jj * 128:(jj + 1) * 128], func=AF.Gelu,
                                                                    accum_out=s1[:, j, vb:vb + 1]), r=[f'ps{b}'], w=[tk, uid('s1')])
                    P.act(lambda h, j=j, tf=tf: h.activation(out=junkb, in_=tf, func=AF.Square, accum_out=s2[:, j, vb:vb + 1]),
                          r=[tk], w=['junkb', uid('s2')])
                    P.dve(lambda h, j=j, tf=tf: h.tensor_copy(out=gv[:, j, vb * 128:(vb + 1) * 128], in_=tf), r=[tk], w=['gv'])
            proj_tm(xT, 'xT', wb, wk, 128, ev_v)
        P.barrier()
        P.dve(lambda h: h.tensor_reduce(out=mean, in_=s1, axis=AX.X, op=ALU.add), w=['mean'])
        P.dve(lambda h: h.tensor_reduce(out=var, in_=s2, axis=AX.X, op=ALU.add), w=['var'])
        P.dve(lambda h: h.tensor_scalar(out=mean, in0=mean, scalar1=1.0 / 1024, scalar2=None, op0=ALU.mult), r=['mean'], w=['mean'])
        P.dve(lambda h: h.tensor_tensor(out=msq, in0=mean, in1=mean, op=ALU.mult), r=['mean'], w=['msq'])
        P.dve(lambda h: h.scalar_tensor_tensor(out=var, in0=var, scalar=1.0 / 1024, in1=msq, op0=ALU.mult, op1=ALU.subtract), r=['var', 'msq'], w=['var'])
        P.act(lambda h: h.activation(out=rstd, in_=var, func=AF.Sqrt, bias=LN_EPS, scale=1.0), r=['var'], w=['rstd'])
        P.dve(lambda h: h.reciprocal(out=rstd, in_=rstd), r=['rstd'], w=['rstd'])
        for j in range(NT):
            P.dve(lambda h, j=j: h.tensor_scalar(out=vn, in0=gv[:, j, :], scalar1=mean[:, j:j + 1], scalar2=rstd[:, j:j + 1],
                                                op0=ALU.subtract, op1=ALU.mult), r=['gv', 'mean', 'rstd'], w=['vn'])
            P.dve(lambda h: h.tensor_tensor(out=vn, in0=vn, in1=glnp[:, 0, :], op=ALU.mult), r=['vn', 'glnp'], w=['vn'])
            P.dve(lambda h: h.tensor_tensor(out=vnb, in0=vn, in1=glnp[:, 1, :], op=ALU.add), r=['vn', 'glnp'], w=['vnb'])
            for half in range(2):
                b = nb()
                for q in range(4):
                    g = half * 4 + q
                    mm(ps[b][:, q * 128:(q + 1) * 128], wsTm[:, g, :], vnb[:, g * 128:(g + 1) * 128], True, True, r=['wsTm', 'vnb'], w=[f'ps{b}'])
                P.dve(lambda h, b=b, half=half: h.tensor_tensor(
                    out=tmp[:, half * 512:(half + 1) * 512].rearrange("p (g c) -> p g c", c=128),
                    in0=ps[b][:, :].rearrange("p (g c) -> p g c", c=128),
                    in1=bsT[:, half * 4:half * 4 + 4].unsqueeze(2).to_broadcast([128, 4, 128]), op=ALU.add), r=[f'ps{b}', 'bsT'], w=[f'tmp{half}'])
                P.pool(lambda h, half=half, j=j: h.tensor_tensor(out=ycb[:, half * 512:(half + 1) * 512], in0=tmp[:, half * 512:(half + 1) * 512],
                                                                in1=gu[:, j, half * 512:(half + 1) * 512], op=ALU.mult), r=[f'tmp{half}', 'gu'], w=[f'ycb{half}'])
            for half in range(2):
                b = nb()
                for q in range(4):
                    g = half * 4 + q
                    mm(ps[b][:, q * 128:(q + 1) * 128], ycb[:, g * 128:(g + 1) * 128], ident[:], True, True, r=[f'ycb{half}', 'ident'], w=[f'ps{b}'])
                if half == 0:
                    P.act(lambda h, b=b, half=half, j=j: h.copy(out=yT[:, 4 * half:4 * half + 4, j * 128:(j + 1) * 128],
                                                                in_=ps[b][:, :].rearrange("p (a b) -> p a b", b=128)), r=[f'ps{b}'], w=['yTc'])
                else:
                    P.dve(lambda h, b=b, half=half, j=j: h.tensor_copy(out=yT[:, 4 * half:4 * half + 4, j * 128:(j + 1) * 128],
                                                                       in_=ps[b][:, :].rearrange("p (a b) -> p a b", b=128)), r=[f'ps{b}'], w=['yTc'])
        P.barrier()
        P.tag = 'phase_a1_conv'
        AR.off = mark
        cw = AR.f32(8 * 31).rearrange("p (a b) -> p a b", b=31)
        cpar = AR.f32(24).rearrange("p (a b) -> p a b", b=8)
        sgf = AR.f32(2048)
        hp = [AR.bf16(30 + 2048) for _ in range(2)]
        Dg = [AR.bf16(31 * 128).rearrange("p (a b) -> p a b", b=128) for _ in range(2)]
        meanT, msqT, varT, rstdT = AR.f32(512), AR.f32(512), AR.f32(512), AR.f32(512)
        zb = [AR.f32(512) for _ in range(2)]
        sqb = [AR.bf16(512) for _ in range(2)]
        sdma(cw, din['cw'], w=['cw'])
        sdma(cpar, din['cpar'], w=['cpar'])
        for i in range(2):
            P.pool(lambda h, i=i: h.memset(hp[i][:, 0:30], 0.0), w=[f'hp{i}'])
        for cc in range(8):
            hpc, hk = hp[cc % 2], f'hp{cc % 2}'
            dgc, dk = Dg[cc % 2], f'Dg{cc % 2}'
            wb, wk = next_w(24 + cc)
            proj_fm(xT, 'xT', wb, wk, lambda tt, b: P.act(
                lambda h: h.activation(out=sgf[:, tt * 512:(tt + 1) * 512], in_=ps[b][:, :], func=AF.Sigmoid), r=[f'ps{b}'], w=['sgf']))
            wb, wk = next_w(16 + cc)
            proj_fm(xT, 'xT', wb, wk, lambda tt, b, hpc=hpc, hk=hk: P.dve(
                lambda h: h.tensor_tensor(out=hpc[:, 30 + tt * 512:30 + (tt + 1) * 512], in0=ps[b][:, :], in1=sgf[:, tt * 512:(tt + 1) * 512], op=ALU.mult),
                r=[f'ps{b}', 'sgf'], w=[hk]))
            P.pool(lambda h, dgc=dgc, cc=cc: h.tensor_tensor(out=dgc, in0=ident[:].unsqueeze(1).to_broadcast([128, 31, 128]),
                                                            in1=cw[:, cc, :].unsqueeze(2).to_broadcast([128, 31, 128]), op=ALU.mult),
                   r=['ident', 'cw'], w=[dk])
            for tt in range(4):
                b = nb()
                for jt in range(31):
                    mm(ps[b][:, :], dgc[:, jt, :], hpc[:, tt * 512 + jt:tt * 512 + jt + 512], jt == 0, jt == 30, r=[dk, hk], w=[f'ps{b}'])
                P.act(lambda h, b=b, cc=cc, tt=tt: h.activation(out=yT[:, 8 + cc, tt * 512:(tt + 1) * 512], in_=ps[b][:, :], func=AF.Identity,
                                                                bias=cpar[:, 0, cc:cc + 1], scale=1.0), r=[f'ps{b}', 'cpar'], w=[f'yd{cc}'])
        scnt = 0
        for tt in range(4):
            ts = slice(tt * 512, (tt + 1) * 512)
            b1 = nb()
            for cc in range(8):
                mm(ps[b1][:, :], ones_bf[:], yT[:, 8 + cc, ts], cc == 0, cc == 7, r=['ones_bf', f'yd{cc}'], w=[f'ps{b1}'])
            b2 = nb()
            for cc in range(8):
                sq_, sk_ = sqb[scnt % 2], f'sqb{scnt % 2}'
                scnt += 1
                P.pool(lambda h, sq_=sq_, cc=cc, ts=ts: h.tensor_tensor(out=sq_, in0=yT[:, 8 + cc, ts], in1=yT[:, 8 + cc, ts], op=ALU.mult),
                       r=[f'yd{cc}'], w=[sk_])
                mm(ps[b2][:, :], ones_bf[:], sq_, cc == 0, cc == 7, r=['ones_bf', sk_], w=[f'ps{b2}'])
            P.act(lambda h, b1=b1: h.mul(out=meanT, in_=ps[b1][:, :], mul=1.0 / 1024), r=[f'ps{b1}'], w=['meanT'])
            P.dve(lambda h: h.tensor_tensor(out=msqT, in0=meanT, in1=meanT, op=ALU.mult), r=['meanT'], w=['msqT'])
            P.dve(lambda h, b2=b2: h.scalar_tensor_tensor(out=varT, in0=ps[b2][:, :], scalar=1.0 / 1024, in1=msqT, op0=ALU.mult, op1=ALU.subtract),
                  r=[f'ps{b2}', 'msqT'], w=['varT'])
            P.act(lambda h: h.activation(out=rstdT, in_=varT, func=AF.Sqrt, bias=LN_EPS, scale=1.0), r=['varT'], w=['rstdT'])
            P.dve(lambda h: h.reciprocal(out=rstdT, in_=rstdT), r=['rstdT'], w=['rstdT'])
            for cc in range(8):
                z_, zk = zb[cc % 2], f'zb{cc % 2}'
                P.dve(lambda h, z_=z_, cc=cc, ts=ts: h.tensor_tensor(out=z_, in0=yT[:, 8 + cc, ts], in1=meanT, op=ALU.subtract),
                      r=[f'yd{cc}', 'meanT'], w=[zk])
                P.dve(lambda h, z_=z_: h.tensor_tensor(out=z_, in0=z_, in1=rstdT, op=ALU.mult), r=[zk, 'rstdT'], w=[zk])
                P.act(lambda h, z_=z_, cc=cc, ts=ts: h.activation(out=yT[:, 8 + cc, ts], in_=z_, func=AF.Silu,
                                                                  bias=cpar[:, 2, cc:cc + 1], scale=cpar[:, 1, cc:cc + 1]),
                      r=[zk, 'cpar'], w=[f'yd{cc}'])
        P.dve(lambda h: h.tensor_copy(out=meanT[:, 0:1], in_=meanT[:, 0:1]), r=['yTc'] + [f'yd{cc}' for cc in range(8)], w=['yT'])
        P.barrier()

    def dump_dram(src_d):
        AR.reset()
        st = [AR.f32(2048) for _ in range(2)]
        keys = []
        for j in range(NT):
            sdma(st[j % 2], src_d[j * 128:(j + 1) * 128, :], w=[f'dst{j % 2}'])
            kk = uid('dbg')
            sdma(dbg_d[j * 128:(j + 1) * 128, :], st[j % 2], r=[f'dst{j % 2}'], w=[kk])
            keys.append(kk)
        P.finish(keys)
        P.emit()
        return nc

    try:
        P.tag = 'phase_a0'
        phase_a0()
    except _Cut:
        pass
    if debug and stage == 0:
        AR.reset()
        st = AR.f32(2048)
        for fc in range(16):
            P.dve(lambda h, fc=fc: h.tensor_copy(out=st, in_=bigB[:, fc, :]), r=['yT'], w=['st'])
            sdma(dbg_d[fc * 128:(fc + 1) * 128, :], st, r=['st'], w=[f'dbg{fc}'])
        P.finish([f'dbg{fc}' for fc in range(16)])
        P.emit()
        return nc
    P.tag = 'phase_c_0'
    phase_c(0, din['x_tm'], x1_d)
    if debug and stage == 1:
        return dump_dram(x1_d)
    P.tag = 'phase_d_0'
    phase_d(0)
    P.tag = 'phase_e_0'
    phase_e(0, x1_d, x2_d, True)
    if debug and stage == 2:
        return dump_dram(x2_d)
    P.tag = 'phase_a1'
    phase_a1()
    if debug and stage == 3:
        AR.reset()
        st = AR.f32(2048)
        for fc in range(16):
            P.dve(lambda h, fc=fc: h.tensor_copy(out=st, in_=bigB[:, fc, :]), r=['yT'], w=['st'])
            sdma(dbg_d[fc * 128:(fc + 1) * 128, :], st, r=['st'], w=[f'dbg{fc}'])
        P.finish([f'dbg{fc}' for fc in range(16)])
        P.emit()
        return nc
    P.tag = 'phase_c_1'
    phase_c(1, x2_d, x3_d)
    P.tag = 'phase_d_1'
    phase_d(1)
    P.tag = 'phase_e_1'
    outs = phase_e(1, x3_d, out_d, False)
    P.finish(outs)
    P.emit()
    k.nops = len(P.ops)
    return nc


def kernel(**inputs):
    inp = {k: np.asarray(v) for k, v in inputs.items()}
    sh = prep_shared(inp)
    x = inp['x']
    in_maps = []
    for b in range(8):
        m = dict(sh)
        m['x_tm'] = np.ascontiguousarray(x[b])
        m['xT'] = np.ascontiguousarray(x[b].T)
        in_maps.append(m)
    nc = build()
    res = run_bass_kernel_spmd(nc, in_maps, core_ids=list(range(8)))
    return np.stack([np.asarray(r['out']) for r in res.results], axis=0).astype(np.float32)
```
